# Optimizing a Trainium2 kernel written in Bass

```python
import jax, jax.numpy as jnp
from jax import lax
import numpy as np

D_MODEL = 1024
BATCH = 16
SEQ = 2048
DEPTH = 4

N_EVEN = (DEPTH + 1) // 2
N_ODD = DEPTH // 2
EPS = 1e-6

RET_HEADS = 4
RET_DK = 64
RET_DV = 128
RET_CHUNK = 128
ROPE_BASE = 10000.0
SB_HEADS = 8
SB_DH = 64
SB_BLOCK = 128
HG_HEADS = 4
HG_DK = 128
HG_DV = 128
HG_CHUNK = 16
LRU_WIDTH = 512
LRU_BLOCKS = 8
LRU_BW = LRU_WIDTH // LRU_BLOCKS
CONV_W = 4
LRU_C = 8.0

RET_QK = RET_HEADS * RET_DK
RET_V = RET_HEADS * RET_DV
SB_W = SB_HEADS * SB_DH
EVEN_SIZES = (RET_QK, RET_QK, RET_V, RET_V, SB_W, SB_W, SB_W, SB_W)
EVEN_IN = sum(EVEN_SIZES)
EVEN_MIX = RET_V + SB_W
HG_KW = HG_HEADS * HG_DK
HG_VW = HG_HEADS * HG_DV
ODD_SIZES = (HG_KW, HG_KW, HG_VW, HG_VW, LRU_WIDTH, LRU_WIDTH)
ODD_IN = sum(ODD_SIZES)
ODD_MIX = HG_VW + LRU_WIDTH

kernel_name = 'hybrid_retention_stickbreak_hgrn2_rglru'


def split_cols(a, sizes):
    idx = [int(v) for v in np.cumsum(sizes)[:-1]]
    return jnp.split(a, idx, axis=-1)


def rms_norm(x, w=None):
    xf = x.astype(jnp.float32)
    y = xf * lax.rsqrt(jnp.mean(xf * xf, axis=-1, keepdims=True) + EPS)
    if w is not None:
        y = y * w.astype(jnp.float32)
    return y


def rotary(x):
    S, D = x.shape[1], x.shape[-1]
    half = D // 2
    inv = ROPE_BASE ** (-jnp.arange(half, dtype=jnp.float32) / half)
    ang = jnp.arange(S, dtype=jnp.float32)[:, None] * inv[None, :]
    cos = jnp.cos(ang)[None, :, None, :]
    sin = jnp.sin(ang)[None, :, None, :]
    x1, x2 = x[..., :half], x[..., half:]
    return jnp.concatenate([x1 * cos - x2 * sin, x1 * sin + x2 * cos], axis=-1)


def retention(q, k, v):
    B, S, H, dk = q.shape
    dv = v.shape[-1]
    C = RET_CHUNK
    N = S // C
    f32 = jnp.float32
    log_g = jnp.log(1.0 - 2.0 ** (-5.0 - jnp.arange(H, dtype=f32)))
    q = rotary(q.astype(f32))
    k = rotary(k.astype(f32)) * (dk ** -0.5)
    v = v.astype(f32)
    to_chunks = lambda a: a.reshape(B, N, C, H, a.shape[-1]).transpose(0, 3, 1, 2, 4)
    qc, kc, vc = to_chunks(q), to_chunks(k), to_chunks(v)
    pos = jnp.arange(C, dtype=f32)
    dist = pos[:, None] - pos[None, :]
    decay = jnp.where(dist >= 0, jnp.exp(log_g[:, None, None] * jnp.maximum(dist, 0.0)), 0.0)
    scores = jnp.einsum('bhnid,bhnjd->bhnij', qc, kc) * decay[None, :, None]
    o_intra = jnp.einsum('bhnij,bhnje->bhnie', scores, vc)
    k_decay = jnp.exp(log_g[:, None] * (C - 1.0 - pos)[None, :])
    q_decay = jnp.exp(log_g[:, None] * (pos + 1.0)[None, :])
    chunk_kv = jnp.einsum('bhnjd,hj,bhnje->nbhde', kc, k_decay, vc)
    g_chunk = jnp.exp(log_g * C)[None, :, None, None]

    def step(state, kv):
        return state * g_chunk + kv, state

    _, r_prev = lax.scan(step, jnp.zeros((B, H, dk, dv), f32), chunk_kv)
    o_inter = jnp.einsum('bhnid,hi,nbhde->bhnie', qc, q_decay, r_prev)
    return (o_intra + o_inter).transpose(0, 2, 3, 1, 4).reshape(B, S, H, dv)


def stick_breaking(q, k, v):
    B, S, H, d = q.shape
    f32 = jnp.float32
    qf, kf, vf = q.astype(f32), k.astype(f32), v.astype(f32)
    scale = d ** -0.5
    outs = []
    for blk in range(S // SB_BLOCK):
        q0 = blk * SB_BLOCK
        kend = q0 + SB_BLOCK
        z = jnp.einsum('bthd,bshd->bhts', qf[:, q0:kend], kf[:, :kend]) * scale
        t_idx = q0 + jnp.arange(SB_BLOCK)[:, None]
        s_idx = jnp.arange(kend)[None, :]
        strict = s_idx < t_idx
        log_beta = jax.nn.log_sigmoid(z)
        log_one_minus = jnp.where(strict, jax.nn.log_sigmoid(-z), 0.0)
        suffix = lax.cumsum(log_one_minus, axis=3, reverse=True) - log_one_minus
        w = jnp.where(strict, jnp.exp(log_beta + suffix), 0.0)
        outs.append(jnp.einsum('bhts,bshd->bthd', w, vf[:, :kend]))
    return jnp.concatenate(outs, axis=1)


def hgrn2(q, f_logit, i, lb):
    B, S, H, dk = q.shape
    dv = i.shape[-1]
    C = HG_CHUNK
    N = S // C
    f32 = jnp.float32
    lb = lb.reshape(H, dk).astype(f32)
    f_logit = f_logit.astype(f32)
    log_f = jnp.logaddexp(jnp.log(lb), jnp.log1p(-lb) + jax.nn.log_sigmoid(f_logit))
    k = (1.0 - lb) * jax.nn.sigmoid(-f_logit)
    to_chunks = lambda a: a.reshape(B, N, C, H, a.shape[-1]).transpose(1, 0, 3, 2, 4)
    xs = (to_chunks(q.astype(f32)), to_chunks(k), to_chunks(log_f), to_chunks(i.astype(f32)))
    tril = jnp.tril(jnp.ones((C, C), dtype=bool))[:, :, None]

    def step(state, inp):
        qc, kc, lfc, vc = inp
        b = jnp.cumsum(lfc, axis=2)
        diff = b[:, :, :, None, :] - b[:, :, None, :, :]
        decay = jnp.exp(jnp.where(tril, diff, -jnp.inf))
        attn = jnp.einsum('bhtk,bhtsk,bhsk->bhts', qc, decay, kc)
        o = jnp.einsum('bhts,bhsv->bhtv', attn, vc) + jnp.einsum('bhtk,bhkv->bhtv', qc * jnp.exp(b), state)
        b_last = b[:, :, -1:, :]
        state = state * jnp.exp(b_last[:, :, 0, :, None]) + jnp.einsum('bhsk,bhsv->bhkv', kc * jnp.exp(b_last - b), vc)
        return state, o

    _, o = lax.scan(step, jnp.zeros((B, H, dk, dv), f32), xs)
    return o.transpose(1, 0, 3, 2, 4).reshape(B, S, H, dv)


def causal_conv(x, w, b):
    S = x.shape[1]
    xp = jnp.pad(x, ((0, 0), (CONV_W - 1, 0), (0, 0)))
    y = b[None, None, :] + xp[:, 0:S] * w[0]
    for j in range(1, CONV_W):
        y = y + xp[:, j:j + S] * w[j]
    return y


def rg_lru(x, w_a, b_a, w_x, b_x, lam):
    B, S, W = x.shape
    xb = x.reshape(B, S, LRU_BLOCKS, LRU_BW)
    r = jax.nn.sigmoid(jnp.einsum('bsni,nij->bsnj', xb, w_a).reshape(B, S, W) + b_a)
    ig = jax.nn.sigmoid(jnp.einsum('bsni,nij->bsnj', xb, w_x).reshape(B, S, W) + b_x)
    log_a = -LRU_C * r * jax.nn.softplus(-lam)
    a = jnp.exp(log_a)
    mult = jnp.sqrt(-jnp.expm1(2.0 * log_a))
    mult = jnp.where(jnp.arange(S)[None, :, None] == 0, 1.0, mult)
    u = mult * ig * x

    def combine(left, right):
        a1, b1 = left
        a2, b2 = right
        return a1 * a2, a2 * b1 + b2

    _, h = lax.associative_scan(combine, (a, u), axis=1)
    return h


def even_mixer(h, w_in, w_out):
    B, S, _ = h.shape
    proj = jnp.einsum('bsd,de->bse', h.astype(w_in.dtype), w_in).astype(jnp.float32)
    rq, rk, rv, rg, sq, sk, sv, sg = split_cols(proj, EVEN_SIZES)
    ret = retention(rq.reshape(B, S, RET_HEADS, RET_DK), rk.reshape(B, S, RET_HEADS, RET_DK),
                    rv.reshape(B, S, RET_HEADS, RET_DV))
    ret = rms_norm(ret).reshape(B, S, RET_V) * jax.nn.silu(rg)
    sb = stick_breaking(sq.reshape(B, S, SB_HEADS, SB_DH), sk.reshape(B, S, SB_HEADS, SB_DH),
                        sv.reshape(B, S, SB_HEADS, SB_DH))
    sb = sb.reshape(B, S, SB_W) * jax.nn.silu(sg)
    mix = jnp.concatenate([ret, sb], axis=-1)
    return jnp.einsum('bse,ed->bsd', mix.astype(w_out.dtype), w_out)


def odd_mixer(h, w_in, w_out, lb, conv_w, conv_b, w_a, b_a, w_x, b_x, lam):
    B, S, _ = h.shape
    f32 = jnp.float32
    proj = jnp.einsum('bsd,de->bse', h.astype(w_in.dtype), w_in).astype(f32)
    hq, hf, hi, hg, lx, lg = split_cols(proj, ODD_SIZES)
    hc = hgrn2(hq.reshape(B, S, HG_HEADS, HG_DK), hf.reshape(B, S, HG_HEADS, HG_DK),
               hi.reshape(B, S, HG_HEADS, HG_DV), lb)
    hc = rms_norm(hc).reshape(B, S, HG_VW) * jax.nn.silu(hg)
    xc = causal_conv(lx, conv_w.astype(f32), conv_b.astype(f32))
    hl = rg_lru(xc, w_a.astype(f32), b_a.astype(f32), w_x.astype(f32), b_x.astype(f32), lam.astype(f32))
    hl = hl * jax.nn.silu(lg)
    mix = jnp.concatenate([hc, hl], axis=-1)
    return jnp.einsum('bse,ed->bsd', mix.astype(w_out.dtype), w_out)


def setup_inputs(seed: int = 0) -> dict:
    key = jax.random.key(seed)
    ks = jax.random.split(key, 16)
    f32 = jnp.float32
    nrm = lambda k, shape, s: jax.random.normal(k, shape, f32) * s
    u = jax.random.uniform(ks[15], (N_ODD, LRU_WIDTH), f32, 0.9, 0.999)
    a0 = u ** (1.0 / LRU_C)
    return {
        'x': nrm(ks[0], (BATCH, SEQ, D_MODEL), 1.0),
        'pre_norm_w': 1.0 + nrm(ks[1], (DEPTH, D_MODEL), 0.02),
        'post_norm_w': 1.0 + nrm(ks[2], (DEPTH, D_MODEL), 0.02),
        'even_w_in': nrm(ks[3], (N_EVEN, D_MODEL, EVEN_IN), D_MODEL ** -0.5),
        'even_w_out': nrm(ks[4], (N_EVEN, EVEN_MIX, D_MODEL), EVEN_MIX ** -0.5),
        'odd_w_in': nrm(ks[5], (N_ODD, D_MODEL, ODD_IN), D_MODEL ** -0.5),
        'odd_w_out': nrm(ks[6], (N_ODD, ODD_MIX, D_MODEL), ODD_MIX ** -0.5),
        'hgrn_lb_logits': nrm(ks[7], (N_ODD, HG_KW), 1.0),
        'conv_w': nrm(ks[8], (N_ODD, CONV_W, LRU_WIDTH), CONV_W ** -0.5),
        'conv_b': nrm(ks[9], (N_ODD, LRU_WIDTH), 0.01),
        'lru_w_a': nrm(ks[10], (N_ODD, LRU_BLOCKS, LRU_BW, LRU_BW), LRU_BW ** -0.5),
        'lru_b_a': nrm(ks[11], (N_ODD, LRU_WIDTH), 0.01),
        'lru_w_x': nrm(ks[12], (N_ODD, LRU_BLOCKS, LRU_BW, LRU_BW), LRU_BW ** -0.5),
        'lru_b_x': nrm(ks[13], (N_ODD, LRU_WIDTH), 0.01),
        'lru_lambda': jnp.log(a0) - jnp.log1p(-a0),
    }


def reference(x, pre_norm_w, post_norm_w, even_w_in, even_w_out, odd_w_in, odd_w_out,
              hgrn_lb_logits, conv_w, conv_b, lru_w_a, lru_b_a, lru_w_x, lru_b_x, lru_lambda):
    cum = jnp.cumsum(jax.nn.softmax(hgrn_lb_logits.astype(jnp.float32), axis=0), axis=0)
    lower_bounds = cum - cum[0:1]
    for layer in range(DEPTH):
        h = rms_norm(x, pre_norm_w[layer])
        if layer % 2 == 0:
            e = layer // 2
            y = even_mixer(h, even_w_in[e], even_w_out[e])
        else:
            o = layer // 2
            y = odd_mixer(h, odd_w_in[o], odd_w_out[o], lower_bounds[o], conv_w[o], conv_b[o],
                          lru_w_a[o], lru_b_a[o], lru_w_x[o], lru_b_x[o], lru_lambda[o])
        x = x + rms_norm(y, post_norm_w[layer]).astype(x.dtype)
    return x
```

```python
import numpy as np
import ml_dtypes
from contextlib import ExitStack
import concourse.bass as bass
import concourse.mybir as mybir
from concourse.bass_utils import run_bass_kernel_spmd

F32 = mybir.dt.float32
BF16 = mybir.dt.bfloat16
AF = mybir.ActivationFunctionType
ALU = mybir.AluOpType
PE, ACT, DVE, POOL, SP = "tensor", "scalar", "vector", "gpsimd", "sync"
ENGS = [PE, ACT, DVE, POOL, SP]

NCORES = 8
SEQ = 2048
D = 1024
NSEQ = 2
EPS = 1e-6
HC = 32
POOL_COMPUTE = True
LAYER_GROUPS = [[0, 1, 2, 3]]


def _is_psum_key(k):
    if isinstance(k, str):
        return k.startswith("pm") or k == "pT"
    return isinstance(k, tuple) and k[0] == "pj"


class Prog:
    def __init__(self, nc, es):
        self.nc = nc
        self.es = es
        self.stream = {e: [] for e in ENGS}
        self.cnt = {}
        self.sem = {}
        self.mult = {}
        self.clock = {e: {} for e in ENGS}
        self.iclock = {}
        self.lastw = {}
        self.readers = {}
        self.marked = set()
        self.pending = {}
        for e in ENGS:
            self._mkq(e, 1)

    def _mkq(self, name, mult):
        self.cnt[name] = 0
        self.sem[name] = self.es.enter_context(self.nc.semaphore("s_" + name))
        self.mult[name] = mult

    def op(self, eng, fn, reads=(), writes=(), vq=None, inc=True):
        me = vq or eng
        if vq is not None and vq not in self.cnt:
            self._mkq(vq, 16)
        deps = {}

        def need(t):
            e, i = t
            if i > deps.get(e, 0):
                deps[e] = i

        for r in reads:
            lw = self.lastw.get(r)
            if lw:
                need(lw)
            if _is_psum_key(r):
                for e2, i2 in self.readers.get(r, {}).items():
                    if e2 != me:
                        need((e2, i2))
        for w in writes:
            lw = self.lastw.get(w)
            if lw and (lw[0] != me or me == POOL):
                need(lw)
            for e2, i2 in self.readers.get(w, {}).items():
                need((e2, i2))
        if self.pending.get(eng):
            for e2, i2 in self.pending.pop(eng).items():
                need((e2, i2))
        clk = self.clock[eng]
        waits = []
        for e, i in deps.items():
            if e == PE and me == PE:
                continue
            if clk.get(e, 0) >= i:
                continue
            waits.append((e, i))
            snap = self.iclock.get((e, i))
            if snap:
                for k, v in snap.items():
                    if clk.get(k, 0) < v:
                        clk[k] = v
            clk[e] = max(clk.get(e, 0), i)
        self.cnt[me] += 1
        idx = self.cnt[me]
        self.iclock[(me, idx)] = dict(clk)
        self.stream[eng].append((waits, fn, me, idx))
        for w_ in waits:
            self.marked.add(w_)
        if self.mult[me] == 16:
            self.marked.add((me, idx))
        for r in reads:
            d = self.readers.setdefault(r, {})
            if d.get(me, 0) < idx:
                d[me] = idx
        for w in writes:
            self.lastw[w] = (me, idx)
            self.readers[w] = {}
        return idx

    def barrier(self, engs=(PE, ACT, DVE, POOL), extra=()):
        deps = {e: self.cnt[e] for e in engs if self.cnt[e] > 0}
        for k in extra:
            lw = self.lastw.get(k)
            if lw:
                deps[lw[0]] = max(deps.get(lw[0], 0), lw[1])
        for e in engs:
            d = self.pending.setdefault(e, {})
            for e2, i2 in deps.items():
                if e2 != e and d.get(e2, 0) < i2:
                    d[e2] = i2

    def emit(self):
        nc = self.nc
        mcount = {}
        for q_ in self.cnt:
            c = 0
            for i in range(1, self.cnt[q_] + 1):
                if (q_, i) in self.marked:
                    c += 1
                    mcount[(q_, i)] = c
        with nc.Block() as block:
            for eng in ENGS:
                stream = self.stream[eng]
                if not stream:
                    continue

                def body(q, stream=stream):
                    for waits, fn, me, idx in stream:
                        for w_ in waits:
                            q.wait_ge(self.sem[w_[0]], mcount[w_] * self.mult[w_[0]])
                        ins = fn(q)
                        if (me, idx) in mcount:
                            ins.then_inc(self.sem[me], self.mult[me])

                getattr(block, eng)(body)


def _consts():
    bf = ml_dtypes.bfloat16
    c = {}
    idx = np.arange(128)
    c["c_idf"] = np.eye(128, dtype=np.float32)
    cb = np.zeros((128, 6, 128), np.float32)
    cb[:, 0] = np.eye(128)
    cb[:, 1] = 1.0
    cb[:, 2] = (idx[:, None] >= idx[None, :])
    cb[:, 3] = (idx[:, None] < idx[None, :])
    c["c_bf"] = cb.astype(bf)
    cm = np.zeros((128, 3, 128), np.float32)
    cm[:, 0] = (idx[None, :] >= idx[:, None])
    cm[:, 1] = (idx[:, None] < idx[None, :])
    cm[:, 2] = ((idx[:, None] // HC) == (idx[None, :] // HC)) & (idx[None, :] >= idx[:, None])
    c["c_mask"] = cm
    half = 32
    inv = (10000.0 ** (-(np.arange(half, dtype=np.float32)) / half)).astype(np.float32)
    pos = np.arange(SEQ, dtype=np.float32)
    ang = (pos[:, None] * inv[None, :]).astype(np.float32)
    cs = np.zeros((128, 2, 16, 32), np.float32)
    cs[:, 0] = np.cos(ang).astype(np.float32).reshape(16, 128, 32).transpose(1, 0, 2)
    cs[:, 1] = np.sin(ang).astype(np.float32).reshape(16, 128, 32).transpose(1, 0, 2)
    c["c_rope"] = cs
    g = 1.0 - 2.0 ** (-5.0 - np.arange(4, dtype=np.float64))
    G = np.zeros((128, 2, 4, 64), np.float64)
    gc = np.zeros((128, 2), np.float64)
    for pr in range(2):
        for hh in range(2):
            h = 2 * pr + hh
            G[:, pr, hh, :] = (g[h] ** idx)[:, None]
            G[:, pr, 2 + hh, :] = (g[h] ** (-idx.astype(np.float64)))[:, None] * (64 ** -0.5)
            gc[hh * 64:(hh + 1) * 64, pr] = g[h] ** 128
    c["c_G"] = G.astype(np.float32)
    c["c_gc"] = gc.astype(np.float32)
    rm = np.ones((128, 512), np.float32)
    rm[:, ::HC] = 0.0
    c["c_reset"] = rm.astype(bf)
    nck = 128 // HC
    c["c_cmask"] = ((idx[:, None] // HC) == np.arange(nck)[None, :]).astype(np.float32).astype(bf)
    return c


def _params(inp):
    p = {}
    f = lambda a: np.ascontiguousarray(a, dtype=np.float32)
    p["p_pre"] = f(inp["pre_norm_w"].reshape(4, 8, 128).transpose(2, 0, 1))
    p["p_post"] = f(inp["post_norm_w"].reshape(4, 8, 128).transpose(2, 0, 1))
    p["p_lbl"] = f(inp["hgrn_lb_logits"].reshape(2, 4, 128).transpose(2, 0, 1))
    p["p_cw"] = f(inp["conv_w"].reshape(2, 4, 4, 128).transpose(3, 0, 1, 2))
    p["p_cb"] = f(inp["conv_b"].reshape(2, 4, 128).transpose(2, 0, 1))
    p["p_ba"] = f(inp["lru_b_a"].reshape(2, 4, 128).transpose(2, 0, 1))
    p["p_bx"] = f(inp["lru_b_x"].reshape(2, 4, 128).transpose(2, 0, 1))
    p["p_lam"] = f(inp["lru_lambda"].reshape(2, 4, 128).transpose(2, 0, 1))
    for nm, src in (("p_wa", inp["lru_w_a"]), ("p_wx", inp["lru_w_x"])):
        bd = np.zeros((2, 128, 4, 128), np.float32)
        for o in range(2):
            for j in range(4):
                bd[o, 0:64, j, 0:64] = src[o, 2 * j]
                bd[o, 64:128, j, 64:128] = src[o, 2 * j + 1]
        p[nm] = bd
    return p


def build(layers, nseq=NSEQ, debug_skip=()):
    if 'one' in debug_skip:
        nseq = 1
    nc = bass.Bass("TRN2", target_bir_lowering=False)
    consts = _consts()
    dram = {}

    def din(name, shape, dt=F32):
        dram[name] = nc.dram_tensor(name, list(shape), dt, kind="ExternalInput").ap()
        return dram[name]

    x_d = din("x", [nseq, SEQ, D])
    out_d = nc.dram_tensor("out", [nseq, SEQ, D], F32, kind="ExternalOutput").ap()
    dbg_d = nc.dram_tensor("dbg", [128, 8, SEQ], BF16, kind="ExternalOutput").ap() if "dbg" in debug_skip else None
    ewin = din("even_w_in", [2, 1024, 3584])
    ewout = din("even_w_out", [2, 1024, 1024])
    owin = din("odd_w_in", [2, 1024, 3072])
    owout = din("odd_w_out", [2, 1024, 1024])
    for k, v in consts.items():
        din(k, v.shape, BF16 if v.dtype == ml_dtypes.bfloat16 else F32)
    pshapes = {"p_pre": [128, 4, 8], "p_post": [128, 4, 8], "p_lbl": [128, 2, 4], "p_cw": [128, 2, 4, 4],
               "p_cb": [128, 2, 4], "p_ba": [128, 2, 4], "p_bx": [128, 2, 4], "p_lam": [128, 2, 4],
               "p_wa": [2, 128, 4, 128], "p_wx": [2, 128, 4, 128]}
    for k, s in pshapes.items():
        din(k, s)

    es = ExitStack()
    with es:
        P = Prog(nc, es)

        def sb(name, shape, dt):
            return es.enter_context(nc.sbuf_tensor(name, list(shape), dt))

        def psum(name, shape, dt):
            return es.enter_context(nc.psum_tensor(name, list(shape), dt))

        X = sb("X", [128, 8, SEQ], F32)
        hT = sb("hT", [128, 8, SEQ], BF16)
        mixT = sb("mixT", [128, 8, SEQ], BF16)
        wg = [sb(f"wg{i}", [128, 8, 512], BF16) for i in range(3)]
        NSTG = 4
        stg = [sb(f"stg{i}", [128, 512], F32) for i in range(NSTG)]
        idf = sb("idf", [128, 128], F32)
        cbf = sb("cbf", [128, 6, 128], BF16)
        cmask = sb("cmask", [128, 3, 128], F32)
        rope = sb("rope", [128, 2, 16, 32], F32)
        Gt = sb("Gt", [128, 2, 256], F32)
        gct = sb("gct", [128, 2], F32)
        resetm = sb("resetm", [128, 512], BF16)
        ckm = sb("ckm", [128, 128 // HC], BF16)
        pre32 = sb("pre32", [128, 4, 8], F32)
        post32 = sb("post32", [128, 4, 8], F32)
        lbt = sb("lbt", [128, 2, 4], F32)
        omlt = sb("omlt", [128, 2, 4], F32)
        cwt = sb("cwt", [128, 2, 4, 4], F32)
        cbt = sb("cbt", [128, 2, 4], F32)
        bat = sb("bat", [128, 2, 4], F32)
        bxt = sb("bxt", [128, 2, 4], F32)
        nbat = sb("nbat", [128, 2, 4], F32)
        nbxt = sb("nbxt", [128, 2, 4], F32)
        nspt = sb("nspt", [128, 2, 4], F32)
        nsp2t = sb("nsp2t", [128, 2, 4], F32)
        wabd = sb("wabd", [128, 2, 4, 128], BF16)
        wxbd = sb("wxbd", [128, 2, 4, 128], BF16)
        lblt = sb("lblt", [128, 2, 4], F32)
        ARENA = 32 * 1024
        arena = sb("arena", [128, ARENA // 4], F32)

        pj = [psum(f"pj{i}", [128, 512], F32) for i in range(2)]
        pm = [psum(f"pm{i}", [128, 512], F32) for i in range(5)]
        pT = psum("pT", [128, 1024], BF16)

        ident = cbf[:, 0, :]
        ones = cbf[:, 1, :]
        tri = cbf[:, 2, :]
        cpl = cbf[:, 3, :]

        class Arena:
            def __init__(self):
                self.off = 0

            def reset(self):
                self.off = 0

            def take(self, shape, dt):
                n = int(np.prod(shape))
                nbytes = n * (2 if dt == BF16 else 4)
                nbytes = (nbytes + 63) // 64 * 64
                assert self.off + nbytes <= ARENA, (self.off, nbytes)
                w0 = self.off // 4
                ap = arena[:, w0:w0 + nbytes // 4]
                self.off += nbytes
                if dt == BF16:
                    ap = ap.bitcast(BF16)
                return ap[:, 0:n]

        AR = Arena()
        _uid = [0]

        def tile(shape, dt):
            ap = AR.take(shape, dt)
            if len(shape) == 2:
                ap = ap.rearrange("p (a b) -> p a b", a=shape[0], b=shape[1])
            elif len(shape) == 3:
                ap = ap.rearrange("p (a b c) -> p a b c", a=shape[0], b=shape[1], c=shape[2])
            _uid[0] += 1
            return ap, ("t", _uid[0])

        def phase_end(extra=(), sp_too=False):
            P.barrier(engs=(PE, ACT, DVE, POOL, SP) if sp_too else (PE, ACT, DVE, POOL), extra=extra)
            AR.reset()

        def mm(out, lhsT, rhs, start, stop, R, W, inc=True):
            P.op(PE, lambda q: q.matmul(out, lhsT=lhsT, rhs=rhs, start=start, stop=stop, skip_group_check=True),
                 reads=R, writes=W, inc=inc)

        def tr(out, in_, idn, R, W):
            P.op(PE, lambda q: q.transpose(out=out, in_=in_, identity=idn), reads=R, writes=W)

        def act(out, in_, func, R, W, scale=1.0, bias=0.0):
            P.op(ACT, lambda q: q.activation(out=out, in_=in_, func=func, bias=bias, scale=scale), reads=R, writes=W)

        def tt(eng, out, in0, in1, op, R, W):
            if eng == POOL and not POOL_COMPUTE:
                eng = DVE
            P.op(eng, lambda q: q.tensor_tensor(out=out, in0=in0, in1=in1, op=op), reads=R, writes=W)

        def ts(eng, out, in0, s1, s2, op0, op1, R, W):
            if op1 is None:
                P.op(eng, lambda q: q.tensor_scalar(out=out, in0=in0, scalar1=s1, scalar2=None, op0=op0), reads=R, writes=W)
            else:
                P.op(eng, lambda q: q.tensor_scalar(out=out, in0=in0, scalar1=s1, scalar2=s2, op0=op0, op1=op1), reads=R, writes=W)

        def stt(out, in0, scalar, in1, op0, op1, R, W):
            P.op(DVE, lambda q: q.scalar_tensor_tensor(out=out, in0=in0, scalar=scalar, in1=in1, op0=op0, op1=op1), reads=R, writes=W)

        def cp(eng, out, in_, R, W):
            if eng == POOL and not POOL_COMPUTE:
                eng = DVE
            if eng == ACT:
                P.op(ACT, lambda q: q.copy(out=out, in_=in_), reads=R, writes=W)
            else:
                P.op(eng, lambda q: q.tensor_copy(out=out, in_=in_), reads=R, writes=W)

        def recip(out, in_, R, W):
            P.op(DVE, lambda q: q.reciprocal(out=out, in_=in_), reads=R, writes=W)

        def scan(out, d0, d1, init, R, W):
            P.op(DVE, lambda q: q.tensor_tensor_scan(out=out, data0=d0, data1=d1, initial=init, op0=ALU.mult, op1=ALU.add), reads=R, writes=W)

        def memset(eng, ap, val, W):
            P.op(eng, lambda q: q.memset(ap, val), writes=W)

        def dma(eng, vq, out, in_, R, W):
            P.op(eng, lambda q: q.dma_start(out=out, in_=in_), reads=R, writes=W, vq=vq)

        cload = [(idf[:], "c_idf"), (cbf[:], "c_bf"), (cmask[:], "c_mask"), (rope[:], "c_rope"),
                 (Gt[:].rearrange("p a (b c) -> p a b c", b=4, c=64), "c_G"), (gct[:], "c_gc"), (resetm[:], "c_reset"),
                 (ckm[:], "c_cmask"), (pre32[:], "p_pre"), (post32[:], "p_post"), (lblt[:], "p_lbl"), (cwt[:], "p_cw"),
                 (cbt[:], "p_cb"), (bat[:], "p_ba"), (bxt[:], "p_bx"), (nspt[:], "p_lam")]
        for i, (dst, nm) in enumerate(cload):
            dma(SP, "dq_c", dst, dram[nm], [], ["consts"])
        for o in range(2):
            dma(POOL, "dq_c2", wabd[:, o, :, :], dram["p_wa"][o], [], ["consts2"])
            dma(POOL, "dq_c2", wxbd[:, o, :, :], dram["p_wx"][o], [], ["consts2"])
        CR = ["consts", "consts2"]
        memset(DVE, lbt[:, 0, :], 0.0, ["consts"])
        tt(DVE, lbt[:, 1, :], lblt[:, 1, :], lblt[:, 0, :], ALU.subtract, CR, ["consts"])
        act(lbt[:, 1, :], lbt[:, 1, :], AF.Exp, CR, ["consts"], scale=-1.0)
        ts(DVE, lbt[:, 1, :], lbt[:, 1, :], 1.0, None, ALU.add, None, CR, ["consts"])
        recip(lbt[:, 1, :], lbt[:, 1, :], CR, ["consts"])
        ts(DVE, omlt[:], lbt[:], -1.0, 1.0, ALU.mult, ALU.add, CR, ["consts"])
        ts(DVE, nbat[:], bat[:], -1.0, None, ALU.mult, None, CR, ["consts"])
        ts(DVE, nbxt[:], bxt[:], -1.0, None, ALU.mult, None, CR, ["consts"])
        act(nspt[:], nspt[:], AF.Exp, CR, ["consts"], scale=-1.0)
        act(nspt[:], nspt[:], AF.Ln, CR, ["consts"], bias=1.0)
        ts(DVE, nsp2t[:], nspt[:], -16.0, None, ALU.mult, None, CR, ["consts"])
        ts(DVE, nspt[:], nspt[:], -8.0, None, ALU.mult, None, CR, ["consts"])
        P.barrier()

        wstate = {"n": 0, "s": 0}

        def issue_load(pieces):
            slot = wstate["n"] % 3
            wstate["n"] += 1
            c = 0
            for (w2d, c0, ncols) in pieces:
                kk = 512 // ncols
                src = w2d[:, c0:c0 + ncols].rearrange("(k p) c -> p k c", p=128)
                for k0 in range(0, 8, kk):
                    si = wstate["s"] % NSTG
                    wstate["s"] += 1
                    st3 = stg[si][:, :].rearrange("p (k c) -> p k c", k=kk)
                    dma(SP, f"dq_s{si}", st3, src[:, k0:k0 + kk, :], [], [("stg", si)])
                    P.op(POOL, lambda q, o=wg[slot][:, k0:k0 + kk, c:c + ncols], i=st3: q.tensor_copy(out=o, in_=i),
                         reads=[("stg", si)], writes=[("wg", slot)])
                c += ncols
            return slot

        pjn = [0]

        def next_pj():
            pjn[0] += 1
            i = pjn[0] % 2
            return pj[i], ("pj", i)

        def proj_fm(slot, col0, g, bank, bkey, ncols=128):
            for k in range(8):
                mm(bank[0:ncols, :], wg[slot][:, k, col0:col0 + ncols], hT[:, k, g * 512:(g + 1) * 512], k == 0, k == 7,
                   [("wg", slot), "hT"], [bkey], inc=(k == 7))

        def proj_tm(slot, col0, ncols, n, out, bkey):
            for k in range(8):
                mm(out, hT[:, k, n * 128:(n + 1) * 128], wg[slot][:, k, col0:col0 + ncols], k == 0, k == 7,
                   [("wg", slot), "hT"], [bkey], inc=(k == 7))

        def load_x(s):
            xin = [tile([1024], F32) for _ in range(2)]
            for n in range(1 if "l1" in debug_skip else (3 if "l3" in debug_skip else 16)):
                xt, xk = xin[n % 2]
                dma(SP, f"dq_x{n % 2}", xt, x_d[s, n * 128:(n + 1) * 128, :], [], [xk])
                for half in range(2):
                    bank, bkey = next_pj()
                    for cc in range(4):
                        c = half * 4 + cc
                        tr(bank[:, cc * 128:(cc + 1) * 128], xt[:, c * 128:(c + 1) * 128], idf[:], [xk], [bkey])
                    dst = X[:, half * 4:half * 4 + 4, n * 128:(n + 1) * 128]
                    if "lnocp" in debug_skip:
                        continue
                    if "l2d" in debug_skip:
                        for cc in range(4):
                            dd = X[:, half * 4 + cc, n * 128:(n + 1) * 128] if "lsc" not in debug_skip else hT[:, half * 4 + cc, 0:256].bitcast(F32)
                            cp(ACT if half == 0 else DVE, dd, bank[:, cc * 128:(cc + 1) * 128], [bkey], [("X", half * 4 + cc)])
                    else:
                        cp(ACT if half == 0 else DVE, dst, bank[:, :].rearrange("p (a b) -> p a b", a=4), [bkey], [("X", half * 4 + i) for i in range(4)])
            phase_end()

        def store_x(s):
            xo = [tile([1024], F32) for _ in range(2)]
            for n in range(16):
                xt, xk = xo[n % 2]
                for half in range(2):
                    bank, bkey = next_pj()
                    for cc in range(4):
                        c = half * 4 + cc
                        tr(bank[:, cc * 128:(cc + 1) * 128], X[:, c, n * 128:(n + 1) * 128], idf[:], [("X", c)], [bkey])
                    cp(ACT if half == 0 else DVE, xt[:, half * 512:(half + 1) * 512], bank[:, :], [bkey], [xk])
                dma(SP, f"dq_o{n % 2}", out_d[s, n * 128:(n + 1) * 128, :], xt, [xk], [("outd", n % 2)])
            phase_end(extra=[("outd", 0), ("outd", 1)], sp_too=True)

        def rstd_from_ssq(dst, ssq_ps, n, R, W):
            act(dst, ssq_ps, AF.Ln, R, W, scale=1.0 / n, bias=EPS)
            act(dst, dst, AF.Exp, W, W, scale=-0.5)

        def pre_norm(L):
            sq = [tile([512], BF16) for _ in range(2)]
            rs = [tile([512], F32) for _ in range(2)]
            for g in range(4):
                sl = slice(g * 512, (g + 1) * 512)
                for c in range(8):
                    st, sk = sq[c % 2]
                    act(st, X[:, c, sl], AF.Square, [("X", c)], [sk])
                    mm(pm[4][:, :], ones, st, c == 0, c == 7, [sk], ["pm4"], inc=True)
                rt, rk = rs[g % 2]
                rstd_from_ssq(rt, pm[4][:, :], D, ["pm4"], [rk])
                for c in range(8):
                    stt(hT[:, c, sl], X[:, c, sl], pre32[:, L, c:c + 1], rt, ALU.mult, ALU.mult, [("X", c), rk, "consts"], ["hT"])
            phase_end()

        def out_proj(L, slots):
            if dbg_d is not None:
                dma(SP, "dq_dbg", dbg_d, mixT[:], [("mixT", c) for c in range(8)], ["dbgd"])
            Yg, yk = tile([8, 512], F32)
            sq = [tile([512], BF16) for _ in range(2)]
            rt, rk = tile([512], F32)
            tmp = [tile([512], F32) for _ in range(2)]
            for g in range(4):
                sl = slice(g * 512, (g + 1) * 512)
                for dc in range(8):
                    bank, bkey = next_pj()
                    slot = slots[dc // 4]
                    for k in range(8):
                        mm(bank[:, :], wg[slot][:, k, (dc % 4) * 128:(dc % 4 + 1) * 128], mixT[:, k, sl], k == 0, k == 7,
                           [("wg", slot), ("mixT", k)], [bkey], inc=(k == 7))
                    st, sk = sq[dc % 2]
                    cp(DVE, Yg[:, dc, :], bank[:, :], [bkey], [(yk, dc)])
                    act(st, bank[:, :], AF.Square, [bkey], [sk])
                    mm(pm[4][:, :], ones, st, dc == 0, dc == 7, [sk], ["pm4"], inc=True)
                if "op1" in debug_skip:
                    continue
                rstd_from_ssq(rt, pm[4][:, :], D, ["pm4"], [rk])
                if "op2" in debug_skip:
                    continue
                for dc in range(8):
                    tp, tk = tmp[dc % 2]
                    stt(tp, Yg[:, dc, :], post32[:, L, dc:dc + 1], rt, ALU.mult, ALU.mult, [(yk, dc), rk, "consts"], [tk])
                    tt(POOL, X[:, dc, sl], X[:, dc, sl], tp, ALU.add, [("X", dc), tk], [("X", dc)])
            phase_end()

        def gate_phase(slot, chunks):
            sg = [tile([512], BF16) for _ in range(2)]
            i = 0
            for ci, c in enumerate(chunks):
                for g in range(4):
                    sl = slice(g * 512, (g + 1) * 512)
                    bank, bkey = next_pj()
                    proj_fm(slot, ci * 128, g, bank, bkey)
                    st, sk = sg[i % 2]
                    i += 1
                    act(st, bank[:, :], AF.Silu, [bkey], [sk])
                    tt(DVE if (i % 2) else POOL, mixT[:, c, sl], mixT[:, c, sl], st, ALU.mult, [sk, ("mixT", c)], [("mixT", c)])
            phase_end()

        def retention_pair(pr, slot):
            qk_tm, k_qk = tile([16, 256], BF16)
            qkT, k_qkT = tile([2, SEQ], BF16)
            v_tm, k_v = tile([16, 256], BF16)
            qs = [tile([256], F32) for _ in range(1)]
            tmp = [[tile([4, 32], F32) for _ in range(4)] for _ in range(1)]
            for n in range(16):
                bank, bkey = next_pj()
                proj_tm(slot, 0, 256, n, bank[:, 0:256], bkey)
                qt, qk_ = qs[0]
                tt(DVE, qt, bank[:, 0:256], Gt[:, pr, :], ALU.mult, [bkey, "consts"], [qk_])
                q3 = qt.rearrange("p (a b) -> p a b", a=4)
                x1 = q3[:, :, 0:32]
                x2 = q3[:, :, 32:64]
                cosb = rope[:, 0, n, :].unsqueeze(1).to_broadcast([128, 4, 32])
                sinb = rope[:, 1, n, :].unsqueeze(1).to_broadcast([128, 4, 32])
                (t1, k1), (t2, k2), (t3, k3), (t4, k4) = tmp[0]
                o3 = qk_tm[:, n, :].rearrange("p (a b) -> p a b", a=4)
                tt(DVE, t1, x1, cosb, ALU.mult, [qk_, "consts"], [k1])
                tt(DVE, t2, x2, sinb, ALU.mult, [qk_, "consts"], [k2])
                tt(DVE, o3[:, :, 0:32], t1, t2, ALU.subtract, [k1, k2], [(k_qk, n)])
                tt(DVE, t3, x1, sinb, ALU.mult, [qk_, "consts"], [k3])
                tt(DVE, t4, x2, cosb, ALU.mult, [qk_, "consts"], [k4])
                tt(DVE, o3[:, :, 32:64], t3, t4, ALU.add, [k3, k4], [(k_qk, n)])
                bank2, bkey2 = next_pj()
                proj_tm(slot, 256, 256, n, bank2[:, 0:256], bkey2)
                cp(ACT, v_tm[:, n, :], bank2[:, 0:256], [bkey2], [(k_v, n)])
            if "r_a" in debug_skip:
                phase_end()
                return
            for n0 in range(0, 16, 4):
                for which in range(2):
                    for j in range(4):
                        n = n0 + j
                        tr(pT[:, which * 512 + j * 128: which * 512 + (j + 1) * 128], qk_tm[:, n, which * 128:(which + 1) * 128],
                           ident, [(k_qk, n), "consts"], ["pT"])
                cp(ACT, qkT[:, :, n0 * 128:(n0 + 4) * 128], pT[:, :].rearrange("p (a b) -> p a b", a=2), ["pT"], [(k_qkT, n0)])
            if "r_b" in debug_skip:
                phase_end()
                return
            Sst, k_S = tile([128], F32)
            SB, k_SB = tile([128], BF16)
            sTm = [tile([256], BF16) for _ in range(2)]
            sq = [tile([256], BF16) for _ in range(2)]
            rs = [tile([256], F32) for _ in range(2)]
            memset(DVE, Sst, 0.0, [k_S])
            for n in range(16):
                n0 = (n // 4) * 4
                csl = slice(n * 128, (n + 1) * 128)
                st, sk = sTm[n % 2]
                for hh in range(2):
                    ps_ = slice(hh * 64, (hh + 1) * 64)
                    sbank, skey = (pm[0], "pm0") if hh == 0 else (pm[4], "pm4")
                    mm(sbank[:, 0:128], qkT[ps_, 1, csl], qkT[ps_, 0, csl], True, True, [(k_qkT, n0)], [skey])
                    tt(DVE, st[:, hh * 128:(hh + 1) * 128], sbank[:, 0:128], cmask[:, 0, :], ALU.mult, [skey, "consts"], [sk])
                for hh in range(2):
                    if "r_c1" in debug_skip:
                        continue
                    mm(pm[1][hh * 64:(hh + 1) * 64, 0:128], qk_tm[:, n, 128 + hh * 64:128 + (hh + 1) * 64], v_tm[:, n, hh * 128:(hh + 1) * 128],
                       True, True, [(k_qk, n), (k_v, n)], ["pm1"], inc=(hh == 1))
                for hh in range(2):
                    ps_ = slice(hh * 64, (hh + 1) * 64)
                    osl = pm[2][:, hh * 128:(hh + 1) * 128]
                    mm(osl, v_tm[:, n, hh * 128:(hh + 1) * 128], st[:, hh * 128:(hh + 1) * 128], True, n == 0, [(k_v, n), sk], ["pm2"],
                       inc=(n == 0 and hh == 1))
                    if n > 0 and "r_c1" not in debug_skip:
                        mm(osl, SB[ps_, :], qkT[ps_, 0, csl], False, True, [k_SB, (k_qkT, n0)], ["pm2"], inc=(hh == 1))
                if "r_c1" not in debug_skip:
                    stt(Sst, Sst, gct[:, pr:pr + 1], pm[1][:, 0:128], ALU.mult, ALU.add, [k_S, "pm1", "consts"], [k_S])
                    act(SB, Sst, AF.Copy, [k_S, "consts"], [k_SB], scale=gct[:, pr:pr + 1])
                qt, qk2 = sq[n % 2]
                act(qt, pm[2][:, 0:256], AF.Square, ["pm2"], [qk2])
                mm(pm[3][:, 0:256], ones, qt, True, True, [qk2, "consts"], ["pm3"])
                rt, rk = rs[n % 2]
                rstd_from_ssq(rt, pm[3][:, 0:256], 128, ["pm3"], [rk])
                tt(DVE, mixT[:, 2 * pr:2 * pr + 2, csl], pm[2][:, 0:256].rearrange("p (a b) -> p a b", a=2),
                   rt.rearrange("p (a b) -> p a b", a=2), ALU.mult, ["pm2", rk], [("mixT", 2 * pr), ("mixT", 2 * pr + 1)])
            phase_end()

        def sb_pair(pr, slot):
            qT, k_q = tile([SEQ], BF16)
            kT, k_k = tile([SEQ], BF16)
            v_tm, k_v = tile([16, 128], BF16)
            for g in range(4):
                sl = slice(g * 512, (g + 1) * 512)
                bank, bkey = next_pj()
                proj_fm(slot, 0, g, bank, bkey)
                cp(ACT, qT[:, sl], bank[:, :], [bkey], [(k_q, g)])
                bank, bkey = next_pj()
                proj_fm(slot, 128, g, bank, bkey)
                act(kT[:, sl], bank[:, :], AF.Copy, [bkey], [(k_k, g)], scale=0.125)
            for n0 in range(0, 16, 4):
                bank, bkey = next_pj()
                for j in range(4):
                    proj_tm(slot, 256, 128, n0 + j, bank[:, j * 128:(j + 1) * 128], bkey)
                cp(DVE, v_tm[:, n0:n0 + 4, :], bank[:, :].rearrange("p (a b) -> p a b", a=4), [bkey], [(k_v, n0 // 4)])
            NB = 2
            e_t = [[tile([512], F32) for _ in range(NB)] for _ in range(2)]
            sp_t = [[tile([512], BF16) for _ in range(NB)] for _ in range(2)]
            ea_t = [[tile([512], BF16) for _ in range(NB)] for _ in range(2)]
            w_t = [[tile([512], BF16) for _ in range(NB)] for _ in range(2)]
            it = 0
            for G in range(4):
                gsl0 = G * 512
                started_P = [[False], [False]]
                started_O = [[False], [False]]
                blocks = list(range(4 * G + 3, -1, -1))

                def cols(b):
                    c0 = (b - 4 * G) * 128 if b >= 4 * G else 0
                    return c0

                def emit_Z(b):
                    c0 = cols(b)
                    for hh in range(2):
                        ps_ = slice(hh * 64, (hh + 1) * 64)
                        mm(pm[hh][:, c0:512], kT[ps_, b * 128:(b + 1) * 128], qT[ps_, gsl0 + c0:gsl0 + 512], True, True,
                           [(k_k, b // 4), (k_q, G)], [f"pm{hh}"])

                def acc(bank, started, lhsT, rhs_ap, c0, R, W, outp=slice(0, 128)):
                    mm(bank[outp, c0:512], lhsT, rhs_ap[:, c0:512], not started[0], True, R, W)
                    started[0] = True

                emit_Z(blocks[0])
                for bi, b in enumerate(blocks):
                    c0 = cols(b)
                    buf = it % NB
                    it += 1
                    diag = b >= 4 * G
                    for hh in range(2):
                        et, ek = e_t[hh][buf]
                        act(et[:, c0:512], pm[hh][:, c0:512], AF.Exp, [f"pm{hh}"], [ek])
                    if diag:
                        for hh in range(2):
                            et, ek = e_t[hh][buf]
                            tt(DVE, et[:, c0:c0 + 128], et[:, c0:c0 + 128], cmask[:, 1, :], ALU.mult, [ek, "consts"], [ek])
                    for hh in range(2):
                        et, ek = e_t[hh][buf]
                        st, sk = sp_t[hh][buf]
                        act(st[:, c0:512], et[:, c0:512], AF.Ln, [ek], [sk], bias=1.0)
                    for hh in range(2):
                        st, sk = sp_t[hh][buf]
                        acc(pm[2 + hh], started_P[hh], tri, st, c0, [sk, "consts"], [f"pm{2 + hh}"])
                    if bi + 1 < len(blocks):
                        emit_Z(blocks[bi + 1])
                    for hh in range(2):
                        at_, ak = ea_t[hh][buf]
                        act(at_[:, c0:512], pm[2 + hh][:, c0:512], AF.Exp, [f"pm{2 + hh}"], [ak], scale=-1.0)
                    for hh in range(2):
                        st, sk = sp_t[hh][buf]
                        acc(pm[2 + hh], started_P[hh], cpl, st, c0, [sk, "consts"], [f"pm{2 + hh}"])
                    for hh in range(2):
                        et, ek = e_t[hh][buf]
                        at_, ak = ea_t[hh][buf]
                        wt, wk = w_t[hh][buf]
                        tt(DVE if hh == 0 else POOL, wt[:, c0:512], et[:, c0:512], at_[:, c0:512], ALU.mult, [ek, ak], [wk])
                    for hh in range(2):
                        wt, wk = w_t[hh][buf]
                        acc(pm[4], started_O[hh], v_tm[:, b, hh * 64:(hh + 1) * 64], wt, c0, [wk, (k_v, b // 4)], ["pm4"],
                            outp=slice(hh * 64, (hh + 1) * 64))
                cp(ACT, mixT[:, 4 + pr, gsl0:gsl0 + 512], pm[4][:, :], ["pm4"], [("mixT", 4 + pr)])
            phase_end()

        def hgrn_head(o, h, slot):
            NCK = 128 // HC
            qT, k_q = tile([SEQ], BF16)
            kT, k_k = tile([SEQ], BF16)
            dch, k_d = tile([SEQ // HC], F32)
            (a1, ka1), (a2, ka2), (a3, ka3) = tile([512], F32), tile([512], F32), tile([512], F32)
            lb_ap = lbt[:, o, h:h + 1]
            oml_ap = omlt[:, o, h:h + 1]
            for g in range(4):
                sl = slice(g * 512, (g + 1) * 512)
                bank, bkey = next_pj()
                proj_fm(slot, 128, g, bank, bkey)
                act(a1, bank[:, :], AF.Exp, [bkey], [ka1], scale=-1.0)
                ts(DVE, a1, a1, 1.0, None, ALU.add, None, [ka1], [ka1])
                recip(a1, a1, [ka1], [ka1])
                ts(DVE, a1, a1, oml_ap, lb_ap, ALU.mult, ALU.add, [ka1, "consts"], [ka1])
                ts(DVE, a2, a1, -1.0, 1.0, ALU.mult, ALU.add, [ka1], [ka2])
                act(a3, a1, AF.Ln, [ka1], [ka3])
                scan(a1, resetm[:, :], a3, 0.0, [ka3, "consts"], [ka1])
                act(a3, a1, AF.Exp, [ka1], [ka3])
                act(a1, a1, AF.Exp, [ka1], [ka1], scale=-1.0)
                bank2, bkey2 = next_pj()
                proj_fm(slot, 0, g, bank2, bkey2)
                tt(DVE, qT[:, sl], bank2[:, :], a3, ALU.mult, [bkey2, ka3], [(k_q, g)])
                tt(POOL, kT[:, sl], a2, a1, ALU.mult, [ka1, ka2], [(k_k, g)])
                nch = 512 // HC
                cp(ACT, dch[:, g * nch:(g + 1) * nch], a3.rearrange("p (c j) -> p c j", j=HC)[:, :, HC - 1], [ka3], [k_d])
            v4 = [tile([4, 128], BF16) for _ in range(2)]
            k4 = [tile([4, 128], BF16) for _ in range(2)]
            vt, vk = tile([NCK, 128], BF16)
            atm = [tile([128], BF16) for _ in range(2)]
            up, uk = tile([NCK, 128], F32)
            Sall = [tile([NCK, 128], F32) for _ in range(2)]
            Sbf = [tile([NCK, 128], BF16) for _ in range(2)]
            sq, k_sq = tile([512], BF16)
            rt, k_rt = tile([512], F32)
            for n in range(16):
                g4 = n // 4
                j4 = n % 4
                vt4, vk4 = v4[g4 % 2]
                kt4, kk4 = k4[g4 % 2]
                if j4 == 0:
                    bank, bkey = next_pj()
                    for j in range(4):
                        proj_tm(slot, 256, 128, n + j, bank[:, j * 128:(j + 1) * 128], bkey)
                    cp(ACT, vt4, bank[:, :].rearrange("p (a b) -> p a b", a=4), [bkey], [vk4])
                    for j in range(4):
                        tr(pT[:, j * 128:(j + 1) * 128], kT[:, (n + j) * 128:(n + j + 1) * 128], ident, [(k_k, g4), "consts"], ["pT"])
                    cp(DVE, kt4, pT[:, 0:512].rearrange("p (a b) -> p a b", a=4), ["pT"], [kk4])
                tsl = slice(n * 128, (n + 1) * 128)
                ob = pm[2 + g4 % 2]
                okey = f"pm{2 + g4 % 2}"
                ocol = j4 * 128
                mm(pm[0][:, 0:128], kT[:, tsl], qT[:, tsl], True, True, [(k_k, g4), (k_q, g4)], ["pm0"])
                am, ak = atm[n % 2]
                tt(DVE, am, pm[0][:, 0:128], cmask[:, 2, :], ALU.mult, ["pm0", "consts"], [ak])
                tt(DVE, vt, vt4[:, j4, :].unsqueeze(1).to_broadcast([128, NCK, 128]),
                   ckm[:, :].unsqueeze(2).to_broadcast([128, NCK, 128]), ALU.mult, [vk4, "consts"], [vk])
                mm(pm[1][:, 0:NCK * 128], kt4[:, j4, :], vt.rearrange("p a b -> p (a b)"), True, True, [kk4, vk], ["pm1"])
                tt(DVE, up, pm[1][:, 0:NCK * 128].rearrange("p (a b) -> p a b", a=NCK),
                   dch[:, n * NCK:(n + 1) * NCK].unsqueeze(2).to_broadcast([128, NCK, 128]), ALU.mult, ["pm1", k_d], [uk])
                sa, sak = Sall[n % 2]
                sprev, spk = Sall[(n - 1) % 2]
                for c in range(NCK):
                    cc = n * NCK + c
                    if cc == 0:
                        cp(DVE, sa[:, 0, :], up[:, 0, :], [uk], [sak])
                    else:
                        prev = sprev[:, NCK - 1, :] if c == 0 else sa[:, c - 1, :]
                        stt(sa[:, c, :], prev, dch[:, cc:cc + 1], up[:, c, :], ALU.mult, ALU.add, [uk, sak, spk, k_d], [sak])
                sb_, sbk = Sbf[n % 2]
                sbp, sbpk = Sbf[(n - 1) % 2]
                cp(ACT, sb_, sa, [sak], [sbk])
                mm(ob[:, ocol:ocol + 128], vt4[:, j4, :], am, True, False, [vk4, ak], [okey])
                for c in range(NCK):
                    cc = n * NCK + c
                    if cc == 0:
                        continue
                    lhs = sbp[:, NCK - 1, :] if c == 0 else sb_[:, c - 1, :]
                    mm(ob[:, ocol + c * HC: ocol + (c + 1) * HC], lhs, qT[:, n * 128 + c * HC: n * 128 + (c + 1) * HC], False, True,
                       [sbk, sbpk, (k_q, g4)], [okey])
                if j4 == 3:
                    n0 = n - 3
                    act(sq, ob[:, :], AF.Square, [okey], [k_sq])
                    mm(pm[4][:, :], ones, sq, True, True, [k_sq, "consts"], ["pm4"])
                    rstd_from_ssq(rt, pm[4][:, :], 128, ["pm4"], [k_rt])
                    tt(DVE, mixT[:, h, n0 * 128:(n0 + 4) * 128], ob[:, :], rt, ALU.mult, [okey, k_rt], [("mixT", h)])
            phase_end()

        def rglru_chunk(o, j, slot):
            lxp, k_lx = tile([SEQ + 4], F32)
            xc_t = [tile([512], F32) for _ in range(1)]
            xcb = [tile([512], BF16) for _ in range(1)]
            (r_t, k_r), (ig_t, k_ig) = tile([512], F32), tile([512], F32)
            (at_, ak), (mt, mk) = tile([512], F32), tile([512], F32)
            h_t = [tile([512], F32) for _ in range(2)]
            memset(DVE, lxp[:, 0:4], 0.0, [(k_lx, -1)])
            for g in range(4):
                bank, bkey = next_pj()
                proj_fm(slot, j * 128, g, bank, bkey)
                cp(ACT, lxp[:, 4 + g * 512: 4 + (g + 1) * 512], bank[:, :], [bkey], [(k_lx, g)])
            for g in range(4):
                sl = slice(g * 512, (g + 1) * 512)
                xc, xck = xc_t[0]
                R = [(k_lx, g), (k_lx, g - 1), "consts"]
                ts(DVE, xc, lxp[:, 1 + g * 512: 1 + (g + 1) * 512], cwt[:, o, 0, j:j + 1], cbt[:, o, j:j + 1], ALU.mult, ALU.add, R, [xck])
                for jj in range(1, 4):
                    stt(xc, lxp[:, 1 + jj + g * 512: 1 + jj + (g + 1) * 512], cwt[:, o, jj, j:j + 1], xc, ALU.mult, ALU.add,
                        R + [xck], [xck])
                xb, xbk = xcb[0]
                cp(ACT, xb, xc, [xck], [xbk])
                mm(pm[0][:, :], wabd[:, o, j, :], xb, True, True, [xbk, "consts2"], ["pm0"])
                mm(pm[1][:, :], wxbd[:, o, j, :], xb, True, True, [xbk, "consts2"], ["pm1"])
                act(r_t, pm[0][:, :], AF.Exp, ["pm0", "consts"], [k_r], scale=-1.0, bias=nbat[:, o, j:j + 1])
                ts(DVE, r_t, r_t, 1.0, None, ALU.add, None, [k_r], [k_r])
                recip(r_t, r_t, [k_r], [k_r])
                act(ig_t, pm[1][:, :], AF.Exp, ["pm1", "consts"], [k_ig], scale=-1.0, bias=nbxt[:, o, j:j + 1])
                ts(DVE, ig_t, ig_t, 1.0, None, ALU.add, None, [k_ig], [k_ig])
                recip(ig_t, ig_t, [k_ig], [k_ig])
                tt(POOL, ig_t, ig_t, xc, ALU.mult, [k_ig, xck], [k_ig])
                ht, hk = h_t[g % 2]
                hp, hpk = h_t[(g - 1) % 2]
                act(at_, r_t, AF.Exp, [k_r, "consts"], [ak], scale=nspt[:, o, j:j + 1])
                act(mt, r_t, AF.Exp, [k_r, "consts"], [mk], scale=nsp2t[:, o, j:j + 1])
                ts(DVE, mt, mt, -1.0, 1.0, ALU.mult, ALU.add, [mk], [mk])
                act(mt, mt, AF.Ln, [mk], [mk], bias=1e-30)
                act(mt, mt, AF.Exp, [mk], [mk], scale=0.5)
                if g == 0:
                    memset(DVE, mt[:, 0:1], 1.0, [mk])
                tt(POOL, mt, mt, ig_t, ALU.mult, [mk, k_ig], [mk])
                init = 0.0 if g == 0 else hp[:, 511:512]
                scan(ht, at_, mt, init, [ak, mk, hpk], [hk])
                cp(ACT, mixT[:, 4 + j, sl], ht, [hk], [("mixT", 4 + j)])
            phase_end()

        tasks = []

        def add(pieces, fn):
            tasks.append((pieces, fn))

        for s in range(nseq):
            if "io" not in debug_skip:
                add(None, lambda slot, s=s: load_x(s))
            for L in layers:
                if "nopre" not in debug_skip:
                    add(None, lambda slot, L=L: pre_norm(L))
                if L % 2 == 0:
                    e = L // 2
                    W = ewin[e]
                    if "ret" in debug_skip:
                        add(None, lambda slot: memset(DVE, mixT[:, 0:4, :], 1.0, [("mixT", c) for c in range(4)]))
                    if "sb" in debug_skip:
                        add(None, lambda slot: memset(DVE, mixT[:, 4:8, :], 1.0, [("mixT", c) for c in range(4, 8)]))
                    for pr in range(2):
                        if "ret" in debug_skip:
                            continue
                        add([(W, pr * 128, 128), (W, 256 + pr * 128, 128), (W, 512 + pr * 256, 256)],
                            lambda slot, pr=pr: retention_pair(pr, slot))
                    if "nogate" not in debug_skip:
                        add([(W, 1024, 512)], lambda slot: gate_phase(slot, [0, 1, 2, 3]))
                    for pr in range(4):
                        if "sb" in debug_skip:
                            continue
                        add([(W, 1536 + pr * 128, 128), (W, 2048 + pr * 128, 128), (W, 2560 + pr * 128, 128)],
                            lambda slot, pr=pr: sb_pair(pr, slot))
                    if "nogate" not in debug_skip:
                        add([(W, 3072, 512)], lambda slot: gate_phase(slot, [4, 5, 6, 7]))
                    WO = ewout[e]
                else:
                    o = L // 2
                    W = owin[o]
                    for h in range(4):
                        if "hgrn" in debug_skip:
                            continue
                        add([(W, h * 128, 128), (W, 512 + h * 128, 128), (W, 1024 + h * 128, 128)],
                            lambda slot, o=o, h=h: hgrn_head(o, h, slot))
                    add([(W, 1536, 512)], lambda slot: gate_phase(slot, [0, 1, 2, 3]))
                    holder = {}
                    add([(W, 2048, 512)], lambda slot, holder=holder: holder.__setitem__("lx", slot))
                    for j in range(4):
                        if "lru" in debug_skip:
                            continue
                        add(None, lambda slot, o=o, j=j, holder=holder: rglru_chunk(o, j, holder["lx"]))
                    add([(W, 2560, 512)], lambda slot: gate_phase(slot, [4, 5, 6, 7]))
                    WO = owout[o]
                if "noout" in debug_skip:
                    continue
                holder2 = {}
                add([(WO, 0, 512)], lambda slot, holder2=holder2: holder2.__setitem__("a", slot))
                add([(WO, 512, 512)], lambda slot, L=L, holder2=holder2: out_proj(L, [holder2["a"], slot]))
            if "io" not in debug_skip and "st" not in debug_skip:
                add(None, lambda slot, s=s: store_x(s))

        wtasks = [i for i, t in enumerate(tasks) if t[0] is not None]
        slots = {}
        nxt = 0

        def ensure_issued(upto):
            nonlocal nxt
            while nxt < len(wtasks) and nxt <= upto:
                ti = wtasks[nxt]
                slots[ti] = issue_load(tasks[ti][0])
                nxt += 1

        wpos = {ti: k for k, ti in enumerate(wtasks)}
        for i, (pieces, fn) in enumerate(tasks):
            if pieces is not None:
                ensure_issued(wpos[i] + 1)
                fn(slots[i])
            else:
                fn(None)
        if "io" in debug_skip or "st" in debug_skip:
            dma(SP, "dq_o0", out_d[0, 0:128, :], X[:, 0, 0:1024], [("X", 0)], [("outd", 0)])
        P.op(SP, lambda q: q.nop(), reads=[("outd", 0), ("outd", 1)])
        P.emit()
    return nc


_CACHE = {}


def _get_prog(layers):
    key = tuple(layers)
    if key not in _CACHE:
        _CACHE[key] = build(list(layers))
    return _CACHE[key]


def kernel(**inputs):
    x = np.ascontiguousarray(inputs["x"], dtype=np.float32)
    consts = _consts()
    params = _params(inputs)
    base = {k: np.ascontiguousarray(inputs[k], dtype=np.float32) for k in ("even_w_in", "even_w_out", "odd_w_in", "odd_w_out")}
    base.update(consts)
    base.update(params)
    cur = x.reshape(NCORES, NSEQ, SEQ, D)
    for layers in LAYER_GROUPS:
        nc = _get_prog(layers)
        in_maps = []
        for c in range(NCORES):
            m = dict(base)
            m["x"] = np.ascontiguousarray(cur[c])
            in_maps.append(m)
        res = run_bass_kernel_spmd(nc, in_maps, core_ids=list(range(NCORES)))
        cur = np.stack([np.asarray(r["out"], dtype=np.float32) for r in res.results], axis=0)
    return cur.reshape(NCORES * NSEQ, SEQ, D)
```

```python
import numpy as np
import ml_dtypes
from contextlib import ExitStack
import concourse.bass as bass
import concourse.mybir as mybir
from concourse.bass_utils import run_bass_kernel_spmd

F32 = mybir.dt.float32
BF16 = mybir.dt.bfloat16
AF = mybir.ActivationFunctionType
ALU = mybir.AluOpType
PE, ACT, DVE, POOL, SP = "tensor", "scalar", "vector", "gpsimd", "sync"
ENGS = [PE, ACT, DVE, POOL, SP]

NCORES = 8
SEQ = 2048
D = 1024
NSEQ = 2
EPS = 1e-6
HC = 32
POOL_COMPUTE = True
LAYER_GROUPS = [[0, 1, 2, 3]]


def _is_psum_key(k):
    if isinstance(k, str):
        return k.startswith("pm") or k == "pT"
    return isinstance(k, tuple) and k[0] == "pj"


class Prog:
    def __init__(self, nc, es):
        self.nc = nc
        self.es = es
        self.stream = {e: [] for e in ENGS}
        self.cnt = {}
        self.sem = {}
        self.mult = {}
        self.clock = {e: {} for e in ENGS}
        self.iclock = {}
        self.lastw = {}
        self.readers = {}
        self.marked = set()
        self.pending = {}
        for e in ENGS:
            self._mkq(e, 1)

    def _mkq(self, name, mult):
        self.cnt[name] = 0
        self.sem[name] = self.es.enter_context(self.nc.semaphore("s_" + name))
        self.mult[name] = mult

    def op(self, eng, fn, reads=(), writes=(), vq=None, inc=True):
        me = vq or eng
        if vq is not None and vq not in self.cnt:
            self._mkq(vq, 16)
        deps = {}

        def need(t):
            e, i = t
            if i > deps.get(e, 0):
                deps[e] = i

        for r in reads:
            lw = self.lastw.get(r)
            if lw:
                need(lw)
            if _is_psum_key(r):
                for e2, i2 in self.readers.get(r, {}).items():
                    if e2 != me:
                        need((e2, i2))
        for w in writes:
            lw = self.lastw.get(w)
            if lw and (lw[0] != me or me == POOL):
                need(lw)
            for e2, i2 in self.readers.get(w, {}).items():
                need((e2, i2))
        if self.pending.get(eng):
            for e2, i2 in self.pending.pop(eng).items():
                need((e2, i2))
        clk = self.clock[eng]
        waits = []
        for e, i in deps.items():
            if e == PE and me == PE:
                continue
            if clk.get(e, 0) >= i:
                continue
            waits.append((e, i))
            snap = self.iclock.get((e, i))
            if snap:
                for k, v in snap.items():
                    if clk.get(k, 0) < v:
                        clk[k] = v
            clk[e] = max(clk.get(e, 0), i)
        self.cnt[me] += 1
        idx = self.cnt[me]
        self.iclock[(me, idx)] = dict(clk)
        self.stream[eng].append((waits, fn, me, idx))
        for w_ in waits:
            self.marked.add(w_)
        if self.mult[me] == 16:
            self.marked.add((me, idx))
        for r in reads:
            d = self.readers.setdefault(r, {})
            if d.get(me, 0) < idx:
                d[me] = idx
        for w in writes:
            self.lastw[w] = (me, idx)
            self.readers[w] = {}
        return idx

    def barrier(self, engs=(PE, ACT, DVE, POOL), extra=()):
        deps = {e: self.cnt[e] for e in engs if self.cnt[e] > 0}
        for k in extra:
            lw = self.lastw.get(k)
            if lw:
                deps[lw[0]] = max(deps.get(lw[0], 0), lw[1])
        for e in engs:
            d = self.pending.setdefault(e, {})
            for e2, i2 in deps.items():
                if e2 != e and d.get(e2, 0) < i2:
                    d[e2] = i2

    def emit(self):
        nc = self.nc
        mcount = {}
        for q_ in self.cnt:
            c = 0
            for i in range(1, self.cnt[q_] + 1):
                if (q_, i) in self.marked:
                    c += 1
                    mcount[(q_, i)] = c
        with nc.Block() as block:
            for eng in ENGS:
                stream = self.stream[eng]
                if not stream:
                    continue

                def body(q, stream=stream):
                    for waits, fn, me, idx in stream:
                        for w_ in waits:
                            q.wait_ge(self.sem[w_[0]], mcount[w_] * self.mult[w_[0]])
                        ins = fn(q)
                        if (me, idx) in mcount:
                            ins.then_inc(self.sem[me], self.mult[me])

                getattr(block, eng)(body)


def _consts():
    bf = ml_dtypes.bfloat16
    c = {}
    idx = np.arange(128)
    c["c_idf"] = np.eye(128, dtype=np.float32)
    cb = np.zeros((128, 6, 128), np.float32)
    cb[:, 0] = np.eye(128)
    cb[:, 1] = 1.0
    cb[:, 2] = (idx[:, None] >= idx[None, :])
    cb[:, 3] = (idx[:, None] < idx[None, :])
    c["c_bf"] = cb.astype(bf)
    cm = np.zeros((128, 3, 128), np.float32)
    cm[:, 0] = (idx[None, :] >= idx[:, None])
    cm[:, 1] = (idx[:, None] < idx[None, :])
    cm[:, 2] = ((idx[:, None] // HC) == (idx[None, :] // HC)) & (idx[None, :] >= idx[:, None])
    c["c_mask"] = cm
    half = 32
    inv = (10000.0 ** (-(np.arange(half, dtype=np.float32)) / half)).astype(np.float32)
    pos = np.arange(SEQ, dtype=np.float32)
    ang = (pos[:, None] * inv[None, :]).astype(np.float32)
    cs = np.zeros((128, 2, 16, 32), np.float32)
    cs[:, 0] = np.cos(ang).astype(np.float32).reshape(16, 128, 32).transpose(1, 0, 2)
    cs[:, 1] = np.sin(ang).astype(np.float32).reshape(16, 128, 32).transpose(1, 0, 2)
    c["c_rope"] = cs
    g = 1.0 - 2.0 ** (-5.0 - np.arange(4, dtype=np.float64))
    G = np.zeros((128, 2, 4, 64), np.float64)
    gc = np.zeros((128, 2), np.float64)
    for pr in range(2):
        for hh in range(2):
            h = 2 * pr + hh
            G[:, pr, hh, :] = (g[h] ** idx)[:, None]
            G[:, pr, 2 + hh, :] = (g[h] ** (-idx.astype(np.float64)))[:, None] * (64 ** -0.5)
            gc[hh * 64:(hh + 1) * 64, pr] = g[h] ** 128
    c["c_G"] = G.astype(np.float32)
    c["c_gc"] = gc.astype(np.float32)
    rm = np.ones((128, 512), np.float32)
    rm[:, ::HC] = 0.0
    c["c_reset"] = rm.astype(bf)
    nck = 128 // HC
    c["c_cmask"] = ((idx[:, None] // HC) == np.arange(nck)[None, :]).astype(np.float32).astype(bf)
    return c


def _params(inp):
    p = {}
    f = lambda a: np.ascontiguousarray(a, dtype=np.float32)
    p["p_pre"] = f(inp["pre_norm_w"].reshape(4, 8, 128).transpose(2, 0, 1))
    p["p_post"] = f(inp["post_norm_w"].reshape(4, 8, 128).transpose(2, 0, 1))
    p["p_lbl"] = f(inp["hgrn_lb_logits"].reshape(2, 4, 128).transpose(2, 0, 1))
    p["p_cw"] = f(inp["conv_w"].reshape(2, 4, 4, 128).transpose(3, 0, 1, 2))
    p["p_cb"] = f(inp["conv_b"].reshape(2, 4, 128).transpose(2, 0, 1))
    p["p_ba"] = f(inp["lru_b_a"].reshape(2, 4, 128).transpose(2, 0, 1))
    p["p_bx"] = f(inp["lru_b_x"].reshape(2, 4, 128).transpose(2, 0, 1))
    p["p_lam"] = f(inp["lru_lambda"].reshape(2, 4, 128).transpose(2, 0, 1))
    for nm, src in (("p_wa", inp["lru_w_a"]), ("p_wx", inp["lru_w_x"])):
        bd = np.zeros((2, 128, 4, 128), np.float32)
        for o in range(2):
            for j in range(4):
                bd[o, 0:64, j, 0:64] = src[o, 2 * j]
                bd[o, 64:128, j, 64:128] = src[o, 2 * j + 1]
        p[nm] = bd
    return p


def build(layers, nseq=NSEQ, debug_skip=()):
    if 'one' in debug_skip:
        nseq = 1
    nc = bass.Bass("TRN2", target_bir_lowering=False)
    consts = _consts()
    dram = {}

    def din(name, shape, dt=F32):
        dram[name] = nc.dram_tensor(name, list(shape), dt, kind="ExternalInput").ap()
        return dram[name]

    x_d = din("x", [nseq, SEQ, D])
    out_d = nc.dram_tensor("out", [nseq, SEQ, D], F32, kind="ExternalOutput").ap()
    dbg_d = nc.dram_tensor("dbg", [128, 8, SEQ], BF16, kind="ExternalOutput").ap() if "dbg" in debug_skip else None
    ewin = din("even_w_in", [2, 1024, 3584])
    ewout = din("even_w_out", [2, 1024, 1024])
    owin = din("odd_w_in", [2, 1024, 3072])
    owout = din("odd_w_out", [2, 1024, 1024])
    for k, v in consts.items():
        din(k, v.shape, BF16 if v.dtype == ml_dtypes.bfloat16 else F32)
    pshapes = {"p_pre": [128, 4, 8], "p_post": [128, 4, 8], "p_lbl": [128, 2, 4], "p_cw": [128, 2, 4, 4],
               "p_cb": [128, 2, 4], "p_ba": [128, 2, 4], "p_bx": [128, 2, 4], "p_lam": [128, 2, 4],
               "p_wa": [2, 128, 4, 128], "p_wx": [2, 128, 4, 128]}
    for k, s in pshapes.items():
        din(k, s)

    es = ExitStack()
    with es:
        P = Prog(nc, es)

        def sb(name, shape, dt):
            return es.enter_context(nc.sbuf_tensor(name, list(shape), dt))

        def psum(name, shape, dt):
            return es.enter_context(nc.psum_tensor(name, list(shape), dt))

        X = sb("X", [128, 8, SEQ], F32)
        hT = sb("hT", [128, 8, SEQ], BF16)
        mixT = sb("mixT", [128, 8, SEQ], BF16)
        wg = [sb(f"wg{i}", [128, 8, 512], BF16) for i in range(3)]
        NSTG = 4
        stg = [sb(f"stg{i}", [128, 512], F32) for i in range(NSTG)]
        idf = sb("idf", [128, 128], F32)
        cbf = sb("cbf", [128, 6, 128], BF16)
        cmask = sb("cmask", [128, 3, 128], F32)
        rope = sb("rope", [128, 2, 16, 32], F32)
        Gt = sb("Gt", [128, 2, 256], F32)
        gct = sb("gct", [128, 2], F32)
        resetm = sb("resetm", [128, 512], BF16)
        ckm = sb("ckm", [128, 128 // HC], BF16)
        pre32 = sb("pre32", [128, 4, 8], F32)
        post32 = sb("post32", [128, 4, 8], F32)
        lbt = sb("lbt", [128, 2, 4], F32)
        omlt = sb("omlt", [128, 2, 4], F32)
        cwt = sb("cwt", [128, 2, 4, 4], F32)
        cbt = sb("cbt", [128, 2, 4], F32)
        bat = sb("bat", [128, 2, 4], F32)
        bxt = sb("bxt", [128, 2, 4], F32)
        nbat = sb("nbat", [128, 2, 4], F32)
        nbxt = sb("nbxt", [128, 2, 4], F32)
        nspt = sb("nspt", [128, 2, 4], F32)
        nsp2t = sb("nsp2t", [128, 2, 4], F32)
        wabd = sb("wabd", [128, 2, 4, 128], BF16)
        wxbd = sb("wxbd", [128, 2, 4, 128], BF16)
        lblt = sb("lblt", [128, 2, 4], F32)
        ARENA = 32 * 1024
        arena = sb("arena", [128, ARENA // 4], F32)

        pj = [psum(f"pj{i}", [128, 512], F32) for i in range(2)]
        pm = [psum(f"pm{i}", [128, 512], F32) for i in range(5)]
        pT = psum("pT", [128, 1024], BF16)

        ident = cbf[:, 0, :]
        ones = cbf[:, 1, :]
        tri = cbf[:, 2, :]
        cpl = cbf[:, 3, :]

        class Arena:
            def __init__(self):
                self.off = 0

            def reset(self):
                self.off = 0

            def take(self, shape, dt):
                n = int(np.prod(shape))
                nbytes = n * (2 if dt == BF16 else 4)
                nbytes = (nbytes + 63) // 64 * 64
                assert self.off + nbytes <= ARENA, (self.off, nbytes)
                w0 = self.off // 4
                ap = arena[:, w0:w0 + nbytes // 4]
                self.off += nbytes
                if dt == BF16:
                    ap = ap.bitcast(BF16)
                return ap[:, 0:n]

        AR = Arena()
        _uid = [0]

        def tile(shape, dt):
            ap = AR.take(shape, dt)
            if len(shape) == 2:
                ap = ap.rearrange("p (a b) -> p a b", a=shape[0], b=shape[1])
            elif len(shape) == 3:
                ap = ap.rearrange("p (a b c) -> p a b c", a=shape[0], b=shape[1], c=shape[2])
            _uid[0] += 1
            return ap, ("t", _uid[0])

        def phase_end(extra=(), sp_too=False):
            P.barrier(engs=(PE, ACT, DVE, POOL, SP) if sp_too else (PE, ACT, DVE, POOL), extra=extra)
            AR.reset()

        def mm(out, lhsT, rhs, start, stop, R, W, inc=True):
            P.op(PE, lambda q: q.matmul(out, lhsT=lhsT, rhs=rhs, start=start, stop=stop, skip_group_check=True),
                 reads=R, writes=W, inc=inc)

        def tr(out, in_, idn, R, W):
            P.op(PE, lambda q: q.transpose(out=out, in_=in_, identity=idn), reads=R, writes=W)

        def act(out, in_, func, R, W, scale=1.0, bias=0.0):
            P.op(ACT, lambda q: q.activation(out=out, in_=in_, func=func, bias=bias, scale=scale), reads=R, writes=W)

        def tt(eng, out, in0, in1, op, R, W):
            if eng == POOL and not POOL_COMPUTE:
                eng = DVE
            P.op(eng, lambda q: q.tensor_tensor(out=out, in0=in0, in1=in1, op=op), reads=R, writes=W)

        def ts(eng, out, in0, s1, s2, op0, op1, R, W):
            if op1 is None:
                P.op(eng, lambda q: q.tensor_scalar(out=out, in0=in0, scalar1=s1, scalar2=None, op0=op0), reads=R, writes=W)
            else:
                P.op(eng, lambda q: q.tensor_scalar(out=out, in0=in0, scalar1=s1, scalar2=s2, op0=op0, op1=op1), reads=R, writes=W)

        def stt(out, in0, scalar, in1, op0, op1, R, W):
            P.op(DVE, lambda q: q.scalar_tensor_tensor(out=out, in0=in0, scalar=scalar, in1=in1, op0=op0, op1=op1), reads=R, writes=W)

        def cp(eng, out, in_, R, W):
            if eng == POOL and not POOL_COMPUTE:
                eng = DVE
            if eng == ACT:
                P.op(ACT, lambda q: q.copy(out=out, in_=in_), reads=R, writes=W)
            else:
                P.op(eng, lambda q: q.tensor_copy(out=out, in_=in_), reads=R, writes=W)

        def recip(out, in_, R, W):
            P.op(DVE, lambda q: q.reciprocal(out=out, in_=in_), reads=R, writes=W)

        def scan(out, d0, d1, init, R, W):
            P.op(DVE, lambda q: q.tensor_tensor_scan(out=out, data0=d0, data1=d1, initial=init, op0=ALU.mult, op1=ALU.add), reads=R, writes=W)

        def memset(eng, ap, val, W):
            P.op(eng, lambda q: q.memset(ap, val), writes=W)

        def dma(eng, vq, out, in_, R, W):
            P.op(eng, lambda q: q.dma_start(out=out, in_=in_), reads=R, writes=W, vq=vq)

        cload = [(idf[:], "c_idf"), (cbf[:], "c_bf"), (cmask[:], "c_mask"), (rope[:], "c_rope"),
                 (Gt[:].rearrange("p a (b c) -> p a b c", b=4, c=64), "c_G"), (gct[:], "c_gc"), (resetm[:], "c_reset"),
                 (ckm[:], "c_cmask"), (pre32[:], "p_pre"), (post32[:], "p_post"), (lblt[:], "p_lbl"), (cwt[:], "p_cw"),
                 (cbt[:], "p_cb"), (bat[:], "p_ba"), (bxt[:], "p_bx"), (nspt[:], "p_lam")]
        for i, (dst, nm) in enumerate(cload):
            dma(SP, "dq_c", dst, dram[nm], [], ["consts"])
        for o in range(2):
            dma(POOL, "dq_c2", wabd[:, o, :, :], dram["p_wa"][o], [], ["consts2"])
            dma(POOL, "dq_c2", wxbd[:, o, :, :], dram["p_wx"][o], [], ["consts2"])
        CR = ["consts", "consts2"]
        memset(DVE, lbt[:, 0, :], 0.0, ["consts"])
        tt(DVE, lbt[:, 1, :], lblt[:, 1, :], lblt[:, 0, :], ALU.subtract, CR, ["consts"])
        act(lbt[:, 1, :], lbt[:, 1, :], AF.Exp, CR, ["consts"], scale=-1.0)
        ts(DVE, lbt[:, 1, :], lbt[:, 1, :], 1.0, None, ALU.add, None, CR, ["consts"])
        recip(lbt[:, 1, :], lbt[:, 1, :], CR, ["consts"])
        ts(DVE, omlt[:], lbt[:], -1.0, 1.0, ALU.mult, ALU.add, CR, ["consts"])
        ts(DVE, nbat[:], bat[:], -1.0, None, ALU.mult, None, CR, ["consts"])
        ts(DVE, nbxt[:], bxt[:], -1.0, None, ALU.mult, None, CR, ["consts"])
        act(nspt[:], nspt[:], AF.Exp, CR, ["consts"], scale=-1.0)
        act(nspt[:], nspt[:], AF.Ln, CR, ["consts"], bias=1.0)
        ts(DVE, nsp2t[:], nspt[:], -16.0, None, ALU.mult, None, CR, ["consts"])
        ts(DVE, nspt[:], nspt[:], -8.0, None, ALU.mult, None, CR, ["consts"])
        P.barrier()

        wstate = {"n": 0, "s": 0}

        def issue_load(pieces):
            slot = wstate["n"] % 3
            wstate["n"] += 1
            c = 0
            for (w2d, c0, ncols) in pieces:
                kk = 512 // ncols
                src = w2d[:, c0:c0 + ncols].rearrange("(k p) c -> p k c", p=128)
                for k0 in range(0, 8, kk):
                    si = wstate["s"] % NSTG
                    wstate["s"] += 1
                    st3 = stg[si][:, :].rearrange("p (k c) -> p k c", k=kk)
                    dma(SP, f"dq_s{si}", st3, src[:, k0:k0 + kk, :], [], [("stg", si)])
                    P.op(POOL, lambda q, o=wg[slot][:, k0:k0 + kk, c:c + ncols], i=st3: q.tensor_copy(out=o, in_=i),
                         reads=[("stg", si)], writes=[("wg", slot)])
                c += ncols
            return slot

        pjn = [0]

        def next_pj():
            pjn[0] += 1
            i = pjn[0] % 2
            return pj[i], ("pj", i)

        def proj_fm(slot, col0, g, bank, bkey, ncols=128):
            for k in range(8):
                mm(bank[0:ncols, :], wg[slot][:, k, col0:col0 + ncols], hT[:, k, g * 512:(g + 1) * 512], k == 0, k == 7,
                   [("wg", slot), "hT"], [bkey], inc=(k == 7))

        def proj_tm(slot, col0, ncols, n, out, bkey):
            for k in range(8):
                mm(out, hT[:, k, n * 128:(n + 1) * 128], wg[slot][:, k, col0:col0 + ncols], k == 0, k == 7,
                   [("wg", slot), "hT"], [bkey], inc=(k == 7))

        def load_x(s):
            xin = [tile([1024], F32) for _ in range(2)]
            for n in range(1 if "l1" in debug_skip else (3 if "l3" in debug_skip else 16)):
                xt, xk = xin[n % 2]
                dma(SP, f"dq_x{n % 2}", xt, x_d[s, n * 128:(n + 1) * 128, :], [], [xk])
                for half in range(2):
                    bank, bkey = next_pj()
                    for cc in range(4):
                        c = half * 4 + cc
                        tr(bank[:, cc * 128:(cc + 1) * 128], xt[:, c * 128:(c + 1) * 128], idf[:], [xk], [bkey])
                    dst = X[:, half * 4:half * 4 + 4, n * 128:(n + 1) * 128]
                    if "lnocp" in debug_skip:
                        continue
                    if "l2d" in debug_skip:
                        for cc in range(4):
                            dd = X[:, half * 4 + cc, n * 128:(n + 1) * 128] if "lsc" not in debug_skip else hT[:, half * 4 + cc, 0:256].bitcast(F32)
                            cp(ACT if half == 0 else DVE, dd, bank[:, cc * 128:(cc + 1) * 128], [bkey], [("X", half * 4 + cc)])
                    else:
                        cp(ACT if half == 0 else DVE, dst, bank[:, :].rearrange("p (a b) -> p a b", a=4), [bkey], [("X", half * 4 + i) for i in range(4)])
            phase_end()

        def store_x(s):
            xo = [tile([1024], F32) for _ in range(2)]
            for n in range(16):
                xt, xk = xo[n % 2]
                for half in range(2):
                    bank, bkey = next_pj()
                    for cc in range(4):
                        c = half * 4 + cc
                        tr(bank[:, cc * 128:(cc + 1) * 128], X[:, c, n * 128:(n + 1) * 128], idf[:], [("X", c)], [bkey])
                    cp(ACT if half == 0 else DVE, xt[:, half * 512:(half + 1) * 512], bank[:, :], [bkey], [xk])
                dma(SP, f"dq_o{n % 2}", out_d[s, n * 128:(n + 1) * 128, :], xt, [xk], [("outd", n % 2)])
            phase_end(extra=[("outd", 0), ("outd", 1)], sp_too=True)

        def rstd_from_ssq(dst, ssq_ps, n, R, W):
            act(dst, ssq_ps, AF.Ln, R, W, scale=1.0 / n, bias=EPS)
            act(dst, dst, AF.Exp, W, W, scale=-0.5)

        def pre_norm(L):
            sq = [tile([512], BF16) for _ in range(2)]
            rs = [tile([512], F32) for _ in range(2)]
            for g in range(4):
                sl = slice(g * 512, (g + 1) * 512)
                for c in range(8):
                    st, sk = sq[c % 2]
                    act(st, X[:, c, sl], AF.Square, [("X", c)], [sk])
                    mm(pm[4][:, :], ones, st, c == 0, c == 7, [sk], ["pm4"], inc=True)
                rt, rk = rs[g % 2]
                rstd_from_ssq(rt, pm[4][:, :], D, ["pm4"], [rk])
                for c in range(8):
                    stt(hT[:, c, sl], X[:, c, sl], pre32[:, L, c:c + 1], rt, ALU.mult, ALU.mult, [("X", c), rk, "consts"], ["hT"])
            phase_end()

        def out_proj(L, slots):
            if dbg_d is not None:
                dma(SP, "dq_dbg", dbg_d, mixT[:], [("mixT", c) for c in range(8)], ["dbgd"])
            Yg, yk = tile([8, 512], F32)
            sq = [tile([512], BF16) for _ in range(2)]
            rt, rk = tile([512], F32)
            tmp = [tile([512], F32) for _ in range(2)]
            for g in range(4):
                sl = slice(g * 512, (g + 1) * 512)
                for dc in range(8):
                    bank, bkey = next_pj()
                    slot = slots[dc // 4]
                    for k in range(8):
                        mm(bank[:, :], wg[slot][:, k, (dc % 4) * 128:(dc % 4 + 1) * 128], mixT[:, k, sl], k == 0, k == 7,
                           [("wg", slot), ("mixT", k)], [bkey], inc=(k == 7))
                    st, sk = sq[dc % 2]
                    cp(DVE, Yg[:, dc, :], bank[:, :], [bkey], [(yk, dc)])
                    act(st, bank[:, :], AF.Square, [bkey], [sk])
                    mm(pm[4][:, :], ones, st, dc == 0, dc == 7, [sk], ["pm4"], inc=True)
                if "op1" in debug_skip:
                    continue
                rstd_from_ssq(rt, pm[4][:, :], D, ["pm4"], [rk])
                if "op2" in debug_skip:
                    continue
                for dc in range(8):
                    tp, tk = tmp[dc % 2]
                    stt(tp, Yg[:, dc, :], post32[:, L, dc:dc + 1], rt, ALU.mult, ALU.mult, [(yk, dc), rk, "consts"], [tk])
                    tt(POOL, X[:, dc, sl], X[:, dc, sl], tp, ALU.add, [("X", dc), tk], [("X", dc)])
            phase_end()

        def gate_phase(slot, chunks):
            sg = [tile([512], BF16) for _ in range(2)]
            i = 0
            for ci, c in enumerate(chunks):
                for g in range(4):
                    sl = slice(g * 512, (g + 1) * 512)
                    bank, bkey = next_pj()
                    proj_fm(slot, ci * 128, g, bank, bkey)
                    st, sk = sg[i % 2]
                    i += 1
                    act(st, bank[:, :], AF.Silu, [bkey], [sk])
                    tt(DVE if (i % 2) else POOL, mixT[:, c, sl], mixT[:, c, sl], st, ALU.mult, [sk, ("mixT", c)], [("mixT", c)])
            phase_end()

        def retention_pair(pr, slot):
            qk_tm, k_qk = tile([16, 256], BF16)
            qkT, k_qkT = tile([2, SEQ], BF16)
            v_tm, k_v = tile([16, 256], BF16)
            qs = [tile([256], F32) for _ in range(1)]
            tmp = [[tile([4, 32], F32) for _ in range(4)] for _ in range(1)]
            for n in range(16):
                bank, bkey = next_pj()
                proj_tm(slot, 0, 256, n, bank[:, 0:256], bkey)
                qt, qk_ = qs[0]
                tt(DVE, qt, bank[:, 0:256], Gt[:, pr, :], ALU.mult, [bkey, "consts"], [qk_])
                q3 = qt.rearrange("p (a b) -> p a b", a=4)
                x1 = q3[:, :, 0:32]
                x2 = q3[:, :, 32:64]
                cosb = rope[:, 0, n, :].unsqueeze(1).to_broadcast([128, 4, 32])
                sinb = rope[:, 1, n, :].unsqueeze(1).to_broadcast([128, 4, 32])
                (t1, k1), (t2, k2), (t3, k3), (t4, k4) = tmp[0]
                o3 = qk_tm[:, n, :].rearrange("p (a b) -> p a b", a=4)
                tt(DVE, t1, x1, cosb, ALU.mult, [qk_, "consts"], [k1])
                tt(DVE, t2, x2, sinb, ALU.mult, [qk_, "consts"], [k2])
                tt(DVE, o3[:, :, 0:32], t1, t2, ALU.subtract, [k1, k2], [(k_qk, n)])
                tt(DVE, t3, x1, sinb, ALU.mult, [qk_, "consts"], [k3])
                tt(DVE, t4, x2, cosb, ALU.mult, [qk_, "consts"], [k4])
                tt(DVE, o3[:, :, 32:64], t3, t4, ALU.add, [k3, k4], [(k_qk, n)])
                bank2, bkey2 = next_pj()
                proj_tm(slot, 256, 256, n, bank2[:, 0:256], bkey2)
                cp(ACT, v_tm[:, n, :], bank2[:, 0:256], [bkey2], [(k_v, n)])
            if "r_a" in debug_skip:
                phase_end()
                return
            for n0 in range(0, 16, 4):
                for which in range(2):
                    for j in range(4):
                        n = n0 + j
                        tr(pT[:, which * 512 + j * 128: which * 512 + (j + 1) * 128], qk_tm[:, n, which * 128:(which + 1) * 128],
                           ident, [(k_qk, n), "consts"], ["pT"])
                cp(ACT, qkT[:, :, n0 * 128:(n0 + 4) * 128], pT[:, :].rearrange("p (a b) -> p a b", a=2), ["pT"], [(k_qkT, n0)])
            if "r_b" in debug_skip:
                phase_end()
                return
            Sst, k_S = tile([128], F32)
            SB, k_SB = tile([128], BF16)
            sTm = [tile([256], BF16) for _ in range(2)]
            sq = [tile([256], BF16) for _ in range(2)]
            rs = [tile([256], F32) for _ in range(2)]
            memset(DVE, Sst, 0.0, [k_S])
            for n in range(16):
                n0 = (n // 4) * 4
                csl = slice(n * 128, (n + 1) * 128)
                st, sk = sTm[n % 2]
                for hh in range(2):
                    ps_ = slice(hh * 64, (hh + 1) * 64)
                    sbank, skey = (pm[0], "pm0") if hh == 0 else (pm[4], "pm4")
                    mm(sbank[:, 0:128], qkT[ps_, 1, csl], qkT[ps_, 0, csl], True, True, [(k_qkT, n0)], [skey])
                    tt(DVE, st[:, hh * 128:(hh + 1) * 128], sbank[:, 0:128], cmask[:, 0, :], ALU.mult, [skey, "consts"], [sk])
                for hh in range(2):
                    if "r_c1" in debug_skip:
                        continue
                    mm(pm[1][hh * 64:(hh + 1) * 64, 0:128], qk_tm[:, n, 128 + hh * 64:128 + (hh + 1) * 64], v_tm[:, n, hh * 128:(hh + 1) * 128],
                       True, True, [(k_qk, n), (k_v, n)], ["pm1"], inc=(hh == 1))
                for hh in range(2):
                    ps_ = slice(hh * 64, (hh + 1) * 64)
                    osl = pm[2][:, hh * 128:(hh + 1) * 128]
                    mm(osl, v_tm[:, n, hh * 128:(hh + 1) * 128], st[:, hh * 128:(hh + 1) * 128], True, n == 0, [(k_v, n), sk], ["pm2"],
                       inc=(n == 0 and hh == 1))
                    if n > 0 and "r_c1" not in debug_skip:
                        mm(osl, SB[ps_, :], qkT[ps_, 0, csl], False, True, [k_SB, (k_qkT, n0)], ["pm2"], inc=(hh == 1))
                if "r_c1" not in debug_skip:
                    stt(Sst, Sst, gct[:, pr:pr + 1], pm[1][:, 0:128], ALU.mult, ALU.add, [k_S, "pm1", "consts"], [k_S])
                    act(SB, Sst, AF.Copy, [k_S, "consts"], [k_SB], scale=gct[:, pr:pr + 1])
                qt, qk2 = sq[n % 2]
                act(qt, pm[2][:, 0:256], AF.Square, ["pm2"], [qk2])
                mm(pm[3][:, 0:256], ones, qt, True, True, [qk2, "consts"], ["pm3"])
                rt, rk = rs[n % 2]
                rstd_from_ssq(rt, pm[3][:, 0:256], 128, ["pm3"], [rk])
                tt(DVE, mixT[:, 2 * pr:2 * pr + 2, csl], pm[2][:, 0:256].rearrange("p (a b) -> p a b", a=2),
                   rt.rearrange("p (a b) -> p a b", a=2), ALU.mult, ["pm2", rk], [("mixT", 2 * pr), ("mixT", 2 * pr + 1)])
            phase_end()

        def sb_pair(pr, slot):
            qT, k_q = tile([SEQ], BF16)
            kT, k_k = tile([SEQ], BF16)
            v_tm, k_v = tile([16, 128], BF16)
            for g in range(4):
                sl = slice(g * 512, (g + 1) * 512)
                bank, bkey = next_pj()
                proj_fm(slot, 0, g, bank, bkey)
                cp(ACT, qT[:, sl], bank[:, :], [bkey], [(k_q, g)])
                bank, bkey = next_pj()
                proj_fm(slot, 128, g, bank, bkey)
                act(kT[:, sl], bank[:, :], AF.Copy, [bkey], [(k_k, g)], scale=0.125)
            for n0 in range(0, 16, 4):
                bank, bkey = next_pj()
                for j in range(4):
                    proj_tm(slot, 256, 128, n0 + j, bank[:, j * 128:(j + 1) * 128], bkey)
                cp(DVE, v_tm[:, n0:n0 + 4, :], bank[:, :].rearrange("p (a b) -> p a b", a=4), [bkey], [(k_v, n0 // 4)])
            NB = 2
            e_t = [[tile([512], F32) for _ in range(NB)] for _ in range(2)]
            sp_t = [[tile([512], BF16) for _ in range(NB)] for _ in range(2)]
            ea_t = [[tile([512], BF16) for _ in range(NB)] for _ in range(2)]
            w_t = [[tile([512], BF16) for _ in range(NB)] for _ in range(2)]
            it = 0
            for G in range(4):
                gsl0 = G * 512
                started_P = [[False], [False]]
                started_O = [[False], [False]]
                blocks = list(range(4 * G + 3, -1, -1))

                def cols(b):
                    c0 = (b - 4 * G) * 128 if b >= 4 * G else 0
                    return c0

                def emit_Z(b):
                    c0 = cols(b)
                    for hh in range(2):
                        ps_ = slice(hh * 64, (hh + 1) * 64)
                        mm(pm[hh][:, c0:512], kT[ps_, b * 128:(b + 1) * 128], qT[ps_, gsl0 + c0:gsl0 + 512], True, True,
                           [(k_k, b // 4), (k_q, G)], [f"pm{hh}"])

                def acc(bank, started, lhsT, rhs_ap, c0, R, W, outp=slice(0, 128)):
                    mm(bank[outp, c0:512], lhsT, rhs_ap[:, c0:512], not started[0], True, R, W)
                    started[0] = True

                emit_Z(blocks[0])
                for bi, b in enumerate(blocks):
                    c0 = cols(b)
                    buf = it % NB
                    it += 1
                    diag = b >= 4 * G
                    for hh in range(2):
                        et, ek = e_t[hh][buf]
                        act(et[:, c0:512], pm[hh][:, c0:512], AF.Exp, [f"pm{hh}"], [ek])
                    if diag:
                        for hh in range(2):
                            et, ek = e_t[hh][buf]
                            tt(DVE, et[:, c0:c0 + 128], et[:, c0:c0 + 128], cmask[:, 1, :], ALU.mult, [ek, "consts"], [ek])
                    for hh in range(2):
                        et, ek = e_t[hh][buf]
                        st, sk = sp_t[hh][buf]
                        act(st[:, c0:512], et[:, c0:512], AF.Ln, [ek], [sk], bias=1.0)
                    for hh in range(2):
                        st, sk = sp_t[hh][buf]
                        acc(pm[2 + hh], started_P[hh], tri, st, c0, [sk, "consts"], [f"pm{2 + hh}"])
                    if bi + 1 < len(blocks):
                        emit_Z(blocks[bi + 1])
                    for hh in range(2):
                        at_, ak = ea_t[hh][buf]
                        act(at_[:, c0:512], pm[2 + hh][:, c0:512], AF.Exp, [f"pm{2 + hh}"], [ak], scale=-1.0)
                    for hh in range(2):
                        st, sk = sp_t[hh][buf]
                        acc(pm[2 + hh], started_P[hh], cpl, st, c0, [sk, "consts"], [f"pm{2 + hh}"])
                    for hh in range(2):
                        et, ek = e_t[hh][buf]
                        at_, ak = ea_t[hh][buf]
                        wt, wk = w_t[hh][buf]
                        tt(DVE, wt[:, c0:512], et[:, c0:512], at_[:, c0:512], ALU.mult, [ek, ak], [wk])
                    for hh in range(2):
                        wt, wk = w_t[hh][buf]
                        acc(pm[4], started_O[hh], v_tm[:, b, hh * 64:(hh + 1) * 64], wt, c0, [wk, (k_v, b // 4)], ["pm4"],
                            outp=slice(hh * 64, (hh + 1) * 64))
                cp(ACT, mixT[:, 4 + pr, gsl0:gsl0 + 512], pm[4][:, :], ["pm4"], [("mixT", 4 + pr)])
            phase_end()

        def hgrn_head(o, h, slot):
            NCK = 128 // HC
            qT, k_q = tile([SEQ], BF16)
            kT, k_k = tile([SEQ], BF16)
            dch, k_d = tile([SEQ // HC], F32)
            (a1, ka1), (a2, ka2), (a3, ka3) = tile([512], F32), tile([512], F32), tile([512], F32)
            lb_ap = lbt[:, o, h:h + 1]
            oml_ap = omlt[:, o, h:h + 1]
            for g in range(4):
                sl = slice(g * 512, (g + 1) * 512)
                bank, bkey = next_pj()
                proj_fm(slot, 128, g, bank, bkey)
                act(a1, bank[:, :], AF.Exp, [bkey], [ka1], scale=-1.0)
                act(a1, a1, AF.Ln, [ka1], [ka1], bias=1.0)
                act(a1, a1, AF.Exp, [ka1], [ka1], scale=-1.0)
                ts(DVE, a1, a1, oml_ap, lb_ap, ALU.mult, ALU.add, [ka1, "consts"], [ka1])
                ts(DVE, a2, a1, -1.0, 1.0, ALU.mult, ALU.add, [ka1], [ka2])
                act(a3, a1, AF.Ln, [ka1], [ka3])
                scan(a1, resetm[:, :], a3, 0.0, [ka3, "consts"], [ka1])
                act(a3, a1, AF.Exp, [ka1], [ka3])
                act(a1, a1, AF.Exp, [ka1], [ka1], scale=-1.0)
                bank2, bkey2 = next_pj()
                proj_fm(slot, 0, g, bank2, bkey2)
                tt(DVE, qT[:, sl], bank2[:, :], a3, ALU.mult, [bkey2, ka3], [(k_q, g)])
                tt(POOL, kT[:, sl], a2, a1, ALU.mult, [ka1, ka2], [(k_k, g)])
                nch = 512 // HC
                cp(ACT, dch[:, g * nch:(g + 1) * nch], a3.rearrange("p (c j) -> p c j", j=HC)[:, :, HC - 1], [ka3], [k_d])
            v4 = [tile([4, 128], BF16) for _ in range(2)]
            k4 = [tile([4, 128], BF16) for _ in range(2)]
            vt, vk = tile([NCK, 128], BF16)
            atm = [tile([128], BF16) for _ in range(2)]
            up, uk = tile([NCK, 128], F32)
            Sall = [tile([NCK, 128], F32) for _ in range(2)]
            Sbf = [tile([NCK, 128], BF16) for _ in range(2)]
            sq, k_sq = tile([512], BF16)
            rt, k_rt = tile([512], F32)
            for n in range(16):
                g4 = n // 4
                j4 = n % 4
                vt4, vk4 = v4[g4 % 2]
                kt4, kk4 = k4[g4 % 2]
                if j4 == 0:
                    bank, bkey = next_pj()
                    for j in range(4):
                        proj_tm(slot, 256, 128, n + j, bank[:, j * 128:(j + 1) * 128], bkey)
                    cp(ACT, vt4, bank[:, :].rearrange("p (a b) -> p a b", a=4), [bkey], [vk4])
                    for j in range(4):
                        tr(pT[:, j * 128:(j + 1) * 128], kT[:, (n + j) * 128:(n + j + 1) * 128], ident, [(k_k, g4), "consts"], ["pT"])
                    cp(DVE, kt4, pT[:, 0:512].rearrange("p (a b) -> p a b", a=4), ["pT"], [kk4])
                tsl = slice(n * 128, (n + 1) * 128)
                ob = pm[2 + g4 % 2]
                okey = f"pm{2 + g4 % 2}"
                ocol = j4 * 128
                mm(pm[0][:, 0:128], kT[:, tsl], qT[:, tsl], True, True, [(k_k, g4), (k_q, g4)], ["pm0"])
                am, ak = atm[n % 2]
                tt(DVE, am, pm[0][:, 0:128], cmask[:, 2, :], ALU.mult, ["pm0", "consts"], [ak])
                tt(DVE, vt, vt4[:, j4, :].unsqueeze(1).to_broadcast([128, NCK, 128]),
                   ckm[:, :].unsqueeze(2).to_broadcast([128, NCK, 128]), ALU.mult, [vk4, "consts"], [vk])
                mm(pm[1][:, 0:NCK * 128], kt4[:, j4, :], vt.rearrange("p a b -> p (a b)"), True, True, [kk4, vk], ["pm1"])
                tt(DVE, up, pm[1][:, 0:NCK * 128].rearrange("p (a b) -> p a b", a=NCK),
                   dch[:, n * NCK:(n + 1) * NCK].unsqueeze(2).to_broadcast([128, NCK, 128]), ALU.mult, ["pm1", k_d], [uk])
                sa, sak = Sall[n % 2]
                sprev, spk = Sall[(n - 1) % 2]
                for c in range(NCK):
                    cc = n * NCK + c
                    if cc == 0:
                        cp(DVE, sa[:, 0, :], up[:, 0, :], [uk], [sak])
                    else:
                        prev = sprev[:, NCK - 1, :] if c == 0 else sa[:, c - 1, :]
                        stt(sa[:, c, :], prev, dch[:, cc:cc + 1], up[:, c, :], ALU.mult, ALU.add, [uk, sak, spk, k_d], [sak])
                sb_, sbk = Sbf[n % 2]
                sbp, sbpk = Sbf[(n - 1) % 2]
                cp(ACT, sb_, sa, [sak], [sbk])
                mm(ob[:, ocol:ocol + 128], vt4[:, j4, :], am, True, False, [vk4, ak], [okey])
                for c in range(NCK):
                    cc = n * NCK + c
                    if cc == 0:
                        continue
                    lhs = sbp[:, NCK - 1, :] if c == 0 else sb_[:, c - 1, :]
                    mm(ob[:, ocol + c * HC: ocol + (c + 1) * HC], lhs, qT[:, n * 128 + c * HC: n * 128 + (c + 1) * HC], False, True,
                       [sbk, sbpk, (k_q, g4)], [okey])
                if j4 == 3:
                    n0 = n - 3
                    act(sq, ob[:, :], AF.Square, [okey], [k_sq])
                    mm(pm[4][:, :], ones, sq, True, True, [k_sq, "consts"], ["pm4"])
                    rstd_from_ssq(rt, pm[4][:, :], 128, ["pm4"], [k_rt])
                    tt(DVE, mixT[:, h, n0 * 128:(n0 + 4) * 128], ob[:, :], rt, ALU.mult, [okey, k_rt], [("mixT", h)])
            phase_end()

        def rglru_chunk(o, j, slot):
            lxp, k_lx = tile([SEQ + 4], F32)
            xc_t = [tile([512], F32) for _ in range(1)]
            xcb = [tile([512], BF16) for _ in range(1)]
            (r_t, k_r), (ig_t, k_ig) = tile([512], F32), tile([512], F32)
            (at_, ak), (mt, mk) = tile([512], F32), tile([512], F32)
            h_t = [tile([512], F32) for _ in range(2)]
            memset(DVE, lxp[:, 0:4], 0.0, [(k_lx, -1)])
            for g in range(4):
                bank, bkey = next_pj()
                proj_fm(slot, j * 128, g, bank, bkey)
                cp(ACT, lxp[:, 4 + g * 512: 4 + (g + 1) * 512], bank[:, :], [bkey], [(k_lx, g)])
            for g in range(4):
                sl = slice(g * 512, (g + 1) * 512)
                xc, xck = xc_t[0]
                R = [(k_lx, g), (k_lx, g - 1), "consts"]
                ts(DVE, xc, lxp[:, 1 + g * 512: 1 + (g + 1) * 512], cwt[:, o, 0, j:j + 1], cbt[:, o, j:j + 1], ALU.mult, ALU.add, R, [xck])
                for jj in range(1, 4):
                    stt(xc, lxp[:, 1 + jj + g * 512: 1 + jj + (g + 1) * 512], cwt[:, o, jj, j:j + 1], xc, ALU.mult, ALU.add,
                        R + [xck], [xck])
                xb, xbk = xcb[0]
                cp(ACT, xb, xc, [xck], [xbk])
                mm(pm[0][:, :], wabd[:, o, j, :], xb, True, True, [xbk, "consts2"], ["pm0"])
                mm(pm[1][:, :], wxbd[:, o, j, :], xb, True, True, [xbk, "consts2"], ["pm1"])
                act(r_t, pm[0][:, :], AF.Exp, ["pm0", "consts"], [k_r], scale=-1.0, bias=nbat[:, o, j:j + 1])
                act(ig_t, pm[1][:, :], AF.Exp, ["pm1", "consts"], [k_ig], scale=-1.0, bias=nbxt[:, o, j:j + 1])
                act(r_t, r_t, AF.Ln, [k_r], [k_r], bias=1.0)
                act(ig_t, ig_t, AF.Ln, [k_ig], [k_ig], bias=1.0)
                act(r_t, r_t, AF.Exp, [k_r], [k_r], scale=-1.0)
                act(ig_t, ig_t, AF.Exp, [k_ig], [k_ig], scale=-1.0)
                tt(POOL, ig_t, ig_t, xc, ALU.mult, [k_ig, xck], [k_ig])
                ht, hk = h_t[g % 2]
                hp, hpk = h_t[(g - 1) % 2]
                act(at_, r_t, AF.Exp, [k_r, "consts"], [ak], scale=nspt[:, o, j:j + 1])
                act(mt, r_t, AF.Exp, [k_r, "consts"], [mk], scale=nsp2t[:, o, j:j + 1])
                ts(DVE, mt, mt, -1.0, 1.0, ALU.mult, ALU.add, [mk], [mk])
                act(mt, mt, AF.Ln, [mk], [mk], bias=1e-30)
                act(mt, mt, AF.Exp, [mk], [mk], scale=0.5)
                if g == 0:
                    memset(DVE, mt[:, 0:1], 1.0, [mk])
                tt(POOL, mt, mt, ig_t, ALU.mult, [mk, k_ig], [mk])
                init = 0.0 if g == 0 else hp[:, 511:512]
                scan(ht, at_, mt, init, [ak, mk, hpk], [hk])
                cp(ACT, mixT[:, 4 + j, sl], ht, [hk], [("mixT", 4 + j)])
            phase_end()

        tasks = []

        def add(pieces, fn):
            tasks.append((pieces, fn))

        for s in range(nseq):
            if "io" not in debug_skip:
                add(None, lambda slot, s=s: load_x(s))
            for L in layers:
                if "nopre" not in debug_skip:
                    add(None, lambda slot, L=L: pre_norm(L))
                if L % 2 == 0:
                    e = L // 2
                    W = ewin[e]
                    if "ret" in debug_skip:
                        add(None, lambda slot: memset(DVE, mixT[:, 0:4, :], 1.0, [("mixT", c) for c in range(4)]))
                    if "sb" in debug_skip:
                        add(None, lambda slot: memset(DVE, mixT[:, 4:8, :], 1.0, [("mixT", c) for c in range(4, 8)]))
                    for pr in range(2):
                        if "ret" in debug_skip:
                            continue
                        add([(W, pr * 128, 128), (W, 256 + pr * 128, 128), (W, 512 + pr * 256, 256)],
                            lambda slot, pr=pr: retention_pair(pr, slot))
                    if "nogate" not in debug_skip:
                        add([(W, 1024, 512)], lambda slot: gate_phase(slot, [0, 1, 2, 3]))
                    for pr in range(4):
                        if "sb" in debug_skip:
                            continue
                        add([(W, 1536 + pr * 128, 128), (W, 2048 + pr * 128, 128), (W, 2560 + pr * 128, 128)],
                            lambda slot, pr=pr: sb_pair(pr, slot))
                    if "nogate" not in debug_skip:
                        add([(W, 3072, 512)], lambda slot: gate_phase(slot, [4, 5, 6, 7]))
                    WO = ewout[e]
                else:
                    o = L // 2
                    W = owin[o]
                    for h in range(4):
                        if "hgrn" in debug_skip:
                            continue
                        add([(W, h * 128, 128), (W, 512 + h * 128, 128), (W, 1024 + h * 128, 128)],
                            lambda slot, o=o, h=h: hgrn_head(o, h, slot))
                    add([(W, 1536, 512)], lambda slot: gate_phase(slot, [0, 1, 2, 3]))
                    holder = {}
                    add([(W, 2048, 512)], lambda slot, holder=holder: holder.__setitem__("lx", slot))
                    for j in range(4):
                        if "lru" in debug_skip:
                            continue
                        add(None, lambda slot, o=o, j=j, holder=holder: rglru_chunk(o, j, holder["lx"]))
                    add([(W, 2560, 512)], lambda slot: gate_phase(slot, [4, 5, 6, 7]))
                    WO = owout[o]
                if "noout" in debug_skip:
                    continue
                holder2 = {}
                add([(WO, 0, 512)], lambda slot, holder2=holder2: holder2.__setitem__("a", slot))
                add([(WO, 512, 512)], lambda slot, L=L, holder2=holder2: out_proj(L, [holder2["a"], slot]))
            if "io" not in debug_skip and "st" not in debug_skip:
                add(None, lambda slot, s=s: store_x(s))

        wtasks = [i for i, t in enumerate(tasks) if t[0] is not None]
        slots = {}
        nxt = 0

        def ensure_issued(upto):
            nonlocal nxt
            while nxt < len(wtasks) and nxt <= upto:
                ti = wtasks[nxt]
                slots[ti] = issue_load(tasks[ti][0])
                nxt += 1

        wpos = {ti: k for k, ti in enumerate(wtasks)}
        for i, (pieces, fn) in enumerate(tasks):
            if pieces is not None:
                ensure_issued(wpos[i] + 1)
                fn(slots[i])
            else:
                fn(None)
        if "io" in debug_skip or "st" in debug_skip:
            dma(SP, "dq_o0", out_d[0, 0:128, :], X[:, 0, 0:1024], [("X", 0)], [("outd", 0)])
        P.op(SP, lambda q: q.nop(), reads=[("outd", 0), ("outd", 1)])
        P.emit()
    return nc


_CACHE = {}


def _get_prog(layers):
    key = tuple(layers)
    if key not in _CACHE:
        _CACHE[key] = build(list(layers))
    return _CACHE[key]


def kernel(**inputs):
    x = np.ascontiguousarray(inputs["x"], dtype=np.float32)
    consts = _consts()
    params = _params(inputs)
    base = {k: np.ascontiguousarray(inputs[k], dtype=np.float32) for k in ("even_w_in", "even_w_out", "odd_w_in", "odd_w_out")}
    base.update(consts)
    base.update(params)
    cur = x.reshape(NCORES, NSEQ, SEQ, D)
    for layers in LAYER_GROUPS:
        nc = _get_prog(layers)
        in_maps = []
        for c in range(NCORES):
            m = dict(base)
            m["x"] = np.ascontiguousarray(cur[c])
            in_maps.append(m)
        res = run_bass_kernel_spmd(nc, in_maps, core_ids=list(range(NCORES)))
        cur = np.stack([np.asarray(r["out"], dtype=np.float32) for r in res.results], axis=0)
    return cur.reshape(NCORES * NSEQ, SEQ, D)
```

```python
import numpy as np
import ml_dtypes
from contextlib import ExitStack
import concourse.bass as bass
import concourse.mybir as mybir
from concourse.bass_utils import run_bass_kernel_spmd

F32 = mybir.dt.float32
BF16 = mybir.dt.bfloat16
AF = mybir.ActivationFunctionType
ALU = mybir.AluOpType
PE, ACT, DVE, POOL, SP = "tensor", "scalar", "vector", "gpsimd", "sync"
ENGS = [PE, ACT, DVE, POOL, SP]

NCORES = 8
SEQ = 2048
D = 1024
NSEQ = 2
EPS = 1e-6
HC = 32
POOL_COMPUTE = True
LAYER_GROUPS = [[0, 1, 2, 3]]


def _is_psum_key(k):
    if isinstance(k, str):
        return k.startswith("pm") or k == "pT"
    return isinstance(k, tuple) and k[0] == "pj"


class Prog:
    def __init__(self, nc, es):
        self.nc = nc
        self.es = es
        self.stream = {e: [] for e in ENGS}
        self.cnt = {}
        self.sem = {}
        self.mult = {}
        self.clock = {e: {} for e in ENGS}
        self.iclock = {}
        self.lastw = {}
        self.readers = {}
        self.marked = set()
        self.pending = {}
        for e in ENGS:
            self._mkq(e, 1)

    def _mkq(self, name, mult):
        self.cnt[name] = 0
        self.sem[name] = self.es.enter_context(self.nc.semaphore("s_" + name))
        self.mult[name] = mult

    def op(self, eng, fn, reads=(), writes=(), vq=None, inc=True):
        me = vq or eng
        if vq is not None and vq not in self.cnt:
            self._mkq(vq, 16)
        deps = {}

        def need(t):
            e, i = t
            if i > deps.get(e, 0):
                deps[e] = i

        for r in reads:
            lw = self.lastw.get(r)
            if lw:
                need(lw)
            if _is_psum_key(r):
                for e2, i2 in self.readers.get(r, {}).items():
                    if e2 != me:
                        need((e2, i2))
        for w in writes:
            lw = self.lastw.get(w)
            if lw and (lw[0] != me or me == POOL):
                need(lw)
            for e2, i2 in self.readers.get(w, {}).items():
                need((e2, i2))
        if self.pending.get(eng):
            for e2, i2 in self.pending.pop(eng).items():
                need((e2, i2))
        clk = self.clock[eng]
        waits = []
        for e, i in deps.items():
            if e == PE and me == PE:
                continue
            if clk.get(e, 0) >= i:
                continue
            waits.append((e, i))
            snap = self.iclock.get((e, i))
            if snap:
                for k, v in snap.items():
                    if clk.get(k, 0) < v:
                        clk[k] = v
            clk[e] = max(clk.get(e, 0), i)
        self.cnt[me] += 1
        idx = self.cnt[me]
        self.iclock[(me, idx)] = dict(clk)
        self.stream[eng].append((waits, fn, me, idx))
        for w_ in waits:
            self.marked.add(w_)
        if self.mult[me] == 16:
            self.marked.add((me, idx))
        for r in reads:
            d = self.readers.setdefault(r, {})
            if d.get(me, 0) < idx:
                d[me] = idx
        for w in writes:
            self.lastw[w] = (me, idx)
            self.readers[w] = {}
        return idx

    def barrier(self, engs=(PE, ACT, DVE, POOL), extra=()):
        deps = {e: self.cnt[e] for e in engs if self.cnt[e] > 0}
        for k in extra:
            lw = self.lastw.get(k)
            if lw:
                deps[lw[0]] = max(deps.get(lw[0], 0), lw[1])
        for e in engs:
            d = self.pending.setdefault(e, {})
            for e2, i2 in deps.items():
                if e2 != e and d.get(e2, 0) < i2:
                    d[e2] = i2

    def emit(self):
        nc = self.nc
        mcount = {}
        for q_ in self.cnt:
            c = 0
            for i in range(1, self.cnt[q_] + 1):
                if (q_, i) in self.marked:
                    c += 1
                    mcount[(q_, i)] = c
        with nc.Block() as block:
            for eng in ENGS:
                stream = self.stream[eng]
                if not stream:
                    continue

                def body(q, stream=stream):
                    for waits, fn, me, idx in stream:
                        for w_ in waits:
                            q.wait_ge(self.sem[w_[0]], mcount[w_] * self.mult[w_[0]])
                        ins = fn(q)
                        if (me, idx) in mcount:
                            ins.then_inc(self.sem[me], self.mult[me])

                getattr(block, eng)(body)


def _consts():
    bf = ml_dtypes.bfloat16
    c = {}
    idx = np.arange(128)
    c["c_idf"] = np.eye(128, dtype=np.float32)
    cb = np.zeros((128, 6, 128), np.float32)
    cb[:, 0] = np.eye(128)
    cb[:, 1] = 1.0
    cb[:, 2] = (idx[:, None] >= idx[None, :])
    cb[:, 3] = (idx[:, None] < idx[None, :])
    c["c_bf"] = cb.astype(bf)
    cm = np.zeros((128, 3, 128), np.float32)
    cm[:, 0] = (idx[None, :] >= idx[:, None])
    cm[:, 1] = (idx[:, None] < idx[None, :])
    cm[:, 2] = ((idx[:, None] // HC) == (idx[None, :] // HC)) & (idx[None, :] >= idx[:, None])
    c["c_mask"] = cm
    half = 32
    inv = (10000.0 ** (-(np.arange(half, dtype=np.float32)) / half)).astype(np.float32)
    pos = np.arange(SEQ, dtype=np.float32)
    ang = (pos[:, None] * inv[None, :]).astype(np.float32)
    cs = np.zeros((128, 2, 16, 32), np.float32)
    cs[:, 0] = np.cos(ang).astype(np.float32).reshape(16, 128, 32).transpose(1, 0, 2)
    cs[:, 1] = np.sin(ang).astype(np.float32).reshape(16, 128, 32).transpose(1, 0, 2)
    c["c_rope"] = cs
    g = 1.0 - 2.0 ** (-5.0 - np.arange(4, dtype=np.float64))
    G = np.zeros((128, 2, 4, 64), np.float64)
    gc = np.zeros((128, 2), np.float64)
    for pr in range(2):
        for hh in range(2):
            h = 2 * pr + hh
            G[:, pr, hh, :] = (g[h] ** idx)[:, None]
            G[:, pr, 2 + hh, :] = (g[h] ** (-idx.astype(np.float64)))[:, None] * (64 ** -0.5)
            gc[hh * 64:(hh + 1) * 64, pr] = g[h] ** 128
    c["c_G"] = G.astype(np.float32)
    c["c_gc"] = gc.astype(np.float32)
    rm = np.ones((128, 512), np.float32)
    rm[:, ::HC] = 0.0
    c["c_reset"] = rm.astype(bf)
    nck = 128 // HC
    c["c_cmask"] = ((idx[:, None] // HC) == np.arange(nck)[None, :]).astype(np.float32).astype(bf)
    return c


def _params(inp):
    p = {}
    f = lambda a: np.ascontiguousarray(a, dtype=np.float32)
    p["p_pre"] = f(inp["pre_norm_w"].reshape(4, 8, 128).transpose(2, 0, 1))
    p["p_post"] = f(inp["post_norm_w"].reshape(4, 8, 128).transpose(2, 0, 1))
    p["p_lbl"] = f(inp["hgrn_lb_logits"].reshape(2, 4, 128).transpose(2, 0, 1))
    p["p_cw"] = f(inp["conv_w"].reshape(2, 4, 4, 128).transpose(3, 0, 1, 2))
    p["p_cb"] = f(inp["conv_b"].reshape(2, 4, 128).transpose(2, 0, 1))
    p["p_ba"] = f(inp["lru_b_a"].reshape(2, 4, 128).transpose(2, 0, 1))
    p["p_bx"] = f(inp["lru_b_x"].reshape(2, 4, 128).transpose(2, 0, 1))
    p["p_lam"] = f(inp["lru_lambda"].reshape(2, 4, 128).transpose(2, 0, 1))
    for nm, src in (("p_wa", inp["lru_w_a"]), ("p_wx", inp["lru_w_x"])):
        bd = np.zeros((2, 128, 4, 128), np.float32)
        for o in range(2):
            for j in range(4):
                bd[o, 0:64, j, 0:64] = src[o, 2 * j]
                bd[o, 64:128, j, 64:128] = src[o, 2 * j + 1]
        p[nm] = bd
    return p


def build(layers, nseq=NSEQ, debug_skip=()):
    if 'one' in debug_skip:
        nseq = 1
    nc = bass.Bass("TRN2", target_bir_lowering=False)
    consts = _consts()
    dram = {}

    def din(name, shape, dt=F32):
        dram[name] = nc.dram_tensor(name, list(shape), dt, kind="ExternalInput").ap()
        return dram[name]

    x_d = din("x", [nseq, SEQ, D])
    out_d = nc.dram_tensor("out", [nseq, SEQ, D], F32, kind="ExternalOutput").ap()
    dbg_d = nc.dram_tensor("dbg", [128, 8, SEQ], BF16, kind="ExternalOutput").ap() if "dbg" in debug_skip else None
    ewin = din("even_w_in", [2, 1024, 3584])
    ewout = din("even_w_out", [2, 1024, 1024])
    owin = din("odd_w_in", [2, 1024, 3072])
    owout = din("odd_w_out", [2, 1024, 1024])
    for k, v in consts.items():
        din(k, v.shape, BF16 if v.dtype == ml_dtypes.bfloat16 else F32)
    pshapes = {"p_pre": [128, 4, 8], "p_post": [128, 4, 8], "p_lbl": [128, 2, 4], "p_cw": [128, 2, 4, 4],
               "p_cb": [128, 2, 4], "p_ba": [128, 2, 4], "p_bx": [128, 2, 4], "p_lam": [128, 2, 4],
               "p_wa": [2, 128, 4, 128], "p_wx": [2, 128, 4, 128]}
    for k, s in pshapes.items():
        din(k, s)

    es = ExitStack()
    with es:
        P = Prog(nc, es)

        def sb(name, shape, dt):
            return es.enter_context(nc.sbuf_tensor(name, list(shape), dt))

        def psum(name, shape, dt):
            return es.enter_context(nc.psum_tensor(name, list(shape), dt))

        X = sb("X", [128, 8, SEQ], F32)
        hT = sb("hT", [128, 8, SEQ], BF16)
        mixT = sb("mixT", [128, 8, SEQ], BF16)
        wg = [sb(f"wg{i}", [128, 8, 512], BF16) for i in range(3)]
        NSTG = 4
        stg = [sb(f"stg{i}", [128, 512], F32) for i in range(NSTG)]
        idf = sb("idf", [128, 128], F32)
        cbf = sb("cbf", [128, 6, 128], BF16)
        cmask = sb("cmask", [128, 3, 128], F32)
        rope = sb("rope", [128, 2, 16, 32], F32)
        Gt = sb("Gt", [128, 2, 256], F32)
        gct = sb("gct", [128, 2], F32)
        resetm = sb("resetm", [128, 512], BF16)
        ckm = sb("ckm", [128, 128 // HC], BF16)
        pre32 = sb("pre32", [128, 4, 8], F32)
        post32 = sb("post32", [128, 4, 8], F32)
        lbt = sb("lbt", [128, 2, 4], F32)
        omlt = sb("omlt", [128, 2, 4], F32)
        cwt = sb("cwt", [128, 2, 4, 4], F32)
        cbt = sb("cbt", [128, 2, 4], F32)
        bat = sb("bat", [128, 2, 4], F32)
        bxt = sb("bxt", [128, 2, 4], F32)
        nbat = sb("nbat", [128, 2, 4], F32)
        nbxt = sb("nbxt", [128, 2, 4], F32)
        nspt = sb("nspt", [128, 2, 4], F32)
        nsp2t = sb("nsp2t", [128, 2, 4], F32)
        wabd = sb("wabd", [128, 2, 4, 128], BF16)
        wxbd = sb("wxbd", [128, 2, 4, 128], BF16)
        lblt = sb("lblt", [128, 2, 4], F32)
        ARENA = 32 * 1024
        arena = sb("arena", [128, ARENA // 4], F32)

        pj = [psum(f"pj{i}", [128, 512], F32) for i in range(2)]
        pm = [psum(f"pm{i}", [128, 512], F32) for i in range(5)]
        pT = psum("pT", [128, 1024], BF16)

        ident = cbf[:, 0, :]
        ones = cbf[:, 1, :]
        tri = cbf[:, 2, :]
        cpl = cbf[:, 3, :]

        class Arena:
            def __init__(self):
                self.off = 0

            def reset(self):
                self.off = 0

            def take(self, shape, dt):
                n = int(np.prod(shape))
                nbytes = n * (2 if dt == BF16 else 4)
                nbytes = (nbytes + 63) // 64 * 64
                assert self.off + nbytes <= ARENA, (self.off, nbytes)
                w0 = self.off // 4
                ap = arena[:, w0:w0 + nbytes // 4]
                self.off += nbytes
                if dt == BF16:
                    ap = ap.bitcast(BF16)
                return ap[:, 0:n]

        AR = Arena()
        _uid = [0]

        _lastoff = [0]
        need_bar = [False]

        def tile(shape, dt):
            _lastoff[0] = AR.off
            ap = AR.take(shape, dt)
            if len(shape) == 2:
                ap = ap.rearrange("p (a b) -> p a b", a=shape[0], b=shape[1])
            elif len(shape) == 3:
                ap = ap.rearrange("p (a b c) -> p a b c", a=shape[0], b=shape[1], c=shape[2])
            return ap, ("t", _lastoff[0], int(np.prod(shape)) * (2 if dt == BF16 else 4))

        def phase_end(extra=(), sp_too=False):
            if sp_too or extra:
                P.barrier(engs=(PE, ACT, DVE, POOL, SP) if sp_too else (PE, ACT, DVE, POOL), extra=extra)
            else:
                need_bar[0] = True
            AR.reset()

        def mm(out, lhsT, rhs, start, stop, R, W, inc=True):
            P.op(PE, lambda q: q.matmul(out, lhsT=lhsT, rhs=rhs, start=start, stop=stop, skip_group_check=True),
                 reads=R, writes=W, inc=inc)

        def tr(out, in_, idn, R, W):
            P.op(PE, lambda q: q.transpose(out=out, in_=in_, identity=idn), reads=R, writes=W)

        def act(out, in_, func, R, W, scale=1.0, bias=0.0):
            P.op(ACT, lambda q: q.activation(out=out, in_=in_, func=func, bias=bias, scale=scale), reads=R, writes=W)

        def tt(eng, out, in0, in1, op, R, W):
            if eng == POOL and not POOL_COMPUTE:
                eng = DVE
            P.op(eng, lambda q: q.tensor_tensor(out=out, in0=in0, in1=in1, op=op), reads=R, writes=W)

        def ts(eng, out, in0, s1, s2, op0, op1, R, W):
            if op1 is None:
                P.op(eng, lambda q: q.tensor_scalar(out=out, in0=in0, scalar1=s1, scalar2=None, op0=op0), reads=R, writes=W)
            else:
                P.op(eng, lambda q: q.tensor_scalar(out=out, in0=in0, scalar1=s1, scalar2=s2, op0=op0, op1=op1), reads=R, writes=W)

        def stt(out, in0, scalar, in1, op0, op1, R, W):
            P.op(DVE, lambda q: q.scalar_tensor_tensor(out=out, in0=in0, scalar=scalar, in1=in1, op0=op0, op1=op1), reads=R, writes=W)

        def cp(eng, out, in_, R, W):
            if eng == POOL and not POOL_COMPUTE:
                eng = DVE
            if eng == ACT:
                P.op(ACT, lambda q: q.copy(out=out, in_=in_), reads=R, writes=W)
            else:
                P.op(eng, lambda q: q.tensor_copy(out=out, in_=in_), reads=R, writes=W)

        def recip(out, in_, R, W):
            P.op(DVE, lambda q: q.reciprocal(out=out, in_=in_), reads=R, writes=W)

        def scan(out, d0, d1, init, R, W):
            P.op(DVE, lambda q: q.tensor_tensor_scan(out=out, data0=d0, data1=d1, initial=init, op0=ALU.mult, op1=ALU.add), reads=R, writes=W)

        def memset(eng, ap, val, W):
            P.op(eng, lambda q: q.memset(ap, val), writes=W)

        def dma(eng, vq, out, in_, R, W):
            P.op(eng, lambda q: q.dma_start(out=out, in_=in_), reads=R, writes=W, vq=vq)

        cload = [(idf[:], "c_idf"), (cbf[:], "c_bf"), (cmask[:], "c_mask"), (rope[:], "c_rope"),
                 (Gt[:].rearrange("p a (b c) -> p a b c", b=4, c=64), "c_G"), (gct[:], "c_gc"), (resetm[:], "c_reset"),
                 (ckm[:], "c_cmask"), (pre32[:], "p_pre"), (post32[:], "p_post"), (lblt[:], "p_lbl"), (cwt[:], "p_cw"),
                 (cbt[:], "p_cb"), (bat[:], "p_ba"), (bxt[:], "p_bx"), (nspt[:], "p_lam")]
        for i, (dst, nm) in enumerate(cload):
            dma(SP, "dq_c", dst, dram[nm], [], ["consts"])
        for o in range(2):
            dma(POOL, "dq_c2", wabd[:, o, :, :], dram["p_wa"][o], [], ["consts2"])
            dma(POOL, "dq_c2", wxbd[:, o, :, :], dram["p_wx"][o], [], ["consts2"])
        CR = ["consts", "consts2"]
        memset(DVE, lbt[:, 0, :], 0.0, ["consts"])
        tt(DVE, lbt[:, 1, :], lblt[:, 1, :], lblt[:, 0, :], ALU.subtract, CR, ["consts"])
        act(lbt[:, 1, :], lbt[:, 1, :], AF.Exp, CR, ["consts"], scale=-1.0)
        ts(DVE, lbt[:, 1, :], lbt[:, 1, :], 1.0, None, ALU.add, None, CR, ["consts"])
        recip(lbt[:, 1, :], lbt[:, 1, :], CR, ["consts"])
        ts(DVE, omlt[:], lbt[:], -1.0, 1.0, ALU.mult, ALU.add, CR, ["consts"])
        ts(DVE, nbat[:], bat[:], -1.0, None, ALU.mult, None, CR, ["consts"])
        ts(DVE, nbxt[:], bxt[:], -1.0, None, ALU.mult, None, CR, ["consts"])
        act(nspt[:], nspt[:], AF.Exp, CR, ["consts"], scale=-1.0)
        act(nspt[:], nspt[:], AF.Ln, CR, ["consts"], bias=1.0)
        ts(DVE, nsp2t[:], nspt[:], -16.0, None, ALU.mult, None, CR, ["consts"])
        ts(DVE, nspt[:], nspt[:], -8.0, None, ALU.mult, None, CR, ["consts"])
        P.barrier()

        wstate = {"n": 0, "s": 0}

        def issue_load(pieces):
            slot = wstate["n"] % 3
            wstate["n"] += 1
            c = 0
            for (w2d, c0, ncols) in pieces:
                kk = 512 // ncols
                src = w2d[:, c0:c0 + ncols].rearrange("(k p) c -> p k c", p=128)
                for k0 in range(0, 8, kk):
                    si = wstate["s"] % NSTG
                    wstate["s"] += 1
                    st3 = stg[si][:, :].rearrange("p (k c) -> p k c", k=kk)
                    dma(SP, f"dq_s{si}", st3, src[:, k0:k0 + kk, :], [], [("stg", si)])
                    P.op(POOL, lambda q, o=wg[slot][:, k0:k0 + kk, c:c + ncols], i=st3: q.tensor_copy(out=o, in_=i),
                         reads=[("stg", si)], writes=[("wg", slot)])
                c += ncols
            return slot

        pjn = [0]

        def next_pj():
            pjn[0] += 1
            i = pjn[0] % 2
            return pj[i], ("pj", i)

        def proj_fm(slot, col0, g, bank, bkey, ncols=128):
            for k in range(8):
                mm(bank[0:ncols, :], wg[slot][:, k, col0:col0 + ncols], hT[:, k, g * 512:(g + 1) * 512], k == 0, k == 7,
                   [("wg", slot), "hT"], [bkey], inc=(k == 7))

        def proj_tm(slot, col0, ncols, n, out, bkey):
            for k in range(8):
                mm(out, hT[:, k, n * 128:(n + 1) * 128], wg[slot][:, k, col0:col0 + ncols], k == 0, k == 7,
                   [("wg", slot), "hT"], [bkey], inc=(k == 7))

        def load_x(s):
            xin = [tile([1024], F32) for _ in range(2)]
            for n in range(1 if "l1" in debug_skip else (3 if "l3" in debug_skip else 16)):
                xt, xk = xin[n % 2]
                dma(SP, f"dq_x{n % 2}", xt, x_d[s, n * 128:(n + 1) * 128, :], [], [xk])
                for half in range(2):
                    bank, bkey = next_pj()
                    for cc in range(4):
                        c = half * 4 + cc
                        tr(bank[:, cc * 128:(cc + 1) * 128], xt[:, c * 128:(c + 1) * 128], idf[:], [xk], [bkey])
                    dst = X[:, half * 4:half * 4 + 4, n * 128:(n + 1) * 128]
                    if "lnocp" in debug_skip:
                        continue
                    if "l2d" in debug_skip:
                        for cc in range(4):
                            dd = X[:, half * 4 + cc, n * 128:(n + 1) * 128] if "lsc" not in debug_skip else hT[:, half * 4 + cc, 0:256].bitcast(F32)
                            cp(ACT if half == 0 else DVE, dd, bank[:, cc * 128:(cc + 1) * 128], [bkey], [("X", half * 4 + cc)])
                    else:
                        cp(ACT if half == 0 else DVE, dst, bank[:, :].rearrange("p (a b) -> p a b", a=4), [bkey], [("X", half * 4 + i) for i in range(4)])
            phase_end()

        def store_x(s):
            xo = [tile([1024], F32) for _ in range(2)]
            for n in range(16):
                xt, xk = xo[n % 2]
                for half in range(2):
                    bank, bkey = next_pj()
                    for cc in range(4):
                        c = half * 4 + cc
                        tr(bank[:, cc * 128:(cc + 1) * 128], X[:, c, n * 128:(n + 1) * 128], idf[:], [("X", c)], [bkey])
                    cp(ACT if half == 0 else DVE, xt[:, half * 512:(half + 1) * 512], bank[:, :], [bkey], [xk])
                dma(SP, f"dq_o{n % 2}", out_d[s, n * 128:(n + 1) * 128, :], xt, [xk], [("outd", n % 2)])
            phase_end(extra=[("outd", 0), ("outd", 1)], sp_too=True)

        def rstd_from_ssq(dst, ssq_ps, n, R, W):
            act(dst, ssq_ps, AF.Ln, R, W, scale=1.0 / n, bias=EPS)
            act(dst, dst, AF.Exp, W, W, scale=-0.5)

        def pre_norm(L):
            sq = [tile([512], BF16) for _ in range(2)]
            rs = [tile([512], F32) for _ in range(2)]
            for g in range(4):
                sl = slice(g * 512, (g + 1) * 512)
                for c in range(8):
                    st, sk = sq[c % 2]
                    act(st, X[:, c, sl], AF.Square, [("X", c)], [sk])
                    mm(pm[4][:, :], ones, st, c == 0, c == 7, [sk], ["pm4"], inc=True)
                rt, rk = rs[g % 2]
                rstd_from_ssq(rt, pm[4][:, :], D, ["pm4"], [rk])
                for c in range(8):
                    stt(hT[:, c, sl], X[:, c, sl], pre32[:, L, c:c + 1], rt, ALU.mult, ALU.mult, [("X", c), rk, "consts"], ["hT"])
            phase_end()

        def out_proj(L, slots):
            if dbg_d is not None:
                dma(SP, "dq_dbg", dbg_d, mixT[:], [("mixT", c) for c in range(8)], ["dbgd"])
            Yg, yk = tile([8, 512], F32)
            sq = [tile([512], BF16) for _ in range(2)]
            rt, rk = tile([512], F32)
            tmp = [tile([512], F32) for _ in range(2)]
            for g in range(4):
                sl = slice(g * 512, (g + 1) * 512)
                for dc in range(8):
                    bank, bkey = next_pj()
                    slot = slots[dc // 4]
                    for k in range(8):
                        mm(bank[:, :], wg[slot][:, k, (dc % 4) * 128:(dc % 4 + 1) * 128], mixT[:, k, sl], k == 0, k == 7,
                           [("wg", slot), ("mixT", k)], [bkey], inc=(k == 7))
                    st, sk = sq[dc % 2]
                    cp(DVE, Yg[:, dc, :], bank[:, :], [bkey], [(yk, dc)])
                    act(st, bank[:, :], AF.Square, [bkey], [sk])
                    mm(pm[4][:, :], ones, st, dc == 0, dc == 7, [sk], ["pm4"], inc=True)
                if "op1" in debug_skip:
                    continue
                rstd_from_ssq(rt, pm[4][:, :], D, ["pm4"], [rk])
                if "op2" in debug_skip:
                    continue
                for dc in range(8):
                    tp, tk = tmp[dc % 2]
                    stt(tp, Yg[:, dc, :], post32[:, L, dc:dc + 1], rt, ALU.mult, ALU.mult, [(yk, dc), rk, "consts"], [tk])
                    tt(POOL, X[:, dc, sl], X[:, dc, sl], tp, ALU.add, [("X", dc), tk], [("X", dc)])
            phase_end()

        def gate_phase(slot, chunks):
            sg = [tile([512], BF16) for _ in range(2)]
            i = 0
            for ci, c in enumerate(chunks):
                for g in range(4):
                    sl = slice(g * 512, (g + 1) * 512)
                    bank, bkey = next_pj()
                    proj_fm(slot, ci * 128, g, bank, bkey)
                    st, sk = sg[i % 2]
                    i += 1
                    act(st, bank[:, :], AF.Silu, [bkey], [sk])
                    tt(DVE if (i % 2) else POOL, mixT[:, c, sl], mixT[:, c, sl], st, ALU.mult, [sk, ("mixT", c)], [("mixT", c)])
            phase_end()

        def retention_pair(pr, slot):
            qk_tm, k_qk = tile([16, 256], BF16)
            qkT, k_qkT = tile([2, SEQ], BF16)
            v_tm, k_v = tile([16, 256], BF16)
            qs = [tile([256], F32) for _ in range(1)]
            tmp = [[tile([4, 32], F32) for _ in range(4)] for _ in range(1)]
            for n in range(16):
                bank, bkey = next_pj()
                proj_tm(slot, 0, 256, n, bank[:, 0:256], bkey)
                qt, qk_ = qs[0]
                tt(DVE, qt, bank[:, 0:256], Gt[:, pr, :], ALU.mult, [bkey, "consts"], [qk_])
                q3 = qt.rearrange("p (a b) -> p a b", a=4)
                x1 = q3[:, :, 0:32]
                x2 = q3[:, :, 32:64]
                cosb = rope[:, 0, n, :].unsqueeze(1).to_broadcast([128, 4, 32])
                sinb = rope[:, 1, n, :].unsqueeze(1).to_broadcast([128, 4, 32])
                (t1, k1), (t2, k2), (t3, k3), (t4, k4) = tmp[0]
                o3 = qk_tm[:, n, :].rearrange("p (a b) -> p a b", a=4)
                tt(DVE, t1, x1, cosb, ALU.mult, [qk_, "consts"], [k1])
                tt(DVE, t2, x2, sinb, ALU.mult, [qk_, "consts"], [k2])
                tt(DVE, o3[:, :, 0:32], t1, t2, ALU.subtract, [k1, k2], [(k_qk, n)])
                tt(DVE, t3, x1, sinb, ALU.mult, [qk_, "consts"], [k3])
                tt(DVE, t4, x2, cosb, ALU.mult, [qk_, "consts"], [k4])
                tt(DVE, o3[:, :, 32:64], t3, t4, ALU.add, [k3, k4], [(k_qk, n)])
                bank2, bkey2 = next_pj()
                proj_tm(slot, 256, 256, n, bank2[:, 0:256], bkey2)
                cp(ACT, v_tm[:, n, :], bank2[:, 0:256], [bkey2], [(k_v, n)])
            if "r_a" in debug_skip:
                phase_end()
                return
            for n0 in range(0, 16, 4):
                for which in range(2):
                    for j in range(4):
                        n = n0 + j
                        tr(pT[:, which * 512 + j * 128: which * 512 + (j + 1) * 128], qk_tm[:, n, which * 128:(which + 1) * 128],
                           ident, [(k_qk, n), "consts"], ["pT"])
                cp(ACT, qkT[:, :, n0 * 128:(n0 + 4) * 128], pT[:, :].rearrange("p (a b) -> p a b", a=2), ["pT"], [(k_qkT, n0)])
            if "r_b" in debug_skip:
                phase_end()
                return
            Sst, k_S = tile([128], F32)
            SB, k_SB = tile([128], BF16)
            sTm = [tile([256], BF16) for _ in range(2)]
            sq = [tile([256], BF16) for _ in range(2)]
            rs = [tile([256], F32) for _ in range(2)]
            memset(DVE, Sst, 0.0, [k_S])
            for n in range(16):
                n0 = (n // 4) * 4
                csl = slice(n * 128, (n + 1) * 128)
                st, sk = sTm[n % 2]
                for hh in range(2):
                    ps_ = slice(hh * 64, (hh + 1) * 64)
                    sbank, skey = (pm[0], "pm0") if hh == 0 else (pm[4], "pm4")
                    mm(sbank[:, 0:128], qkT[ps_, 1, csl], qkT[ps_, 0, csl], True, True, [(k_qkT, n0)], [skey])
                    tt(DVE, st[:, hh * 128:(hh + 1) * 128], sbank[:, 0:128], cmask[:, 0, :], ALU.mult, [skey, "consts"], [sk])
                for hh in range(2):
                    if "r_c1" in debug_skip:
                        continue
                    mm(pm[1][hh * 64:(hh + 1) * 64, 0:128], qk_tm[:, n, 128 + hh * 64:128 + (hh + 1) * 64], v_tm[:, n, hh * 128:(hh + 1) * 128],
                       True, True, [(k_qk, n), (k_v, n)], ["pm1"], inc=(hh == 1))
                for hh in range(2):
                    ps_ = slice(hh * 64, (hh + 1) * 64)
                    osl = pm[2][:, hh * 128:(hh + 1) * 128]
                    mm(osl, v_tm[:, n, hh * 128:(hh + 1) * 128], st[:, hh * 128:(hh + 1) * 128], True, n == 0, [(k_v, n), sk], ["pm2"],
                       inc=(n == 0 and hh == 1))
                    if n > 0 and "r_c1" not in debug_skip:
                        mm(osl, SB[ps_, :], qkT[ps_, 0, csl], False, True, [k_SB, (k_qkT, n0)], ["pm2"], inc=(hh == 1))
                if "r_c1" not in debug_skip:
                    stt(Sst, Sst, gct[:, pr:pr + 1], pm[1][:, 0:128], ALU.mult, ALU.add, [k_S, "pm1", "consts"], [k_S])
                    act(SB, Sst, AF.Copy, [k_S, "consts"], [k_SB], scale=gct[:, pr:pr + 1])
                qt, qk2 = sq[n % 2]
                act(qt, pm[2][:, 0:256], AF.Square, ["pm2"], [qk2])
                mm(pm[3][:, 0:256], ones, qt, True, True, [qk2, "consts"], ["pm3"])
                rt, rk = rs[n % 2]
                rstd_from_ssq(rt, pm[3][:, 0:256], 128, ["pm3"], [rk])
                tt(DVE, mixT[:, 2 * pr:2 * pr + 2, csl], pm[2][:, 0:256].rearrange("p (a b) -> p a b", a=2),
                   rt.rearrange("p (a b) -> p a b", a=2), ALU.mult, ["pm2", rk], [("mixT", 2 * pr), ("mixT", 2 * pr + 1)])
            phase_end()

        def sb_pair(pr, slot):
            qT, k_q = tile([SEQ], BF16)
            kT, k_k = tile([SEQ], BF16)
            v_tm, k_v = tile([16, 128], BF16)
            for g in range(4):
                sl = slice(g * 512, (g + 1) * 512)
                bank, bkey = next_pj()
                proj_fm(slot, 0, g, bank, bkey)
                cp(ACT, qT[:, sl], bank[:, :], [bkey], [(k_q, g)])
                bank, bkey = next_pj()
                proj_fm(slot, 128, g, bank, bkey)
                act(kT[:, sl], bank[:, :], AF.Copy, [bkey], [(k_k, g)], scale=0.125)
            for n0 in range(0, 16, 4):
                bank, bkey = next_pj()
                for j in range(4):
                    proj_tm(slot, 256, 128, n0 + j, bank[:, j * 128:(j + 1) * 128], bkey)
                cp(DVE, v_tm[:, n0:n0 + 4, :], bank[:, :].rearrange("p (a b) -> p a b", a=4), [bkey], [(k_v, n0 // 4)])
            NB = 2
            e_t = [[tile([512], F32) for _ in range(NB)] for _ in range(2)]
            sp_t = [[tile([512], BF16) for _ in range(NB)] for _ in range(2)]
            ea_t = [[tile([512], BF16) for _ in range(NB)] for _ in range(2)]
            w_t = [[tile([512], BF16) for _ in range(NB)] for _ in range(2)]
            it = 0
            for G in range(4):
                gsl0 = G * 512
                started_P = [[False], [False]]
                started_O = [[False], [False]]
                blocks = list(range(4 * G + 3, -1, -1))

                def cols(b):
                    c0 = (b - 4 * G) * 128 if b >= 4 * G else 0
                    return c0

                def emit_Z(b):
                    c0 = cols(b)
                    for hh in range(2):
                        ps_ = slice(hh * 64, (hh + 1) * 64)
                        mm(pm[hh][:, c0:512], kT[ps_, b * 128:(b + 1) * 128], qT[ps_, gsl0 + c0:gsl0 + 512], True, True,
                           [(k_k, b // 4), (k_q, G)], [f"pm{hh}"])

                def acc(bank, started, lhsT, rhs_ap, c0, R, W, outp=slice(0, 128)):
                    mm(bank[outp, c0:512], lhsT, rhs_ap[:, c0:512], not started[0], True, R, W)
                    started[0] = True

                emit_Z(blocks[0])
                for bi, b in enumerate(blocks):
                    c0 = cols(b)
                    buf = it % NB
                    it += 1
                    diag = b >= 4 * G
                    for hh in range(2):
                        et, ek = e_t[hh][buf]
                        act(et[:, c0:512], pm[hh][:, c0:512], AF.Exp, [f"pm{hh}"], [ek])
                    if diag:
                        for hh in range(2):
                            et, ek = e_t[hh][buf]
                            tt(DVE, et[:, c0:c0 + 128], et[:, c0:c0 + 128], cmask[:, 1, :], ALU.mult, [ek, "consts"], [ek])
                    for hh in range(2):
                        et, ek = e_t[hh][buf]
                        st, sk = sp_t[hh][buf]
                        act(st[:, c0:512], et[:, c0:512], AF.Ln, [ek], [sk], bias=1.0)
                    for hh in range(2):
                        st, sk = sp_t[hh][buf]
                        acc(pm[2 + hh], started_P[hh], tri, st, c0, [sk, "consts"], [f"pm{2 + hh}"])
                    if bi + 1 < len(blocks):
                        emit_Z(blocks[bi + 1])
                    for hh in range(2):
                        at_, ak = ea_t[hh][buf]
                        act(at_[:, c0:512], pm[2 + hh][:, c0:512], AF.Exp, [f"pm{2 + hh}"], [ak], scale=-1.0)
                    for hh in range(2):
                        st, sk = sp_t[hh][buf]
                        acc(pm[2 + hh], started_P[hh], cpl, st, c0, [sk, "consts"], [f"pm{2 + hh}"])
                    for hh in range(2):
                        et, ek = e_t[hh][buf]
                        at_, ak = ea_t[hh][buf]
                        wt, wk = w_t[hh][buf]
                        tt(DVE, wt[:, c0:512], et[:, c0:512], at_[:, c0:512], ALU.mult, [ek, ak], [wk])
                    for hh in range(2):
                        wt, wk = w_t[hh][buf]
                        acc(pm[4], started_O[hh], v_tm[:, b, hh * 64:(hh + 1) * 64], wt, c0, [wk, (k_v, b // 4)], ["pm4"],
                            outp=slice(hh * 64, (hh + 1) * 64))
                cp(ACT, mixT[:, 4 + pr, gsl0:gsl0 + 512], pm[4][:, :], ["pm4"], [("mixT", 4 + pr)])
            phase_end()

        def hgrn_head(o, h, slot):
            NCK = 128 // HC
            qT, k_q = tile([SEQ], BF16)
            kT, k_k = tile([SEQ], BF16)
            dch, k_d = tile([SEQ // HC], F32)
            (a1, ka1), (a2, ka2), (a3, ka3) = tile([512], F32), tile([512], F32), tile([512], F32)
            lb_ap = lbt[:, o, h:h + 1]
            oml_ap = omlt[:, o, h:h + 1]
            for g in range(4):
                sl = slice(g * 512, (g + 1) * 512)
                bank, bkey = next_pj()
                proj_fm(slot, 128, g, bank, bkey)
                act(a1, bank[:, :], AF.Exp, [bkey], [ka1], scale=-1.0)
                act(a1, a1, AF.Ln, [ka1], [ka1], bias=1.0)
                act(a1, a1, AF.Exp, [ka1], [ka1], scale=-1.0)
                ts(DVE, a1, a1, oml_ap, lb_ap, ALU.mult, ALU.add, [ka1, "consts"], [ka1])
                ts(DVE, a2, a1, -1.0, 1.0, ALU.mult, ALU.add, [ka1], [ka2])
                act(a3, a1, AF.Ln, [ka1], [ka3])
                scan(a1, resetm[:, :], a3, 0.0, [ka3, "consts"], [ka1])
                act(a3, a1, AF.Exp, [ka1], [ka3])
                act(a1, a1, AF.Exp, [ka1], [ka1], scale=-1.0)
                bank2, bkey2 = next_pj()
                proj_fm(slot, 0, g, bank2, bkey2)
                tt(DVE, qT[:, sl], bank2[:, :], a3, ALU.mult, [bkey2, ka3], [(k_q, g)])
                tt(POOL, kT[:, sl], a2, a1, ALU.mult, [ka1, ka2], [(k_k, g)])
                nch = 512 // HC
                cp(ACT, dch[:, g * nch:(g + 1) * nch], a3.rearrange("p (c j) -> p c j", j=HC)[:, :, HC - 1], [ka3], [k_d])
            v4 = [tile([4, 128], BF16) for _ in range(2)]
            k4 = [tile([4, 128], BF16) for _ in range(2)]
            vt, vk = tile([NCK, 128], BF16)
            atm = [tile([128], BF16) for _ in range(2)]
            up, uk = tile([NCK, 128], F32)
            Sall = [tile([NCK, 128], F32) for _ in range(2)]
            Sbf = [tile([NCK, 128], BF16) for _ in range(2)]
            sq, k_sq = tile([512], BF16)
            rt, k_rt = tile([512], F32)
            for n in range(16):
                g4 = n // 4
                j4 = n % 4
                vt4, vk4 = v4[g4 % 2]
                kt4, kk4 = k4[g4 % 2]
                if j4 == 0:
                    bank, bkey = next_pj()
                    for j in range(4):
                        proj_tm(slot, 256, 128, n + j, bank[:, j * 128:(j + 1) * 128], bkey)
                    cp(ACT, vt4, bank[:, :].rearrange("p (a b) -> p a b", a=4), [bkey], [vk4])
                    for j in range(4):
                        tr(pT[:, j * 128:(j + 1) * 128], kT[:, (n + j) * 128:(n + j + 1) * 128], ident, [(k_k, g4), "consts"], ["pT"])
                    cp(DVE, kt4, pT[:, 0:512].rearrange("p (a b) -> p a b", a=4), ["pT"], [kk4])
                tsl = slice(n * 128, (n + 1) * 128)
                ob = pm[2 + g4 % 2]
                okey = f"pm{2 + g4 % 2}"
                ocol = j4 * 128
                mm(pm[0][:, 0:128], kT[:, tsl], qT[:, tsl], True, True, [(k_k, g4), (k_q, g4)], ["pm0"])
                am, ak = atm[n % 2]
                tt(DVE, am, pm[0][:, 0:128], cmask[:, 2, :], ALU.mult, ["pm0", "consts"], [ak])
                tt(DVE, vt, vt4[:, j4, :].unsqueeze(1).to_broadcast([128, NCK, 128]),
                   ckm[:, :].unsqueeze(2).to_broadcast([128, NCK, 128]), ALU.mult, [vk4, "consts"], [vk])
                mm(pm[1][:, 0:NCK * 128], kt4[:, j4, :], vt.rearrange("p a b -> p (a b)"), True, True, [kk4, vk], ["pm1"])
                tt(DVE, up, pm[1][:, 0:NCK * 128].rearrange("p (a b) -> p a b", a=NCK),
                   dch[:, n * NCK:(n + 1) * NCK].unsqueeze(2).to_broadcast([128, NCK, 128]), ALU.mult, ["pm1", k_d], [uk])
                sa, sak = Sall[n % 2]
                sprev, spk = Sall[(n - 1) % 2]
                for c in range(NCK):
                    cc = n * NCK + c
                    if cc == 0:
                        cp(DVE, sa[:, 0, :], up[:, 0, :], [uk], [sak])
                    else:
                        prev = sprev[:, NCK - 1, :] if c == 0 else sa[:, c - 1, :]
                        stt(sa[:, c, :], prev, dch[:, cc:cc + 1], up[:, c, :], ALU.mult, ALU.add, [uk, sak, spk, k_d], [sak])
                sb_, sbk = Sbf[n % 2]
                sbp, sbpk = Sbf[(n - 1) % 2]
                cp(ACT, sb_, sa, [sak], [sbk])
                mm(ob[:, ocol:ocol + 128], vt4[:, j4, :], am, True, False, [vk4, ak], [okey])
                for c in range(NCK):
                    cc = n * NCK + c
                    if cc == 0:
                        continue
                    lhs = sbp[:, NCK - 1, :] if c == 0 else sb_[:, c - 1, :]
                    mm(ob[:, ocol + c * HC: ocol + (c + 1) * HC], lhs, qT[:, n * 128 + c * HC: n * 128 + (c + 1) * HC], False, True,
                       [sbk, sbpk, (k_q, g4)], [okey])
                if j4 == 3:
                    n0 = n - 3
                    act(sq, ob[:, :], AF.Square, [okey], [k_sq])
                    mm(pm[4][:, :], ones, sq, True, True, [k_sq, "consts"], ["pm4"])
                    rstd_from_ssq(rt, pm[4][:, :], 128, ["pm4"], [k_rt])
                    tt(DVE, mixT[:, h, n0 * 128:(n0 + 4) * 128], ob[:, :], rt, ALU.mult, [okey, k_rt], [("mixT", h)])
            phase_end()

        def rglru_chunk(o, j, slot):
            lxp, k_lx = tile([SEQ + 4], F32)
            xc_t = [tile([512], F32) for _ in range(1)]
            xcb = [tile([512], BF16) for _ in range(1)]
            (r_t, k_r), (ig_t, k_ig) = tile([512], F32), tile([512], F32)
            (at_, ak), (mt, mk) = tile([512], F32), tile([512], F32)
            h_t = [tile([512], F32) for _ in range(2)]
            memset(DVE, lxp[:, 0:4], 0.0, [(k_lx, -1)])
            for g in range(4):
                bank, bkey = next_pj()
                proj_fm(slot, j * 128, g, bank, bkey)
                cp(ACT, lxp[:, 4 + g * 512: 4 + (g + 1) * 512], bank[:, :], [bkey], [(k_lx, g)])
            for g in range(4):
                sl = slice(g * 512, (g + 1) * 512)
                xc, xck = xc_t[0]
                R = [(k_lx, g), (k_lx, g - 1), "consts"]
                ts(DVE, xc, lxp[:, 1 + g * 512: 1 + (g + 1) * 512], cwt[:, o, 0, j:j + 1], cbt[:, o, j:j + 1], ALU.mult, ALU.add, R, [xck])
                for jj in range(1, 4):
                    stt(xc, lxp[:, 1 + jj + g * 512: 1 + jj + (g + 1) * 512], cwt[:, o, jj, j:j + 1], xc, ALU.mult, ALU.add,
                        R + [xck], [xck])
                xb, xbk = xcb[0]
                cp(ACT, xb, xc, [xck], [xbk])
                mm(pm[0][:, :], wabd[:, o, j, :], xb, True, True, [xbk, "consts2"], ["pm0"])
                mm(pm[1][:, :], wxbd[:, o, j, :], xb, True, True, [xbk, "consts2"], ["pm1"])
                act(r_t, pm[0][:, :], AF.Exp, ["pm0", "consts"], [k_r], scale=-1.0, bias=nbat[:, o, j:j + 1])
                act(ig_t, pm[1][:, :], AF.Exp, ["pm1", "consts"], [k_ig], scale=-1.0, bias=nbxt[:, o, j:j + 1])
                act(r_t, r_t, AF.Ln, [k_r], [k_r], bias=1.0)
                act(ig_t, ig_t, AF.Ln, [k_ig], [k_ig], bias=1.0)
                act(r_t, r_t, AF.Exp, [k_r], [k_r], scale=-1.0)
                act(ig_t, ig_t, AF.Exp, [k_ig], [k_ig], scale=-1.0)
                tt(POOL, ig_t, ig_t, xc, ALU.mult, [k_ig, xck], [k_ig])
                ht, hk = h_t[g % 2]
                hp, hpk = h_t[(g - 1) % 2]
                act(at_, r_t, AF.Exp, [k_r, "consts"], [ak], scale=nspt[:, o, j:j + 1])
                act(mt, r_t, AF.Exp, [k_r, "consts"], [mk], scale=nsp2t[:, o, j:j + 1])
                ts(DVE, mt, mt, -1.0, 1.0, ALU.mult, ALU.add, [mk], [mk])
                act(mt, mt, AF.Ln, [mk], [mk], bias=1e-30)
                act(mt, mt, AF.Exp, [mk], [mk], scale=0.5)
                if g == 0:
                    memset(DVE, mt[:, 0:1], 1.0, [mk])
                tt(POOL, mt, mt, ig_t, ALU.mult, [mk, k_ig], [mk])
                init = 0.0 if g == 0 else hp[:, 511:512]
                scan(ht, at_, mt, init, [ak, mk, hpk], [hk])
                cp(ACT, mixT[:, 4 + j, sl], ht, [hk], [("mixT", 4 + j)])
            phase_end()

        tasks = []

        def add(pieces, fn, kind=None):
            tasks.append((pieces, fn, kind))

        for s in range(nseq):
            if "io" not in debug_skip:
                add(None, lambda slot, s=s: load_x(s))
            for L in layers:
                if "nopre" not in debug_skip:
                    add(None, lambda slot, L=L: pre_norm(L))
                if L % 2 == 0:
                    e = L // 2
                    W = ewin[e]
                    if "ret" in debug_skip:
                        add(None, lambda slot: memset(DVE, mixT[:, 0:4, :], 1.0, [("mixT", c) for c in range(4)]))
                    if "sb" in debug_skip:
                        add(None, lambda slot: memset(DVE, mixT[:, 4:8, :], 1.0, [("mixT", c) for c in range(4, 8)]))
                    for pr in range(2):
                        if "ret" in debug_skip:
                            continue
                        add([(W, pr * 128, 128), (W, 256 + pr * 128, 128), (W, 512 + pr * 256, 256)],
                            lambda slot, pr=pr: retention_pair(pr, slot), 'ret')
                    if "nogate" not in debug_skip:
                        add([(W, 1024, 512)], lambda slot: gate_phase(slot, [0, 1, 2, 3]))
                    for pr in range(4):
                        if "sb" in debug_skip:
                            continue
                        add([(W, 1536 + pr * 128, 128), (W, 2048 + pr * 128, 128), (W, 2560 + pr * 128, 128)],
                            lambda slot, pr=pr: sb_pair(pr, slot), 'sb')
                    if "nogate" not in debug_skip:
                        add([(W, 3072, 512)], lambda slot: gate_phase(slot, [4, 5, 6, 7]))
                    WO = ewout[e]
                else:
                    o = L // 2
                    W = owin[o]
                    for h in range(4):
                        if "hgrn" in debug_skip:
                            continue
                        add([(W, h * 128, 128), (W, 512 + h * 128, 128), (W, 1024 + h * 128, 128)],
                            lambda slot, o=o, h=h: hgrn_head(o, h, slot), 'hgrn')
                    add([(W, 1536, 512)], lambda slot: gate_phase(slot, [0, 1, 2, 3]))
                    holder = {}
                    add([(W, 2048, 512)], lambda slot, holder=holder: holder.__setitem__("lx", slot))
                    for j in range(4):
                        if "lru" in debug_skip:
                            continue
                        add(None, lambda slot, o=o, j=j, holder=holder: rglru_chunk(o, j, holder["lx"]), 'lru')
                    add([(W, 2560, 512)], lambda slot: gate_phase(slot, [4, 5, 6, 7]))
                    WO = owout[o]
                if "noout" in debug_skip:
                    continue
                holder2 = {}
                add([(WO, 0, 512)], lambda slot, holder2=holder2: holder2.__setitem__("a", slot))
                add([(WO, 512, 512)], lambda slot, L=L, holder2=holder2: out_proj(L, [holder2["a"], slot]))
            if "io" not in debug_skip and "st" not in debug_skip:
                add(None, lambda slot, s=s: store_x(s))

        wtasks = [i for i, t in enumerate(tasks) if t[0] is not None]
        slots = {}
        nxt = 0

        def ensure_issued(upto):
            nonlocal nxt
            while nxt < len(wtasks) and nxt <= upto:
                ti = wtasks[nxt]
                slots[ti] = issue_load(tasks[ti][0])
                nxt += 1

        wpos = {ti: k for k, ti in enumerate(wtasks)}
        prev_kind = None
        for i, (pieces, fn, kind) in enumerate(tasks):
            if need_bar[0] and not (kind is not None and kind == prev_kind):
                P.barrier()
            need_bar[0] = False
            if pieces is not None:
                ensure_issued(wpos[i] + 1)
                fn(slots[i])
            else:
                fn(None)
            if need_bar[0]:
                prev_kind = kind
        if "io" in debug_skip or "st" in debug_skip:
            dma(SP, "dq_o0", out_d[0, 0:128, :], X[:, 0, 0:1024], [("X", 0)], [("outd", 0)])
        P.op(SP, lambda q: q.nop(), reads=[("outd", 0), ("outd", 1)])
        P.emit()
    return nc


_CACHE = {}


def _get_prog(layers):
    key = tuple(layers)
    if key not in _CACHE:
        _CACHE[key] = build(list(layers))
    return _CACHE[key]


def kernel(**inputs):
    x = np.ascontiguousarray(inputs["x"], dtype=np.float32)
    consts = _consts()
    params = _params(inputs)
    base = {k: np.ascontiguousarray(inputs[k], dtype=np.float32) for k in ("even_w_in", "even_w_out", "odd_w_in", "odd_w_out")}
    base.update(consts)
    base.update(params)
    cur = x.reshape(NCORES, NSEQ, SEQ, D)
    for layers in LAYER_GROUPS:
        nc = _get_prog(layers)
        in_maps = []
        for c in range(NCORES):
            m = dict(base)
            m["x"] = np.ascontiguousarray(cur[c])
            in_maps.append(m)
        res = run_bass_kernel_spmd(nc, in_maps, core_ids=list(range(NCORES)))
        cur = np.stack([np.asarray(r["out"], dtype=np.float32) for r in res.results], axis=0)
    return cur.reshape(NCORES * NSEQ, SEQ, D)
```

```python
import numpy as np
import ml_dtypes
from contextlib import ExitStack
import concourse.bass as bass
import concourse.mybir as mybir
from concourse.bass_utils import run_bass_kernel_spmd

F32 = mybir.dt.float32
BF16 = mybir.dt.bfloat16
AF = mybir.ActivationFunctionType
ALU = mybir.AluOpType
PE, ACT, DVE, POOL, SP = "tensor", "scalar", "vector", "gpsimd", "sync"
ENGS = [PE, ACT, DVE, POOL, SP]

NCORES = 8
SEQ = 2048
D = 1024
NSEQ = 2
EPS = 1e-6
HC = 32
POOL_COMPUTE = True
LAYER_GROUPS = [[0, 1, 2, 3]]


def _base_of(k):
    if isinstance(k, tuple):
        if len(k) == 3 and k[0] == "t":
            return k
        if len(k) == 2 and isinstance(k[0], tuple) and len(k[0]) == 3 and k[0][0] == "t":
            return k[0]
    return None


def _is_psum_key(k):
    if isinstance(k, str):
        return k.startswith("pm") or k == "pT"
    return isinstance(k, tuple) and k[0] == "pj"


class Prog:
    def __init__(self, nc, es):
        self.nc = nc
        self.es = es
        self.stream = {e: [] for e in ENGS}
        self.cnt = {}
        self.sem = {}
        self.mult = {}
        self.clock = {e: {} for e in ENGS}
        self.iclock = {}
        self.lastw = {}
        self.readers = {}
        self.marked = set()
        self.pending = {}
        self.base_acc = {}
        self.predeps = {}
        for e in ENGS:
            self._mkq(e, 1)

    def _mkq(self, name, mult):
        self.cnt[name] = 0
        self.sem[name] = self.es.enter_context(self.nc.semaphore("s_" + name))
        self.mult[name] = mult

    def op(self, eng, fn, reads=(), writes=(), vq=None, inc=True):
        me = vq or eng
        if vq is not None and vq not in self.cnt:
            self._mkq(vq, 16)
        deps = {}

        def need(t):
            e, i = t
            if i > deps.get(e, 0):
                deps[e] = i

        bases = set()
        for k in list(reads) + list(writes):
            b = _base_of(k)
            if b is not None:
                bases.add(b)
        for b in bases:
            pd = self.predeps.get(b)
            if pd:
                for e2, i2 in pd.items():
                    need((e2, i2))
        for r in reads:
            lw = self.lastw.get(r)
            if lw:
                need(lw)
            if _is_psum_key(r):
                for e2, i2 in self.readers.get(r, {}).items():
                    if e2 != me:
                        need((e2, i2))
        for w in writes:
            lw = self.lastw.get(w)
            if lw and (lw[0] != me or me == POOL):
                need(lw)
            for e2, i2 in self.readers.get(w, {}).items():
                need((e2, i2))
        if self.pending.get(eng):
            for e2, i2 in self.pending.pop(eng).items():
                need((e2, i2))
        clk = self.clock[eng]
        waits = []
        for e, i in deps.items():
            if e == PE and me == PE:
                continue
            if clk.get(e, 0) >= i:
                continue
            waits.append((e, i))
            snap = self.iclock.get((e, i))
            if snap:
                for k, v in snap.items():
                    if clk.get(k, 0) < v:
                        clk[k] = v
            clk[e] = max(clk.get(e, 0), i)
        self.cnt[me] += 1
        idx = self.cnt[me]
        self.iclock[(me, idx)] = dict(clk)
        self.stream[eng].append((waits, fn, me, idx))
        for b in bases:
            d = self.base_acc.setdefault(b, {})
            if d.get(me, 0) < idx:
                d[me] = idx
        for w_ in waits:
            self.marked.add(w_)
        if self.mult[me] == 16:
            self.marked.add((me, idx))
        for r in reads:
            d = self.readers.setdefault(r, {})
            if d.get(me, 0) < idx:
                d[me] = idx
        for w in writes:
            self.lastw[w] = (me, idx)
            self.readers[w] = {}
        return idx

    def barrier(self, engs=(PE, ACT, DVE, POOL), extra=()):
        deps = {e: self.cnt[e] for e in engs if self.cnt[e] > 0}
        for k in extra:
            lw = self.lastw.get(k)
            if lw:
                deps[lw[0]] = max(deps.get(lw[0], 0), lw[1])
        for e in engs:
            d = self.pending.setdefault(e, {})
            for e2, i2 in deps.items():
                if e2 != e and d.get(e2, 0) < i2:
                    d[e2] = i2

    def emit(self):
        nc = self.nc
        mcount = {}
        for q_ in self.cnt:
            c = 0
            for i in range(1, self.cnt[q_] + 1):
                if (q_, i) in self.marked:
                    c += 1
                    mcount[(q_, i)] = c
        with nc.Block() as block:
            for eng in ENGS:
                stream = self.stream[eng]
                if not stream:
                    continue

                def body(q, stream=stream):
                    for waits, fn, me, idx in stream:
                        for w_ in waits:
                            q.wait_ge(self.sem[w_[0]], mcount[w_] * self.mult[w_[0]])
                        ins = fn(q)
                        if (me, idx) in mcount:
                            ins.then_inc(self.sem[me], self.mult[me])

                getattr(block, eng)(body)


def _consts():
    bf = ml_dtypes.bfloat16
    c = {}
    idx = np.arange(128)
    c["c_idf"] = np.eye(128, dtype=np.float32)
    cb = np.zeros((128, 6, 128), np.float32)
    cb[:, 0] = np.eye(128)
    cb[:, 1] = 1.0
    cb[:, 2] = (idx[:, None] >= idx[None, :])
    cb[:, 3] = (idx[:, None] < idx[None, :])
    c["c_bf"] = cb.astype(bf)
    cm = np.zeros((128, 3, 128), np.float32)
    cm[:, 0] = (idx[None, :] >= idx[:, None])
    cm[:, 1] = (idx[:, None] < idx[None, :])
    cm[:, 2] = ((idx[:, None] // HC) == (idx[None, :] // HC)) & (idx[None, :] >= idx[:, None])
    c["c_mask"] = cm
    half = 32
    inv = (10000.0 ** (-(np.arange(half, dtype=np.float32)) / half)).astype(np.float32)
    pos = np.arange(SEQ, dtype=np.float32)
    ang = (pos[:, None] * inv[None, :]).astype(np.float32)
    cs = np.zeros((128, 2, 16, 32), np.float32)
    cs[:, 0] = np.cos(ang).astype(np.float32).reshape(16, 128, 32).transpose(1, 0, 2)
    cs[:, 1] = np.sin(ang).astype(np.float32).reshape(16, 128, 32).transpose(1, 0, 2)
    c["c_rope"] = cs
    g = 1.0 - 2.0 ** (-5.0 - np.arange(4, dtype=np.float64))
    G = np.zeros((128, 2, 4, 64), np.float64)
    gc = np.zeros((128, 2), np.float64)
    for pr in range(2):
        for hh in range(2):
            h = 2 * pr + hh
            G[:, pr, hh, :] = (g[h] ** idx)[:, None]
            G[:, pr, 2 + hh, :] = (g[h] ** (-idx.astype(np.float64)))[:, None] * (64 ** -0.5)
            gc[hh * 64:(hh + 1) * 64, pr] = g[h] ** 128
    c["c_G"] = G.astype(np.float32)
    c["c_gc"] = gc.astype(np.float32)
    rm = np.ones((128, 512), np.float32)
    rm[:, ::HC] = 0.0
    c["c_reset"] = rm.astype(bf)
    nck = 128 // HC
    c["c_cmask"] = ((idx[:, None] // HC) == np.arange(nck)[None, :]).astype(np.float32).astype(bf)
    return c


def _params(inp):
    p = {}
    f = lambda a: np.ascontiguousarray(a, dtype=np.float32)
    p["p_pre"] = f(inp["pre_norm_w"].reshape(4, 8, 128).transpose(2, 0, 1))
    p["p_post"] = f(inp["post_norm_w"].reshape(4, 8, 128).transpose(2, 0, 1))
    p["p_lbl"] = f(inp["hgrn_lb_logits"].reshape(2, 4, 128).transpose(2, 0, 1))
    p["p_cw"] = f(inp["conv_w"].reshape(2, 4, 4, 128).transpose(3, 0, 1, 2))
    p["p_cb"] = f(inp["conv_b"].reshape(2, 4, 128).transpose(2, 0, 1))
    p["p_ba"] = f(inp["lru_b_a"].reshape(2, 4, 128).transpose(2, 0, 1))
    p["p_bx"] = f(inp["lru_b_x"].reshape(2, 4, 128).transpose(2, 0, 1))
    p["p_lam"] = f(inp["lru_lambda"].reshape(2, 4, 128).transpose(2, 0, 1))
    for nm, src in (("p_wa", inp["lru_w_a"]), ("p_wx", inp["lru_w_x"])):
        bd = np.zeros((2, 128, 4, 128), np.float32)
        for o in range(2):
            for j in range(4):
                bd[o, 0:64, j, 0:64] = src[o, 2 * j]
                bd[o, 64:128, j, 64:128] = src[o, 2 * j + 1]
        p[nm] = bd
    return p


def build(layers, nseq=NSEQ, debug_skip=()):
    if 'one' in debug_skip:
        nseq = 1
    nc = bass.Bass("TRN2", target_bir_lowering=False)
    consts = _consts()
    dram = {}

    def din(name, shape, dt=F32):
        dram[name] = nc.dram_tensor(name, list(shape), dt, kind="ExternalInput").ap()
        return dram[name]

    x_d = din("x", [nseq, SEQ, D])
    out_d = nc.dram_tensor("out", [nseq, SEQ, D], F32, kind="ExternalOutput").ap()
    dbg_d = nc.dram_tensor("dbg", [128, 8, SEQ], BF16, kind="ExternalOutput").ap() if "dbg" in debug_skip else None
    ewin = din("even_w_in", [2, 1024, 3584])
    ewout = din("even_w_out", [2, 1024, 1024])
    owin = din("odd_w_in", [2, 1024, 3072])
    owout = din("odd_w_out", [2, 1024, 1024])
    for k, v in consts.items():
        din(k, v.shape, BF16 if v.dtype == ml_dtypes.bfloat16 else F32)
    pshapes = {"p_pre": [128, 4, 8], "p_post": [128, 4, 8], "p_lbl": [128, 2, 4], "p_cw": [128, 2, 4, 4],
               "p_cb": [128, 2, 4], "p_ba": [128, 2, 4], "p_bx": [128, 2, 4], "p_lam": [128, 2, 4],
               "p_wa": [2, 128, 4, 128], "p_wx": [2, 128, 4, 128]}
    for k, s in pshapes.items():
        din(k, s)

    es = ExitStack()
    with es:
        P = Prog(nc, es)

        def sb(name, shape, dt):
            return es.enter_context(nc.sbuf_tensor(name, list(shape), dt))

        def psum(name, shape, dt):
            return es.enter_context(nc.psum_tensor(name, list(shape), dt))

        X = sb("X", [128, 8, SEQ], F32)
        hT = sb("hT", [128, 8, SEQ], BF16)
        mixT = sb("mixT", [128, 8, SEQ], BF16)
        wg = [sb(f"wg{i}", [128, 8, 512], BF16) for i in range(3)]
        NSTG = 4
        stg = [sb(f"stg{i}", [128, 512], F32) for i in range(NSTG)]
        idf = sb("idf", [128, 128], F32)
        cbf = sb("cbf", [128, 6, 128], BF16)
        cmask = sb("cmask", [128, 3, 128], F32)
        rope = sb("rope", [128, 2, 16, 32], F32)
        Gt = sb("Gt", [128, 2, 256], F32)
        gct = sb("gct", [128, 2], F32)
        resetm = sb("resetm", [128, 512], BF16)
        ckm = sb("ckm", [128, 128 // HC], BF16)
        pre32 = sb("pre32", [128, 4, 8], F32)
        post32 = sb("post32", [128, 4, 8], F32)
        lbt = sb("lbt", [128, 2, 4], F32)
        omlt = sb("omlt", [128, 2, 4], F32)
        cwt = sb("cwt", [128, 2, 4, 4], F32)
        cbt = sb("cbt", [128, 2, 4], F32)
        bat = sb("bat", [128, 2, 4], F32)
        bxt = sb("bxt", [128, 2, 4], F32)
        nbat = sb("nbat", [128, 2, 4], F32)
        nbxt = sb("nbxt", [128, 2, 4], F32)
        nspt = sb("nspt", [128, 2, 4], F32)
        nsp2t = sb("nsp2t", [128, 2, 4], F32)
        wabd = sb("wabd", [128, 2, 4, 128], BF16)
        wxbd = sb("wxbd", [128, 2, 4, 128], BF16)
        lblt = sb("lblt", [128, 2, 4], F32)
        ARENA = 32 * 1024
        arena = sb("arena", [128, ARENA // 4], F32)

        pj = [psum(f"pj{i}", [128, 512], F32) for i in range(2)]
        pm = [psum(f"pm{i}", [128, 512], F32) for i in range(5)]
        pT = psum("pT", [128, 1024], BF16)

        ident = cbf[:, 0, :]
        ones = cbf[:, 1, :]
        tri = cbf[:, 2, :]
        cpl = cbf[:, 3, :]

        class Arena:
            def __init__(self):
                self.off = 0

            def reset(self):
                self.off = 0

            def take(self, shape, dt):
                n = int(np.prod(shape))
                nbytes = n * (2 if dt == BF16 else 4)
                nbytes = (nbytes + 63) // 64 * 64
                assert self.off + nbytes <= ARENA, (self.off, nbytes)
                w0 = self.off // 4
                ap = arena[:, w0:w0 + nbytes // 4]
                self.off += nbytes
                if dt == BF16:
                    ap = ap.bitcast(BF16)
                return ap[:, 0:n]

        AR = Arena()
        _uid = [0]

        _lastoff = [0]
        need_bar = [False]
        old_tiles = []
        cur_tiles = []

        def tile(shape, dt):
            _lastoff[0] = AR.off
            ap = AR.take(shape, dt)
            if len(shape) == 2:
                ap = ap.rearrange("p (a b) -> p a b", a=shape[0], b=shape[1])
            elif len(shape) == 3:
                ap = ap.rearrange("p (a b c) -> p a b c", a=shape[0], b=shape[1], c=shape[2])
            nb_ = int(np.prod(shape)) * (2 if dt == BF16 else 4)
            key = ("t", _lastoff[0], nb_)
            lo, hi = _lastoff[0], _lastoff[0] + nb_
            pd = P.predeps.setdefault(key, {})
            for (bk, o2, n2) in old_tiles:
                if bk != key and o2 < hi and lo < o2 + n2:
                    for e2, i2 in P.base_acc.get(bk, {}).items():
                        if pd.get(e2, 0) < i2:
                            pd[e2] = i2
            cur_tiles.append((key, lo, nb_))
            return ap, key

        def phase_end(extra=(), sp_too=False):
            if sp_too or extra:
                P.barrier(engs=(PE, ACT, DVE, POOL, SP) if sp_too else (PE, ACT, DVE, POOL), extra=extra)
                old_tiles.clear()
            old_tiles.extend(cur_tiles)
            cur_tiles.clear()
            AR.reset()

        def mm(out, lhsT, rhs, start, stop, R, W, inc=True):
            P.op(PE, lambda q: q.matmul(out, lhsT=lhsT, rhs=rhs, start=start, stop=stop, skip_group_check=True),
                 reads=R, writes=W, inc=inc)

        def tr(out, in_, idn, R, W):
            P.op(PE, lambda q: q.transpose(out=out, in_=in_, identity=idn), reads=R, writes=W)

        def act(out, in_, func, R, W, scale=1.0, bias=0.0):
            P.op(ACT, lambda q: q.activation(out=out, in_=in_, func=func, bias=bias, scale=scale), reads=R, writes=W)

        def tt(eng, out, in0, in1, op, R, W):
            if eng == POOL and not POOL_COMPUTE:
                eng = DVE
            P.op(eng, lambda q: q.tensor_tensor(out=out, in0=in0, in1=in1, op=op), reads=R, writes=W)

        def ts(eng, out, in0, s1, s2, op0, op1, R, W):
            if op1 is None:
                P.op(eng, lambda q: q.tensor_scalar(out=out, in0=in0, scalar1=s1, scalar2=None, op0=op0), reads=R, writes=W)
            else:
                P.op(eng, lambda q: q.tensor_scalar(out=out, in0=in0, scalar1=s1, scalar2=s2, op0=op0, op1=op1), reads=R, writes=W)

        def stt(out, in0, scalar, in1, op0, op1, R, W):
            P.op(DVE, lambda q: q.scalar_tensor_tensor(out=out, in0=in0, scalar=scalar, in1=in1, op0=op0, op1=op1), reads=R, writes=W)

        def cp(eng, out, in_, R, W):
            if eng == POOL and not POOL_COMPUTE:
                eng = DVE
            if eng == ACT:
                P.op(ACT, lambda q: q.copy(out=out, in_=in_), reads=R, writes=W)
            else:
                P.op(eng, lambda q: q.tensor_copy(out=out, in_=in_), reads=R, writes=W)

        def recip(out, in_, R, W):
            P.op(DVE, lambda q: q.reciprocal(out=out, in_=in_), reads=R, writes=W)

        def scan(out, d0, d1, init, R, W):
            P.op(DVE, lambda q: q.tensor_tensor_scan(out=out, data0=d0, data1=d1, initial=init, op0=ALU.mult, op1=ALU.add), reads=R, writes=W)

        def memset(eng, ap, val, W):
            P.op(eng, lambda q: q.memset(ap, val), writes=W)

        def dma(eng, vq, out, in_, R, W):
            P.op(eng, lambda q: q.dma_start(out=out, in_=in_), reads=R, writes=W, vq=vq)

        cload = [(idf[:], "c_idf"), (cbf[:], "c_bf"), (cmask[:], "c_mask"), (rope[:], "c_rope"),
                 (Gt[:].rearrange("p a (b c) -> p a b c", b=4, c=64), "c_G"), (gct[:], "c_gc"), (resetm[:], "c_reset"),
                 (ckm[:], "c_cmask"), (pre32[:], "p_pre"), (post32[:], "p_post"), (lblt[:], "p_lbl"), (cwt[:], "p_cw"),
                 (cbt[:], "p_cb"), (bat[:], "p_ba"), (bxt[:], "p_bx"), (nspt[:], "p_lam")]
        for i, (dst, nm) in enumerate(cload):
            dma(SP, "dq_c", dst, dram[nm], [], ["consts"])
        for o in range(2):
            dma(POOL, "dq_c2", wabd[:, o, :, :], dram["p_wa"][o], [], ["consts2"])
            dma(POOL, "dq_c2", wxbd[:, o, :, :], dram["p_wx"][o], [], ["consts2"])
        CR = ["consts", "consts2"]
        memset(DVE, lbt[:, 0, :], 0.0, ["consts"])
        tt(DVE, lbt[:, 1, :], lblt[:, 1, :], lblt[:, 0, :], ALU.subtract, CR, ["consts"])
        act(lbt[:, 1, :], lbt[:, 1, :], AF.Exp, CR, ["consts"], scale=-1.0)
        ts(DVE, lbt[:, 1, :], lbt[:, 1, :], 1.0, None, ALU.add, None, CR, ["consts"])
        recip(lbt[:, 1, :], lbt[:, 1, :], CR, ["consts"])
        ts(DVE, omlt[:], lbt[:], -1.0, 1.0, ALU.mult, ALU.add, CR, ["consts"])
        ts(DVE, nbat[:], bat[:], -1.0, None, ALU.mult, None, CR, ["consts"])
        ts(DVE, nbxt[:], bxt[:], -1.0, None, ALU.mult, None, CR, ["consts"])
        act(nspt[:], nspt[:], AF.Exp, CR, ["consts"], scale=-1.0)
        act(nspt[:], nspt[:], AF.Ln, CR, ["consts"], bias=1.0)
        ts(DVE, nsp2t[:], nspt[:], -16.0, None, ALU.mult, None, CR, ["consts"])
        ts(DVE, nspt[:], nspt[:], -8.0, None, ALU.mult, None, CR, ["consts"])
        P.barrier()

        wstate = {"n": 0, "s": 0}

        def issue_load(pieces):
            slot = wstate["n"] % 3
            wstate["n"] += 1
            c = 0
            for (w2d, c0, ncols) in pieces:
                kk = 512 // ncols
                src = w2d[:, c0:c0 + ncols].rearrange("(k p) c -> p k c", p=128)
                for k0 in range(0, 8, kk):
                    si = wstate["s"] % NSTG
                    wstate["s"] += 1
                    st3 = stg[si][:, :].rearrange("p (k c) -> p k c", k=kk)
                    dma(SP, f"dq_s{si}", st3, src[:, k0:k0 + kk, :], [], [("stg", si)])
                    P.op(POOL, lambda q, o=wg[slot][:, k0:k0 + kk, c:c + ncols], i=st3: q.tensor_copy(out=o, in_=i),
                         reads=[("stg", si)], writes=[("wg", slot)])
                c += ncols
            return slot

        pjn = [0]

        def next_pj():
            pjn[0] += 1
            i = pjn[0] % 2
            return pj[i], ("pj", i)

        def proj_fm(slot, col0, g, bank, bkey, ncols=128):
            for k in range(8):
                mm(bank[0:ncols, :], wg[slot][:, k, col0:col0 + ncols], hT[:, k, g * 512:(g + 1) * 512], k == 0, k == 7,
                   [("wg", slot), "hT"], [bkey], inc=(k == 7))

        def proj_tm(slot, col0, ncols, n, out, bkey):
            for k in range(8):
                mm(out, hT[:, k, n * 128:(n + 1) * 128], wg[slot][:, k, col0:col0 + ncols], k == 0, k == 7,
                   [("wg", slot), "hT"], [bkey], inc=(k == 7))

        def load_x(s):
            xin = [tile([1024], F32) for _ in range(2)]
            for n in range(1 if "l1" in debug_skip else (3 if "l3" in debug_skip else 16)):
                xt, xk = xin[n % 2]
                dma(SP, f"dq_x{n % 2}", xt, x_d[s, n * 128:(n + 1) * 128, :], [], [xk])
                for half in range(2):
                    bank, bkey = next_pj()
                    for cc in range(4):
                        c = half * 4 + cc
                        tr(bank[:, cc * 128:(cc + 1) * 128], xt[:, c * 128:(c + 1) * 128], idf[:], [xk], [bkey])
                    dst = X[:, half * 4:half * 4 + 4, n * 128:(n + 1) * 128]
                    if "lnocp" in debug_skip:
                        continue
                    if "l2d" in debug_skip:
                        for cc in range(4):
                            dd = X[:, half * 4 + cc, n * 128:(n + 1) * 128] if "lsc" not in debug_skip else hT[:, half * 4 + cc, 0:256].bitcast(F32)
                            cp(ACT if half == 0 else DVE, dd, bank[:, cc * 128:(cc + 1) * 128], [bkey], [("X", half * 4 + cc)])
                    else:
                        cp(ACT if half == 0 else DVE, dst, bank[:, :].rearrange("p (a b) -> p a b", a=4), [bkey], [("X", half * 4 + i) for i in range(4)])
            phase_end()

        def store_x(s):
            xo = [tile([1024], F32) for _ in range(2)]
            for n in range(16):
                xt, xk = xo[n % 2]
                for half in range(2):
                    bank, bkey = next_pj()
                    for cc in range(4):
                        c = half * 4 + cc
                        tr(bank[:, cc * 128:(cc + 1) * 128], X[:, c, n * 128:(n + 1) * 128], idf[:], [("X", c)], [bkey])
                    cp(ACT if half == 0 else DVE, xt[:, half * 512:(half + 1) * 512], bank[:, :], [bkey], [xk])
                dma(SP, f"dq_o{n % 2}", out_d[s, n * 128:(n + 1) * 128, :], xt, [xk], [("outd", n % 2)])
            phase_end(extra=[("outd", 0), ("outd", 1)], sp_too=True)

        def rstd_from_ssq(dst, ssq_ps, n, R, W):
            act(dst, ssq_ps, AF.Ln, R, W, scale=1.0 / n, bias=EPS)
            act(dst, dst, AF.Exp, W, W, scale=-0.5)

        def pre_norm(L):
            sq = [tile([512], BF16) for _ in range(2)]
            rs = [tile([512], F32) for _ in range(2)]
            for g in range(4):
                sl = slice(g * 512, (g + 1) * 512)
                for c in range(8):
                    st, sk = sq[c % 2]
                    act(st, X[:, c, sl], AF.Square, [("X", c)], [sk])
                    mm(pm[4][:, :], ones, st, c == 0, c == 7, [sk], ["pm4"], inc=True)
                rt, rk = rs[g % 2]
                rstd_from_ssq(rt, pm[4][:, :], D, ["pm4"], [rk])
                for c in range(8):
                    stt(hT[:, c, sl], X[:, c, sl], pre32[:, L, c:c + 1], rt, ALU.mult, ALU.mult, [("X", c), rk, "consts"], ["hT"])
            phase_end()

        def out_proj(L, slots):
            if dbg_d is not None:
                dma(SP, "dq_dbg", dbg_d, mixT[:], [("mixT", c) for c in range(8)], ["dbgd"])
            Yg, yk = tile([8, 512], F32)
            sq = [tile([512], BF16) for _ in range(2)]
            rt, rk = tile([512], F32)
            tmp = [tile([512], F32) for _ in range(2)]
            for g in range(4):
                sl = slice(g * 512, (g + 1) * 512)
                for dc in range(8):
                    bank, bkey = next_pj()
                    slot = slots[dc // 4]
                    for k in range(8):
                        mm(bank[:, :], wg[slot][:, k, (dc % 4) * 128:(dc % 4 + 1) * 128], mixT[:, k, sl], k == 0, k == 7,
                           [("wg", slot), ("mixT", k)], [bkey], inc=(k == 7))
                    st, sk = sq[dc % 2]
                    cp(DVE, Yg[:, dc, :], bank[:, :], [bkey], [(yk, dc)])
                    act(st, bank[:, :], AF.Square, [bkey], [sk])
                    mm(pm[4][:, :], ones, st, dc == 0, dc == 7, [sk], ["pm4"], inc=True)
                if "op1" in debug_skip:
                    continue
                rstd_from_ssq(rt, pm[4][:, :], D, ["pm4"], [rk])
                if "op2" in debug_skip:
                    continue
                for dc in range(8):
                    tp, tk = tmp[dc % 2]
                    stt(tp, Yg[:, dc, :], post32[:, L, dc:dc + 1], rt, ALU.mult, ALU.mult, [(yk, dc), rk, "consts"], [tk])
                    tt(POOL, X[:, dc, sl], X[:, dc, sl], tp, ALU.add, [("X", dc), tk], [("X", dc)])
            phase_end()

        def gate_phase(slot, chunks):
            sg = [tile([512], BF16) for _ in range(2)]
            i = 0
            for ci, c in enumerate(chunks):
                for g in range(4):
                    sl = slice(g * 512, (g + 1) * 512)
                    bank, bkey = next_pj()
                    proj_fm(slot, ci * 128, g, bank, bkey)
                    st, sk = sg[i % 2]
                    i += 1
                    act(st, bank[:, :], AF.Silu, [bkey], [sk])
                    tt(DVE if (i % 2) else POOL, mixT[:, c, sl], mixT[:, c, sl], st, ALU.mult, [sk, ("mixT", c)], [("mixT", c)])
            phase_end()

        def retention_pair(pr, slot):
            qk_tm, k_qk = tile([16, 256], BF16)
            qkT, k_qkT = tile([2, SEQ], BF16)
            v_tm, k_v = tile([16, 256], BF16)
            qs = [tile([256], F32) for _ in range(1)]
            tmp = [[tile([4, 32], F32) for _ in range(4)] for _ in range(1)]
            for n in range(16):
                bank, bkey = next_pj()
                proj_tm(slot, 0, 256, n, bank[:, 0:256], bkey)
                qt, qk_ = qs[0]
                tt(DVE, qt, bank[:, 0:256], Gt[:, pr, :], ALU.mult, [bkey, "consts"], [qk_])
                q3 = qt.rearrange("p (a b) -> p a b", a=4)
                x1 = q3[:, :, 0:32]
                x2 = q3[:, :, 32:64]
                cosb = rope[:, 0, n, :].unsqueeze(1).to_broadcast([128, 4, 32])
                sinb = rope[:, 1, n, :].unsqueeze(1).to_broadcast([128, 4, 32])
                (t1, k1), (t2, k2), (t3, k3), (t4, k4) = tmp[0]
                o3 = qk_tm[:, n, :].rearrange("p (a b) -> p a b", a=4)
                tt(DVE, t1, x1, cosb, ALU.mult, [qk_, "consts"], [k1])
                tt(DVE, t2, x2, sinb, ALU.mult, [qk_, "consts"], [k2])
                tt(DVE, o3[:, :, 0:32], t1, t2, ALU.subtract, [k1, k2], [(k_qk, n)])
                tt(DVE, t3, x1, sinb, ALU.mult, [qk_, "consts"], [k3])
                tt(DVE, t4, x2, cosb, ALU.mult, [qk_, "consts"], [k4])
                tt(DVE, o3[:, :, 32:64], t3, t4, ALU.add, [k3, k4], [(k_qk, n)])
                bank2, bkey2 = next_pj()
                proj_tm(slot, 256, 256, n, bank2[:, 0:256], bkey2)
                cp(ACT, v_tm[:, n, :], bank2[:, 0:256], [bkey2], [(k_v, n)])
            if "r_a" in debug_skip:
                phase_end()
                return
            for n0 in range(0, 16, 4):
                for which in range(2):
                    for j in range(4):
                        n = n0 + j
                        tr(pT[:, which * 512 + j * 128: which * 512 + (j + 1) * 128], qk_tm[:, n, which * 128:(which + 1) * 128],
                           ident, [(k_qk, n), "consts"], ["pT"])
                cp(ACT, qkT[:, :, n0 * 128:(n0 + 4) * 128], pT[:, :].rearrange("p (a b) -> p a b", a=2), ["pT"], [(k_qkT, n0)])
            if "r_b" in debug_skip:
                phase_end()
                return
            Sst, k_S = tile([128], F32)
            SB, k_SB = tile([128], BF16)
            sTm = [tile([256], BF16) for _ in range(2)]
            sq = [tile([256], BF16) for _ in range(2)]
            rs = [tile([256], F32) for _ in range(2)]
            memset(DVE, Sst, 0.0, [k_S])
            for n in range(16):
                n0 = (n // 4) * 4
                csl = slice(n * 128, (n + 1) * 128)
                st, sk = sTm[n % 2]
                for hh in range(2):
                    ps_ = slice(hh * 64, (hh + 1) * 64)
                    sbank, skey = (pm[0], "pm0") if hh == 0 else (pm[4], "pm4")
                    mm(sbank[:, 0:128], qkT[ps_, 1, csl], qkT[ps_, 0, csl], True, True, [(k_qkT, n0)], [skey])
                    tt(DVE, st[:, hh * 128:(hh + 1) * 128], sbank[:, 0:128], cmask[:, 0, :], ALU.mult, [skey, "consts"], [sk])
                for hh in range(2):
                    if "r_c1" in debug_skip:
                        continue
                    mm(pm[1][hh * 64:(hh + 1) * 64, 0:128], qk_tm[:, n, 128 + hh * 64:128 + (hh + 1) * 64], v_tm[:, n, hh * 128:(hh + 1) * 128],
                       True, True, [(k_qk, n), (k_v, n)], ["pm1"], inc=(hh == 1))
                for hh in range(2):
                    ps_ = slice(hh * 64, (hh + 1) * 64)
                    osl = pm[2][:, hh * 128:(hh + 1) * 128]
                    mm(osl, v_tm[:, n, hh * 128:(hh + 1) * 128], st[:, hh * 128:(hh + 1) * 128], True, n == 0, [(k_v, n), sk], ["pm2"],
                       inc=(n == 0 and hh == 1))
                    if n > 0 and "r_c1" not in debug_skip:
                        mm(osl, SB[ps_, :], qkT[ps_, 0, csl], False, True, [k_SB, (k_qkT, n0)], ["pm2"], inc=(hh == 1))
                if "r_c1" not in debug_skip:
                    stt(Sst, Sst, gct[:, pr:pr + 1], pm[1][:, 0:128], ALU.mult, ALU.add, [k_S, "pm1", "consts"], [k_S])
                    act(SB, Sst, AF.Copy, [k_S, "consts"], [k_SB], scale=gct[:, pr:pr + 1])
                qt, qk2 = sq[n % 2]
                act(qt, pm[2][:, 0:256], AF.Square, ["pm2"], [qk2])
                mm(pm[3][:, 0:256], ones, qt, True, True, [qk2, "consts"], ["pm3"])
                rt, rk = rs[n % 2]
                rstd_from_ssq(rt, pm[3][:, 0:256], 128, ["pm3"], [rk])
                tt(DVE, mixT[:, 2 * pr:2 * pr + 2, csl], pm[2][:, 0:256].rearrange("p (a b) -> p a b", a=2),
                   rt.rearrange("p (a b) -> p a b", a=2), ALU.mult, ["pm2", rk], [("mixT", 2 * pr), ("mixT", 2 * pr + 1)])
            phase_end()

        def sb_pair(pr, slot):
            qT, k_q = tile([SEQ], BF16)
            kT, k_k = tile([SEQ], BF16)
            v_tm, k_v = tile([16, 128], BF16)
            for g in range(4):
                sl = slice(g * 512, (g + 1) * 512)
                bank, bkey = next_pj()
                proj_fm(slot, 0, g, bank, bkey)
                cp(ACT, qT[:, sl], bank[:, :], [bkey], [(k_q, g)])
                bank, bkey = next_pj()
                proj_fm(slot, 128, g, bank, bkey)
                act(kT[:, sl], bank[:, :], AF.Copy, [bkey], [(k_k, g)], scale=0.125)
            for n0 in range(0, 16, 4):
                bank, bkey = next_pj()
                for j in range(4):
                    proj_tm(slot, 256, 128, n0 + j, bank[:, j * 128:(j + 1) * 128], bkey)
                cp(DVE, v_tm[:, n0:n0 + 4, :], bank[:, :].rearrange("p (a b) -> p a b", a=4), [bkey], [(k_v, n0 // 4)])
            NB = 2
            e_t = [[tile([512], F32) for _ in range(NB)] for _ in range(2)]
            sp_t = [[tile([512], BF16) for _ in range(NB)] for _ in range(2)]
            ea_t = [[tile([512], BF16) for _ in range(NB)] for _ in range(2)]
            w_t = [[tile([512], BF16) for _ in range(NB)] for _ in range(2)]
            it = 0
            for G in range(4):
                gsl0 = G * 512
                started_P = [[False], [False]]
                started_O = [[False], [False]]
                blocks = list(range(4 * G + 3, -1, -1))

                def cols(b):
                    c0 = (b - 4 * G) * 128 if b >= 4 * G else 0
                    return c0

                def emit_Z(b):
                    c0 = cols(b)
                    for hh in range(2):
                        ps_ = slice(hh * 64, (hh + 1) * 64)
                        mm(pm[hh][:, c0:512], kT[ps_, b * 128:(b + 1) * 128], qT[ps_, gsl0 + c0:gsl0 + 512], True, True,
                           [(k_k, b // 4), (k_q, G)], [f"pm{hh}"])

                def acc(bank, started, lhsT, rhs_ap, c0, R, W, outp=slice(0, 128)):
                    mm(bank[outp, c0:512], lhsT, rhs_ap[:, c0:512], not started[0], True, R, W)
                    started[0] = True

                emit_Z(blocks[0])
                for bi, b in enumerate(blocks):
                    c0 = cols(b)
                    buf = it % NB
                    it += 1
                    diag = b >= 4 * G
                    for hh in range(2):
                        et, ek = e_t[hh][buf]
                        act(et[:, c0:512], pm[hh][:, c0:512], AF.Exp, [f"pm{hh}"], [ek])
                    if diag:
                        for hh in range(2):
                            et, ek = e_t[hh][buf]
                            tt(DVE, et[:, c0:c0 + 128], et[:, c0:c0 + 128], cmask[:, 1, :], ALU.mult, [ek, "consts"], [ek])
                    for hh in range(2):
                        et, ek = e_t[hh][buf]
                        st, sk = sp_t[hh][buf]
                        act(st[:, c0:512], et[:, c0:512], AF.Ln, [ek], [sk], bias=1.0)
                    for hh in range(2):
                        st, sk = sp_t[hh][buf]
                        acc(pm[2 + hh], started_P[hh], tri, st, c0, [sk, "consts"], [f"pm{2 + hh}"])
                    if bi + 1 < len(blocks):
                        emit_Z(blocks[bi + 1])
                    for hh in range(2):
                        at_, ak = ea_t[hh][buf]
                        act(at_[:, c0:512], pm[2 + hh][:, c0:512], AF.Exp, [f"pm{2 + hh}"], [ak], scale=-1.0)
                    for hh in range(2):
                        st, sk = sp_t[hh][buf]
                        acc(pm[2 + hh], started_P[hh], cpl, st, c0, [sk, "consts"], [f"pm{2 + hh}"])
                    for hh in range(2):
                        et, ek = e_t[hh][buf]
                        at_, ak = ea_t[hh][buf]
                        wt, wk = w_t[hh][buf]
                        tt(DVE, wt[:, c0:512], et[:, c0:512], at_[:, c0:512], ALU.mult, [ek, ak], [wk])
                    for hh in range(2):
                        wt, wk = w_t[hh][buf]
                        acc(pm[4], started_O[hh], v_tm[:, b, hh * 64:(hh + 1) * 64], wt, c0, [wk, (k_v, b // 4)], ["pm4"],
                            outp=slice(hh * 64, (hh + 1) * 64))
                cp(ACT, mixT[:, 4 + pr, gsl0:gsl0 + 512], pm[4][:, :], ["pm4"], [("mixT", 4 + pr)])
            phase_end()

        def hgrn_head(o, h, slot):
            NCK = 128 // HC
            qT, k_q = tile([SEQ], BF16)
            kT, k_k = tile([SEQ], BF16)
            dch, k_d = tile([SEQ // HC], F32)
            (a1, ka1), (a2, ka2), (a3, ka3) = tile([512], F32), tile([512], F32), tile([512], F32)
            lb_ap = lbt[:, o, h:h + 1]
            oml_ap = omlt[:, o, h:h + 1]
            for g in range(4):
                sl = slice(g * 512, (g + 1) * 512)
                bank, bkey = next_pj()
                proj_fm(slot, 128, g, bank, bkey)
                act(a1, bank[:, :], AF.Exp, [bkey], [ka1], scale=-1.0)
                act(a1, a1, AF.Ln, [ka1], [ka1], bias=1.0)
                act(a1, a1, AF.Exp, [ka1], [ka1], scale=-1.0)
                ts(DVE, a1, a1, oml_ap, lb_ap, ALU.mult, ALU.add, [ka1, "consts"], [ka1])
                ts(DVE, a2, a1, -1.0, 1.0, ALU.mult, ALU.add, [ka1], [ka2])
                act(a3, a1, AF.Ln, [ka1], [ka3])
                scan(a1, resetm[:, :], a3, 0.0, [ka3, "consts"], [ka1])
                act(a3, a1, AF.Exp, [ka1], [ka3])
                act(a1, a1, AF.Exp, [ka1], [ka1], scale=-1.0)
                bank2, bkey2 = next_pj()
                proj_fm(slot, 0, g, bank2, bkey2)
                tt(DVE, qT[:, sl], bank2[:, :], a3, ALU.mult, [bkey2, ka3], [(k_q, g)])
                tt(POOL, kT[:, sl], a2, a1, ALU.mult, [ka1, ka2], [(k_k, g)])
                nch = 512 // HC
                cp(ACT, dch[:, g * nch:(g + 1) * nch], a3.rearrange("p (c j) -> p c j", j=HC)[:, :, HC - 1], [ka3], [k_d])
            v4 = [tile([4, 128], BF16) for _ in range(2)]
            k4 = [tile([4, 128], BF16) for _ in range(2)]
            vt, vk = tile([NCK, 128], BF16)
            atm = [tile([128], BF16) for _ in range(2)]
            up, uk = tile([NCK, 128], F32)
            Sall = [tile([NCK, 128], F32) for _ in range(2)]
            Sbf = [tile([NCK, 128], BF16) for _ in range(2)]
            sq, k_sq = tile([512], BF16)
            rt, k_rt = tile([512], F32)
            for n in range(16):
                g4 = n // 4
                j4 = n % 4
                vt4, vk4 = v4[g4 % 2]
                kt4, kk4 = k4[g4 % 2]
                if j4 == 0:
                    bank, bkey = next_pj()
                    for j in range(4):
                        proj_tm(slot, 256, 128, n + j, bank[:, j * 128:(j + 1) * 128], bkey)
                    cp(ACT, vt4, bank[:, :].rearrange("p (a b) -> p a b", a=4), [bkey], [vk4])
                    for j in range(4):
                        tr(pT[:, j * 128:(j + 1) * 128], kT[:, (n + j) * 128:(n + j + 1) * 128], ident, [(k_k, g4), "consts"], ["pT"])
                    cp(DVE, kt4, pT[:, 0:512].rearrange("p (a b) -> p a b", a=4), ["pT"], [kk4])
                tsl = slice(n * 128, (n + 1) * 128)
                ob = pm[2 + g4 % 2]
                okey = f"pm{2 + g4 % 2}"
                ocol = j4 * 128
                mm(pm[0][:, 0:128], kT[:, tsl], qT[:, tsl], True, True, [(k_k, g4), (k_q, g4)], ["pm0"])
                am, ak = atm[n % 2]
                tt(DVE, am, pm[0][:, 0:128], cmask[:, 2, :], ALU.mult, ["pm0", "consts"], [ak])
                tt(DVE, vt, vt4[:, j4, :].unsqueeze(1).to_broadcast([128, NCK, 128]),
                   ckm[:, :].unsqueeze(2).to_broadcast([128, NCK, 128]), ALU.mult, [vk4, "consts"], [vk])
                mm(pm[1][:, 0:NCK * 128], kt4[:, j4, :], vt.rearrange("p a b -> p (a b)"), True, True, [kk4, vk], ["pm1"])
                tt(DVE, up, pm[1][:, 0:NCK * 128].rearrange("p (a b) -> p a b", a=NCK),
                   dch[:, n * NCK:(n + 1) * NCK].unsqueeze(2).to_broadcast([128, NCK, 128]), ALU.mult, ["pm1", k_d], [uk])
                sa, sak = Sall[n % 2]
                sprev, spk = Sall[(n - 1) % 2]
                for c in range(NCK):
                    cc = n * NCK + c
                    if cc == 0:
                        cp(DVE, sa[:, 0, :], up[:, 0, :], [uk], [sak])
                    else:
                        prev = sprev[:, NCK - 1, :] if c == 0 else sa[:, c - 1, :]
                        stt(sa[:, c, :], prev, dch[:, cc:cc + 1], up[:, c, :], ALU.mult, ALU.add, [uk, sak, spk, k_d], [sak])
                sb_, sbk = Sbf[n % 2]
                sbp, sbpk = Sbf[(n - 1) % 2]
                cp(ACT, sb_, sa, [sak], [sbk])
                mm(ob[:, ocol:ocol + 128], vt4[:, j4, :], am, True, False, [vk4, ak], [okey])
                for c in range(NCK):
                    cc = n * NCK + c
                    if cc == 0:
                        continue
                    lhs = sbp[:, NCK - 1, :] if c == 0 else sb_[:, c - 1, :]
                    mm(ob[:, ocol + c * HC: ocol + (c + 1) * HC], lhs, qT[:, n * 128 + c * HC: n * 128 + (c + 1) * HC], False, True,
                       [sbk, sbpk, (k_q, g4)], [okey])
                if j4 == 3:
                    n0 = n - 3
                    act(sq, ob[:, :], AF.Square, [okey], [k_sq])
                    mm(pm[4][:, :], ones, sq, True, True, [k_sq, "consts"], ["pm4"])
                    rstd_from_ssq(rt, pm[4][:, :], 128, ["pm4"], [k_rt])
                    tt(DVE, mixT[:, h, n0 * 128:(n0 + 4) * 128], ob[:, :], rt, ALU.mult, [okey, k_rt], [("mixT", h)])
            phase_end()

        def rglru_chunk(o, j, slot):
            lxp, k_lx = tile([SEQ + 4], F32)
            xc_t = [tile([512], F32) for _ in range(1)]
            xcb = [tile([512], BF16) for _ in range(1)]
            (r_t, k_r), (ig_t, k_ig) = tile([512], F32), tile([512], F32)
            (at_, ak), (mt, mk) = tile([512], F32), tile([512], F32)
            h_t = [tile([512], F32) for _ in range(2)]
            memset(DVE, lxp[:, 0:4], 0.0, [(k_lx, -1)])
            for g in range(4):
                bank, bkey = next_pj()
                proj_fm(slot, j * 128, g, bank, bkey)
                cp(ACT, lxp[:, 4 + g * 512: 4 + (g + 1) * 512], bank[:, :], [bkey], [(k_lx, g)])
            for g in range(4):
                sl = slice(g * 512, (g + 1) * 512)
                xc, xck = xc_t[0]
                R = [(k_lx, g), (k_lx, g - 1), "consts"]
                ts(DVE, xc, lxp[:, 1 + g * 512: 1 + (g + 1) * 512], cwt[:, o, 0, j:j + 1], cbt[:, o, j:j + 1], ALU.mult, ALU.add, R, [xck])
                for jj in range(1, 4):
                    stt(xc, lxp[:, 1 + jj + g * 512: 1 + jj + (g + 1) * 512], cwt[:, o, jj, j:j + 1], xc, ALU.mult, ALU.add,
                        R + [xck], [xck])
                xb, xbk = xcb[0]
                cp(ACT, xb, xc, [xck], [xbk])
                mm(pm[0][:, :], wabd[:, o, j, :], xb, True, True, [xbk, "consts2"], ["pm0"])
                mm(pm[1][:, :], wxbd[:, o, j, :], xb, True, True, [xbk, "consts2"], ["pm1"])
                act(r_t, pm[0][:, :], AF.Exp, ["pm0", "consts"], [k_r], scale=-1.0, bias=nbat[:, o, j:j + 1])
                act(ig_t, pm[1][:, :], AF.Exp, ["pm1", "consts"], [k_ig], scale=-1.0, bias=nbxt[:, o, j:j + 1])
                act(r_t, r_t, AF.Ln, [k_r], [k_r], bias=1.0)
                act(ig_t, ig_t, AF.Ln, [k_ig], [k_ig], bias=1.0)
                act(r_t, r_t, AF.Exp, [k_r], [k_r], scale=-1.0)
                act(ig_t, ig_t, AF.Exp, [k_ig], [k_ig], scale=-1.0)
                tt(POOL, ig_t, ig_t, xc, ALU.mult, [k_ig, xck], [k_ig])
                ht, hk = h_t[g % 2]
                hp, hpk = h_t[(g - 1) % 2]
                act(at_, r_t, AF.Exp, [k_r, "consts"], [ak], scale=nspt[:, o, j:j + 1])
                act(mt, r_t, AF.Exp, [k_r, "consts"], [mk], scale=nsp2t[:, o, j:j + 1])
                ts(DVE, mt, mt, -1.0, 1.0, ALU.mult, ALU.add, [mk], [mk])
                act(mt, mt, AF.Ln, [mk], [mk], bias=1e-30)
                act(mt, mt, AF.Exp, [mk], [mk], scale=0.5)
                if g == 0:
                    memset(DVE, mt[:, 0:1], 1.0, [mk])
                tt(POOL, mt, mt, ig_t, ALU.mult, [mk, k_ig], [mk])
                init = 0.0 if g == 0 else hp[:, 511:512]
                scan(ht, at_, mt, init, [ak, mk, hpk], [hk])
                cp(ACT, mixT[:, 4 + j, sl], ht, [hk], [("mixT", 4 + j)])
            phase_end()

        tasks = []

        def add(pieces, fn, kind=None):
            tasks.append((pieces, fn, kind))

        for s in range(nseq):
            if "io" not in debug_skip:
                add(None, lambda slot, s=s: load_x(s))
            for L in layers:
                if "nopre" not in debug_skip:
                    add(None, lambda slot, L=L: pre_norm(L))
                if L % 2 == 0:
                    e = L // 2
                    W = ewin[e]
                    if "ret" in debug_skip:
                        add(None, lambda slot: memset(DVE, mixT[:, 0:4, :], 1.0, [("mixT", c) for c in range(4)]))
                    if "sb" in debug_skip:
                        add(None, lambda slot: memset(DVE, mixT[:, 4:8, :], 1.0, [("mixT", c) for c in range(4, 8)]))
                    for pr in range(2):
                        if "ret" in debug_skip:
                            continue
                        add([(W, pr * 128, 128), (W, 256 + pr * 128, 128), (W, 512 + pr * 256, 256)],
                            lambda slot, pr=pr: retention_pair(pr, slot), 'ret')
                    if "nogate" not in debug_skip:
                        add([(W, 1024, 512)], lambda slot: gate_phase(slot, [0, 1, 2, 3]))
                    for pr in range(4):
                        if "sb" in debug_skip:
                            continue
                        add([(W, 1536 + pr * 128, 128), (W, 2048 + pr * 128, 128), (W, 2560 + pr * 128, 128)],
                            lambda slot, pr=pr: sb_pair(pr, slot), 'sb')
                    if "nogate" not in debug_skip:
                        add([(W, 3072, 512)], lambda slot: gate_phase(slot, [4, 5, 6, 7]))
                    WO = ewout[e]
                else:
                    o = L // 2
                    W = owin[o]
                    for h in range(4):
                        if "hgrn" in debug_skip:
                            continue
                        add([(W, h * 128, 128), (W, 512 + h * 128, 128), (W, 1024 + h * 128, 128)],
                            lambda slot, o=o, h=h: hgrn_head(o, h, slot), 'hgrn')
                    add([(W, 1536, 512)], lambda slot: gate_phase(slot, [0, 1, 2, 3]))
                    holder = {}
                    add([(W, 2048, 512)], lambda slot, holder=holder: holder.__setitem__("lx", slot))
                    for j in range(4):
                        if "lru" in debug_skip:
                            continue
                        add(None, lambda slot, o=o, j=j, holder=holder: rglru_chunk(o, j, holder["lx"]), 'lru')
                    add([(W, 2560, 512)], lambda slot: gate_phase(slot, [4, 5, 6, 7]))
                    WO = owout[o]
                if "noout" in debug_skip:
                    continue
                holder2 = {}
                add([(WO, 0, 512)], lambda slot, holder2=holder2: holder2.__setitem__("a", slot))
                add([(WO, 512, 512)], lambda slot, L=L, holder2=holder2: out_proj(L, [holder2["a"], slot]))
            if "io" not in debug_skip and "st" not in debug_skip:
                add(None, lambda slot, s=s: store_x(s))

        wtasks = [i for i, t in enumerate(tasks) if t[0] is not None]
        slots = {}
        nxt = 0

        def ensure_issued(upto):
            nonlocal nxt
            while nxt < len(wtasks) and nxt <= upto:
                ti = wtasks[nxt]
                slots[ti] = issue_load(tasks[ti][0])
                nxt += 1

        wpos = {ti: k for k, ti in enumerate(wtasks)}
        prev_kind = None
        for i, (pieces, fn, kind) in enumerate(tasks):
            if need_bar[0] and not (kind is not None and kind == prev_kind):
                P.barrier()
            need_bar[0] = False
            if pieces is not None:
                ensure_issued(wpos[i] + 1)
                fn(slots[i])
            else:
                fn(None)
            if need_bar[0]:
                prev_kind = kind
        if "io" in debug_skip or "st" in debug_skip:
            dma(SP, "dq_o0", out_d[0, 0:128, :], X[:, 0, 0:1024], [("X", 0)], [("outd", 0)])
        P.op(SP, lambda q: q.nop(), reads=[("outd", 0), ("outd", 1)])
        P.emit()
    return nc


_CACHE = {}


def _get_prog(layers):
    key = tuple(layers)
    if key not in _CACHE:
        _CACHE[key] = build(list(layers))
    return _CACHE[key]


def kernel(**inputs):
    x = np.ascontiguousarray(inputs["x"], dtype=np.float32)
    consts = _consts()
    params = _params(inputs)
    base = {k: np.ascontiguousarray(inputs[k], dtype=np.float32) for k in ("even_w_in", "even_w_out", "odd_w_in", "odd_w_out")}
    base.update(consts)
    base.update(params)
    cur = x.reshape(NCORES, NSEQ, SEQ, D)
    for layers in LAYER_GROUPS:
        nc = _get_prog(layers)
        in_maps = []
        for c in range(NCORES):
            m = dict(base)
            m["x"] = np.ascontiguousarray(cur[c])
            in_maps.append(m)
        res = run_bass_kernel_spmd(nc, in_maps, core_ids=list(range(NCORES)))
        cur = np.stack([np.asarray(r["out"], dtype=np.float32) for r in res.results], axis=0)
    return cur.reshape(NCORES * NSEQ, SEQ, D)
```
